# Optimizing a Trainium2 kernel written in Bass

```python
import math
import jax
import jax.numpy as jnp
from jax import lax
import numpy as np

D_MODEL = 1024
BATCH = 8
SEQ = 4096
DEPTH = 2

D_MIX = D_MODEL
SSM_WIDTH = D_MIX // 4
POOL_WIDTH = D_MIX // 4
ATT_WIDTH = D_MIX // 2
N_PROJ = SSM_WIDTH + POOL_WIDTH + 3 * ATT_WIDTH
SSM_GROUP = 16
SSM_GROUPS = SSM_WIDTH // SSM_GROUP
SSM_STATE = 64
SSM_DT_MIN = 1e-3
SSM_DT_MAX = 1e-1
POOL_WINDOWS = (2, 4, 8, 16)
POOL_GROUP = POOL_WIDTH // len(POOL_WINDOWS)
ATT_HEAD_DIM = 64
ATT_HEADS = ATT_WIDTH // ATT_HEAD_DIM
MOBA_BLOCK = 256
MOBA_TOPK = 3
MOBA_Q_CHUNK = 32
REL_BUCKETS = 32
REL_MAX_DIST = 128
MOE_GROUPS = 4
MOE_EXPERTS_PER_GROUP = 4
MOE_EXPERTS = MOE_GROUPS * MOE_EXPERTS_PER_GROUP
MOE_TOPK = 2
D_EXPERT = D_MODEL // 4
RMS_EPS = 1e-6
NEG_INF = -1e30

kernel_name = 'hybrid_s5_pool_moba_hmoe'


def rms_norm(x, g):
    xf = x.astype(jnp.float32)
    y = xf * lax.rsqrt(jnp.mean(xf * xf, axis=-1, keepdims=True) + RMS_EPS)
    return (y * g.astype(jnp.float32)).astype(x.dtype)


def t5_bucket(rel):
    n = jnp.maximum(rel, 0)
    max_exact = REL_BUCKETS // 2
    nf = jnp.maximum(n, 1).astype(jnp.float32)
    large = max_exact + (jnp.log(nf / max_exact) / math.log(REL_MAX_DIST / max_exact)
                         * (REL_BUCKETS - max_exact)).astype(jnp.int32)
    large = jnp.minimum(large, REL_BUCKETS - 1)
    return jnp.where(n < max_exact, n, large)


def ssm_mixer(u, a_re, a_im, log_dt, b_re, b_im, c_re, c_im, d, glu_w, glu_b):
    f32 = jnp.float32
    bsz, seq, _ = u.shape
    lam = lax.complex(a_re.astype(f32), a_im.astype(f32))
    dt = jnp.exp(log_dt.astype(f32))[:, None]
    lam_bar = jnp.exp(lam * dt)
    b = lax.complex(b_re.astype(f32), b_im.astype(f32))
    b_bar = ((lam_bar - 1.0) / lam)[..., None] * b
    c = lax.complex(c_re.astype(f32), c_im.astype(f32))
    uf = u.astype(f32)
    ug = uf.reshape(bsz, seq, SSM_GROUPS, SSM_GROUP).astype(jnp.complex64)
    bu = jnp.einsum('blgh,gph->blgp', ug, b_bar)
    a = jnp.broadcast_to(lam_bar, bu.shape)

    def combine(e1, e2):
        a1, b1 = e1
        a2, b2 = e2
        return a1 * a2, a2 * b1 + b2

    _, states = lax.associative_scan(combine, (a, bu), axis=1)
    y = jnp.einsum('blgp,ghp->blgh', states, c).real.reshape(bsz, seq, SSM_WIDTH)
    y = jax.nn.gelu(y + d.astype(f32) * uf)
    y = y * jax.nn.sigmoid(y @ glu_w.astype(f32) + glu_b.astype(f32))
    return y.astype(u.dtype)


def pool_mixer(p, w, b, scale):
    f32 = jnp.float32
    bsz, seq, _ = p.shape
    pf = p.astype(f32)
    cs = lax.cumsum(pf, axis=1)
    t = jnp.arange(seq)
    outs = []
    for gi, win in enumerate(POOL_WINDOWS):
        sl = slice(gi * POOL_GROUP, (gi + 1) * POOL_GROUP)
        csg = cs[..., sl]
        lag = jnp.pad(csg, ((0, 0), (win, 0), (0, 0)))[:, :seq]
        cnt = jnp.minimum(t + 1, win).astype(f32)[None, :, None]
        outs.append((csg - lag) / cnt - pf[..., sl])
    dlt = jnp.stack(outs, axis=2)
    y = jnp.einsum('blgc,gcd->blgd', dlt, w.astype(f32)).reshape(bsz, seq, POOL_WIDTH)
    y = (y + b.astype(f32)) * scale.astype(f32)
    return y.astype(p.dtype)


def moba_attention(q, k, v, rel_bias):
    f32 = jnp.float32
    bsz, seq, nh, dh = q.shape
    nb = -(-seq // MOBA_BLOCK)
    lp = nb * MOBA_BLOCK
    pad = lp - seq

    def to_bhld(t):
        return jnp.pad(t, ((0, 0), (0, pad), (0, 0), (0, 0))).transpose(0, 2, 1, 3)

    qh = to_bhld(q)
    kb = to_bhld(k).reshape(bsz, nh, nb, MOBA_BLOCK, dh)
    vb = to_bhld(v).reshape(bsz, nh, nb, MOBA_BLOCK, dh)
    kmean = jnp.mean(kb.astype(f32), axis=3)
    n_sel = min(MOBA_TOPK, nb - 1)
    bias_t = rel_bias.astype(f32).T
    b_ix = jnp.arange(bsz)[:, None, None, None]
    h_ix = jnp.arange(nh)[None, :, None, None]
    offs = jnp.arange(MOBA_BLOCK)
    scale = dh ** -0.5

    def chunk(ci):
        q0 = ci * MOBA_Q_CHUNK
        qc = lax.dynamic_slice_in_dim(qh, q0, MOBA_Q_CHUNK, axis=2).astype(f32)
        qpos = q0 + jnp.arange(MOBA_Q_CHUNK)
        qblk = q0 // MOBA_BLOCK
        k_own = lax.dynamic_index_in_dim(kb, qblk, axis=2, keepdims=False).astype(f32)
        v_own = lax.dynamic_index_in_dim(vb, qblk, axis=2, keepdims=False).astype(f32)
        rel_own = qpos[:, None] - (qblk * MOBA_BLOCK + offs)[None, :]
        s_own = jnp.einsum('bhqd,bhkd->bhqk', qc, k_own) * scale + bias_t[:, t5_bucket(rel_own)]
        s_own = jnp.where(rel_own >= 0, s_own, NEG_INF)
        if n_sel == 0:
            return jnp.einsum('bhqk,bhkd->bhqd', jax.nn.softmax(s_own, axis=-1), v_own)
        gate = jnp.einsum('bhqd,bhnd->bhqn', qc, kmean)
        gate = jnp.where(jnp.arange(nb) < qblk, gate, NEG_INF)
        _, sel = lax.top_k(gate, n_sel)
        k_sel = kb[b_ix, h_ix, sel].astype(f32)
        v_sel = vb[b_ix, h_ix, sel].astype(f32)
        rel_sel = qpos[:, None, None] - (sel[..., None] * MOBA_BLOCK + offs)
        s_sel = (jnp.einsum('bhqd,bhqnkd->bhqnk', qc, k_sel) * scale
                 + bias_t[h_ix[..., None], t5_bucket(rel_sel)])
        s_sel = jnp.where((sel < qblk)[..., None], s_sel, NEG_INF)
        logits = jnp.concatenate(
            [s_sel.reshape(bsz, nh, MOBA_Q_CHUNK, n_sel * MOBA_BLOCK), s_own], axis=-1)
        probs = jax.nn.softmax(logits, axis=-1)
        p_sel = probs[..., :n_sel * MOBA_BLOCK].reshape(bsz, nh, MOBA_Q_CHUNK, n_sel, MOBA_BLOCK)
        p_own = probs[..., n_sel * MOBA_BLOCK:]
        return (jnp.einsum('bhqnk,bhqnkd->bhqd', p_sel, v_sel)
                + jnp.einsum('bhqk,bhkd->bhqd', p_own, v_own))

    out = lax.map(chunk, jnp.arange(lp // MOBA_Q_CHUNK))
    out = out.transpose(1, 0, 3, 2, 4).reshape(bsz, lp, nh * dh)[:, :seq]
    return out.astype(q.dtype)


def hier_moe(h, group_w, group_b, router_w, router_b, w_gate, w_up, w_down):
    f32 = jnp.float32
    bsz, seq, dm = h.shape
    t = h.reshape(-1, dm)
    g_logits = (t @ group_w + group_b).astype(f32)
    g_prob = jax.nn.softmax(g_logits, axis=-1)
    g_sel = jnp.argmax(g_logits, axis=-1)
    g_wt = jnp.take_along_axis(g_prob, g_sel[:, None], axis=1)[:, 0]
    e_all = jnp.einsum('td,gde->tge', t, router_w) + router_b
    e_logits = jnp.take_along_axis(e_all, g_sel[:, None, None], axis=1)[:, 0].astype(f32)
    top_v, top_i = lax.top_k(e_logits, MOE_TOPK)
    top_w = jax.nn.softmax(top_v, axis=-1)
    within = jnp.sum(jax.nn.one_hot(top_i, MOE_EXPERTS_PER_GROUP, dtype=f32) * top_w[..., None], axis=1)
    comb = ((jax.nn.one_hot(g_sel, MOE_GROUPS, dtype=f32) * g_wt[:, None])[:, :, None]
            * within[:, None, :]).reshape(-1, MOE_EXPERTS)
    hg = jnp.einsum('td,edf->tef', t, w_gate)
    hu = jnp.einsum('td,edf->tef', t, w_up)
    act = jax.nn.silu(hg) * hu * comb[..., None]
    y = jnp.einsum('tef,efd->td', act, w_down)
    return y.reshape(bsz, seq, dm).astype(h.dtype)


def setup_inputs(seed: int = 0) -> dict:
    key = jax.random.key(seed)
    ks = jax.random.split(key, 32)
    f32 = jnp.float32

    def nrm(k, shape, s):
        return jax.random.normal(k, shape, f32) * s

    L, G, P, H = DEPTH, SSM_GROUPS, SSM_STATE, SSM_GROUP
    x = nrm(ks[0], (BATCH, SEQ, D_MODEL), 1.0)
    rel_bias = nrm(ks[1], (REL_BUCKETS, ATT_HEADS), 0.5)
    norm1_g = 1.0 + nrm(ks[2], (L, D_MODEL), 0.02)
    w_in = nrm(ks[3], (L, D_MODEL, N_PROJ), D_MODEL ** -0.5)
    ssm_a_re = -0.5 + nrm(ks[4], (L, G, P), 0.01)
    ssm_a_im = jnp.pi * jnp.arange(P, dtype=f32)[None, None, :] + nrm(ks[5], (L, G, P), 0.01)
    ssm_log_dt = jax.random.uniform(ks[6], (L, G), f32, math.log(SSM_DT_MIN), math.log(SSM_DT_MAX))
    ssm_b_re = nrm(ks[7], (L, G, P, H), (2.0 * H) ** -0.5)
    ssm_b_im = nrm(ks[8], (L, G, P, H), (2.0 * H) ** -0.5)
    ssm_c_re = nrm(ks[9], (L, G, H, P), (2.0 * P) ** -0.5 * 4.0)
    ssm_c_im = nrm(ks[10], (L, G, H, P), (2.0 * P) ** -0.5 * 4.0)
    ssm_d = nrm(ks[11], (L, SSM_WIDTH), 1.0)
    ssm_glu_w = nrm(ks[12], (L, SSM_WIDTH, SSM_WIDTH), SSM_WIDTH ** -0.5)
    ssm_glu_b = nrm(ks[13], (L, SSM_WIDTH), 0.02)
    pool_w = nrm(ks[14], (L, len(POOL_WINDOWS), POOL_GROUP, POOL_GROUP), POOL_GROUP ** -0.5)
    pool_b = nrm(ks[15], (L, POOL_WIDTH), 0.02)
    pool_scale = 1.0 + nrm(ks[16], (L, POOL_WIDTH), 0.02)
    w_out = nrm(ks[17], (L, D_MIX, D_MODEL), D_MIX ** -0.5)
    norm2_g = 1.0 + nrm(ks[18], (L, D_MODEL), 0.02)
    moe_group_w = nrm(ks[19], (L, D_MODEL, MOE_GROUPS), D_MODEL ** -0.5)
    moe_group_b = nrm(ks[20], (L, MOE_GROUPS), 0.01)
    moe_router_w = nrm(ks[21], (L, MOE_GROUPS, D_MODEL, MOE_EXPERTS_PER_GROUP), D_MODEL ** -0.5)
    moe_router_b = nrm(ks[22], (L, MOE_GROUPS, MOE_EXPERTS_PER_GROUP), 0.01)
    moe_w_gate = nrm(ks[23], (L, MOE_EXPERTS, D_MODEL, D_EXPERT), D_MODEL ** -0.5)
    moe_w_up = nrm(ks[24], (L, MOE_EXPERTS, D_MODEL, D_EXPERT), D_MODEL ** -0.5)
    moe_w_down = nrm(ks[25], (L, MOE_EXPERTS, D_EXPERT, D_MODEL), D_EXPERT ** -0.5)
    final_norm_g = 1.0 + nrm(ks[26], (D_MODEL,), 0.02)
    return {'x': x, 'rel_bias': rel_bias, 'norm1_g': norm1_g, 'w_in': w_in,
            'ssm_a_re': ssm_a_re, 'ssm_a_im': ssm_a_im, 'ssm_log_dt': ssm_log_dt,
            'ssm_b_re': ssm_b_re, 'ssm_b_im': ssm_b_im, 'ssm_c_re': ssm_c_re, 'ssm_c_im': ssm_c_im,
            'ssm_d': ssm_d, 'ssm_glu_w': ssm_glu_w, 'ssm_glu_b': ssm_glu_b,
            'pool_w': pool_w, 'pool_b': pool_b, 'pool_scale': pool_scale, 'w_out': w_out,
            'norm2_g': norm2_g, 'moe_group_w': moe_group_w, 'moe_group_b': moe_group_b,
            'moe_router_w': moe_router_w, 'moe_router_b': moe_router_b,
            'moe_w_gate': moe_w_gate, 'moe_w_up': moe_w_up, 'moe_w_down': moe_w_down,
            'final_norm_g': final_norm_g}


def reference(x, rel_bias, norm1_g, w_in, ssm_a_re, ssm_a_im, ssm_log_dt, ssm_b_re, ssm_b_im,
              ssm_c_re, ssm_c_im, ssm_d, ssm_glu_w, ssm_glu_b, pool_w, pool_b, pool_scale,
              w_out, norm2_g, moe_group_w, moe_group_b, moe_router_w, moe_router_b,
              moe_w_gate, moe_w_up, moe_w_down, final_norm_g):
    bsz, seq, _ = x.shape
    s0 = SSM_WIDTH
    s1 = s0 + POOL_WIDTH
    s2 = s1 + ATT_WIDTH
    s3 = s2 + ATT_WIDTH
    for l in range(DEPTH):
        h = rms_norm(x, norm1_g[l])
        proj = h @ w_in[l]
        y_ssm = ssm_mixer(proj[..., :s0], ssm_a_re[l], ssm_a_im[l], ssm_log_dt[l],
                          ssm_b_re[l], ssm_b_im[l], ssm_c_re[l], ssm_c_im[l],
                          ssm_d[l], ssm_glu_w[l], ssm_glu_b[l])
        y_pool = pool_mixer(proj[..., s0:s1], pool_w[l], pool_b[l], pool_scale[l])
        att_shape = (bsz, seq, ATT_HEADS, ATT_HEAD_DIM)
        y_att = moba_attention(proj[..., s1:s2].reshape(att_shape),
                               proj[..., s2:s3].reshape(att_shape),
                               proj[..., s3:].reshape(att_shape), rel_bias)
        x = x + jnp.concatenate([y_ssm, y_pool, y_att], axis=-1) @ w_out[l]
        h = rms_norm(x, norm2_g[l])
        x = x + hier_moe(h, moe_group_w[l], moe_group_b[l], moe_router_w[l], moe_router_b[l],
                         moe_w_gate[l], moe_w_up[l], moe_w_down[l])
    return rms_norm(x, final_norm_g)
```

```python
import math
from contextlib import ExitStack

import numpy as np
import concourse.bass as bass
import concourse.mybir as mybir
from concourse.bass_utils import run_bass_kernel_spmd

F32 = mybir.dt.float32
BF16 = mybir.dt.bfloat16
ALU = mybir.AluOpType
AF = mybir.ActivationFunctionType
AX = mybir.AxisListType

SEQ = 4096
DM = 1024
NL = 2
EPS = 1e-6
NEG = -30000.0

ENGINES = ("pe", "act", "dve", "pool", "sp")
N_DMA_SEMS = 24


class _Op:
    __slots__ = ("eng", "fn", "deps", "is_dma", "needs_inc", "inc_idx", "dsem", "dval", "gidx")

    def __init__(self, eng, fn, is_dma, gidx):
        self.eng = eng
        self.fn = fn
        self.deps = []
        self.is_dma = is_dma
        self.needs_inc = False
        self.inc_idx = 0
        self.dsem = None
        self.dval = 0
        self.gidx = gidx


class Sched:
    uid = 0

    def __init__(self, nc):
        self.nc = nc
        self.ops = {e: [] for e in ENGINES}
        self.last_write = {}
        self.readers = {}
        self.n = 0
        self.dma_count = 0
        self.dma_last = {}

    def _add(self, eng, fn, reads, writes, is_dma):
        op = _Op(eng, fn, is_dma, self.n)
        self.n += 1
        deps = {}
        raw = set()
        for k in reads:
            w = self.last_write.get(k)
            if w is not None:
                deps[w.gidx] = w
                raw.add(w.gidx)
        for k in writes:
            w = self.last_write.get(k)
            if w is not None:
                deps[w.gidx] = w
            for r in self.readers.get(k, ()):
                deps[r.gidx] = r
        if is_dma:
            j = self.dma_count % N_DMA_SEMS
            self.dma_count += 1
            prev = self.dma_last.get(j)
            if prev is not None:
                deps[prev.gidx] = prev
                op.dval = prev.dval + 16
            else:
                op.dval = 16
            op.dsem = j
            self.dma_last[j] = op
        deps.pop(op.gidx, None)
        op.deps = [(dd, (g in raw) and eng != "pe") for g, dd in deps.items()]
        for k in writes:
            self.last_write[k] = op
            self.readers[k] = []
        for k in reads:
            if k in writes:
                continue
            self.readers.setdefault(k, []).append(op)
        self.ops[eng].append(op)
        return op

    def op(self, eng, fn, reads=(), writes=()):
        return self._add(eng, fn, tuple(reads), tuple(writes), False)

    def dma(self, eng, out, in_, r=(), w=()):
        return self._add(eng, lambda e: e.dma_start(out=out, in_=in_), tuple(r), tuple(w), True)

    def emit(self):
        nc = self.nc
        for e in ENGINES:
            for op in self.ops[e]:
                for d, raw in op.deps:
                    if (not d.is_dma) and (d.eng != op.eng or raw):
                        d.needs_inc = True
        for e in ENGINES:
            c = 0
            for op in self.ops[e]:
                if op.needs_inc and not op.is_dma:
                    c += 1
                    op.inc_idx = c
        Sched.uid += 1
        esem = {e: nc.alloc_semaphore(name="s%d_%s" % (Sched.uid, e)) for e in ENGINES}
        dsem = [nc.alloc_semaphore(name="d%d_%d" % (Sched.uid, j)) for j in range(N_DMA_SEMS)]
        with ExitStack() as es:
            block = es.enter_context(nc.Block())
            engobj = {"pe": "tensor", "act": "scalar", "dve": "vector", "pool": "gpsimd", "sp": "sync"}

            def make(e):
                def body(eng):
                    waited = {}
                    for op in self.ops[e]:
                        for d, raw in op.deps:
                            if d.is_dma:
                                key = ("d", d.dsem)
                                val = d.dval
                                sem = dsem[d.dsem]
                            else:
                                if d.eng == e and not raw:
                                    continue
                                key = ("e", d.eng)
                                val = d.inc_idx
                                sem = esem[d.eng]
                            if waited.get(key, 0) >= val:
                                continue
                            waited[key] = val
                            eng.wait_ge(sem, val)
                        ins = op.fn(eng)
                        if op.is_dma:
                            ins.then_inc(dsem[op.dsem], 16)
                        elif op.needs_inc:
                            ins.then_inc(esem[e], 1)
                    if e == "sp":
                        for d in self.dma_last.values():
                            key = ("d", d.dsem)
                            if waited.get(key, 0) >= d.dval:
                                continue
                            waited[key] = d.dval
                            eng.wait_ge(dsem[d.dsem], d.dval)
                return body

            for e in ENGINES:
                getattr(block, engobj[e])(make(e))
        nc.clear_and_free_semaphores(list(esem.values()) + dsem)
        nc.all_engine_barrier()


def MM(S, out, lhsT, rhs, start=True, stop=True, r=(), w=()):
    S.op("pe", lambda e: e.matmul(out, lhsT, rhs, start=start, stop=stop), r, w)


def TR(S, out, in_, ident, r=(), w=()):
    S.op("pe", lambda e: e.transpose(out, in_, ident), r, w)


def ACT(S, out, in_, func, r=(), w=(), **kw):
    S.op("act", lambda e: e.activation(out=out, in_=in_, func=func, **kw), r, w)


def TT(S, out, in0, in1, op, r=(), w=(), eng="dve"):
    S.op(eng, lambda e: e.tensor_tensor(out=out, in0=in0, in1=in1, op=op), r, w)


def TS(S, out, in0, s1, s2, op0, op1=None, r=(), w=(), eng="dve"):
    if op1 is None:
        S.op(eng, lambda e: e.tensor_scalar(out=out, in0=in0, scalar1=s1, scalar2=None, op0=op0), r, w)
    else:
        S.op(eng, lambda e: e.tensor_scalar(out=out, in0=in0, scalar1=s1, scalar2=s2, op0=op0, op1=op1), r, w)


def STT(S, out, in0, scalar, in1, op0, op1, r=(), w=()):
    S.op("dve", lambda e: e.scalar_tensor_tensor(out=out, in0=in0, scalar=scalar, in1=in1, op0=op0, op1=op1), r, w)


def CP(S, out, in_, r=(), w=(), eng="dve"):
    S.op(eng, lambda e: e.tensor_copy(out=out, in_=in_), r, w)


def MSET(S, ap, val, w=(), eng="pool"):
    S.op(eng, lambda e: e.memset(ap, val), (), w)


def _t5_bucket_np(rel):
    rel = np.asarray(rel, dtype=np.int64)
    n = np.maximum(rel, 0)
    max_exact = 16
    nf = np.maximum(n, 1).astype(np.float32)
    large = max_exact + (np.log(nf / np.float32(max_exact)) / np.float32(math.log(128 / max_exact))
                         * np.float32(32 - max_exact)).astype(np.int32)
    large = np.minimum(large, 31)
    return np.where(n < max_exact, n, large)


def _consts():
    c = {}
    c["ident"] = np.eye(128, dtype=np.float32)
    c["exch"] = np.eye(128, dtype=np.float32)[::-1].copy()
    rel = np.arange(512) - 127
    b = _t5_bucket_np(rel)
    oh = np.zeros((32, 512), np.float32)
    oh[b[rel >= 0], np.arange(512)[rel >= 0]] = 1.0
    ohm = np.zeros((128, 512), np.float32)
    ohm[0:32] = oh
    ohm[32, rel < 0] = NEG
    c["ohm"] = ohm
    negm = np.zeros((128, 16, 16), np.float32)
    for qb in range(16):
        negm[:, qb, qb:] = -1e30
    c["negm"] = negm
    wins = (2, 4, 8, 16)
    winv = np.zeros((128, 2), np.float32)
    invc = np.zeros((128, 2, 16), np.float32)
    t = np.arange(16)
    for ct in range(2):
        for half in range(2):
            w = wins[2 * ct + half]
            winv[64 * half:64 * half + 64, ct] = 1.0 / w
            invc[64 * half:64 * half + 64, ct, :] = 1.0 / np.minimum(t + 1, w)
    c["winv"] = winv
    c["invc"] = invc
    esel = np.zeros((32, 16, 128), np.float32)
    for e in range(16):
        esel[e, e, :] = 1.0
        esel[16 + e, e, :] = 1.0
    c["esel"] = esel
    c["neghalfpi"] = np.full((128, 1), math.pi / 2, np.float32)
    return c


CONST_SHAPES = {"ident": [128, 128], "exch": [128, 128], "ohm": [128, 512],
                "negm": [128, 16, 16], "winv": [128, 2], "invc": [128, 2, 16], "esel": [32, 16, 128],
                "neghalfpi": [128, 1]}


def _host_layouts(inp):
    o = {}
    rep = lambda v: np.ascontiguousarray(np.broadcast_to(v[:, None, :], (v.shape[0], 128, v.shape[1])), np.float32)
    o["g1b"] = rep(inp["norm1_g"])
    o["g2b"] = rep(inp["norm2_g"])
    o["gfb"] = np.ascontiguousarray(np.broadcast_to(inp["final_norm_g"][None, :], (128, DM)), np.float32)
    rbp = np.zeros((128, 128), np.float32)
    rbp[0:32, 0:8] = inp["rel_bias"]
    rbp[32, 0:8] = 1.0
    o["rb"] = rbp
    o["c31b"] = np.ascontiguousarray(np.broadcast_to(inp["rel_bias"][31][None, :], (128, 8)), np.float32)
    def sm(a):
        return np.ascontiguousarray(a.reshape(NL, 8, 2, 64).transpose(0, 2, 3, 1).reshape(NL, 128, 8), np.float32)
    ldt = np.broadcast_to(inp["ssm_log_dt"][:, :, None], (NL, 16, 64))
    o["ssmA"] = np.ascontiguousarray(np.stack([sm(inp["ssm_a_re"]), sm(inp["ssm_a_im"]), sm(ldt)], axis=-1))
    def bexp(b):
        out = np.zeros((NL, 128, 8, 128), np.float32)
        for g in range(16):
            P, two = g // 2, g % 2
            cl = (g % 8) * 16
            out[:, two * 64:(two + 1) * 64, P, cl:cl + 16] = b[:, g]
        return out
    def cexp(cm):
        out = np.zeros((NL, 128, 8, 128), np.float32)
        for g in range(16):
            P, two = g // 2, g % 2
            cl = (g % 8) * 16
            out[:, two * 64:(two + 1) * 64, P, cl:cl + 16] = cm[:, g].transpose(0, 2, 1)
        return out
    o["bexp"] = np.stack([bexp(inp["ssm_b_re"]), bexp(inp["ssm_b_im"])], axis=1)
    o["cexp"] = np.stack([cexp(inp["ssm_c_re"]), cexp(inp["ssm_c_im"])], axis=1)
    chm = lambda v: np.ascontiguousarray(v.reshape(NL, 2, 128).transpose(0, 2, 1), np.float32)
    o["chp"] = np.ascontiguousarray(np.stack([chm(inp["ssm_d"]), chm(inp["ssm_glu_b"]), chm(inp["pool_b"]),
                                              chm(inp["pool_scale"])], axis=-1))
    pw = np.zeros((NL, 2, 128, 128), np.float32)
    for ct in range(2):
        for half in range(2):
            pw[:, ct, 64 * half:64 * half + 64, 64 * half:64 * half + 64] = inp["pool_w"][:, 2 * ct + half]
    o["poolw"] = pw
    wr = np.concatenate([inp["moe_group_w"], inp["moe_router_w"].transpose(0, 2, 1, 3).reshape(NL, DM, 16)], axis=-1)
    o["wr"] = np.ascontiguousarray(wr, np.float32)
    br = np.concatenate([inp["moe_group_b"], inp["moe_router_b"].reshape(NL, 16)], axis=-1)
    o["brb"] = np.ascontiguousarray(np.broadcast_to(br[:, None, :], (NL, 128, 20)), np.float32)
    return o


LAYOUT_SHAPES = {"g1b": [NL, 128, DM], "g2b": [NL, 128, DM], "gfb": [128, DM], "rb": [128, 128], "c31b": [128, 8],
                 "ssmA": [NL, 128, 8, 3], "bexp": [NL, 2, 128, 8, 128], "cexp": [NL, 2, 128, 8, 128],
                 "chp": [NL, 128, 2, 4], "poolw": [NL, 2, 128, 128], "wr": [NL, DM, 20], "brb": [NL, 128, 20]}
RAW_SHAPES = {"x": [SEQ, DM], "w_in": [NL, DM, 2048], "ssm_glu_w": [NL, 256, 256], "w_out": [NL, DM, DM],
              "moe_w_gate": [NL, 16, DM, 256], "moe_w_up": [NL, 16, DM, 256], "moe_w_down": [NL, 16, 256, DM]}


class Prog:
    def __init__(self, n_layers=NL, stop_after=None, dbg=()):
        self.nc = nc = bass.Bass("TRN2", target_bir_lowering=False)
        self.n_layers = n_layers
        self.stop_after = stop_after
        self.dbg = set(dbg)
        self.d = {}
        for name, shp in list(RAW_SHAPES.items()) + list(LAYOUT_SHAPES.items()) + list(CONST_SHAPES.items()):
            self.d[name] = nc.dram_tensor(name, shp, F32, kind="ExternalInput").ap()
        self.out = nc.dram_tensor("out", [SEQ, DM], F32, kind="ExternalOutput").ap()
        self.xmid = nc.dram_tensor("xmid", [SEQ, DM], F32).ap()
        self.x1 = nc.dram_tensor("x1", [SEQ, DM], F32).ap()
        self.qT_d = nc.dram_tensor("qT_d", [4, 128, SEQ], BF16).ap()
        self.Fd = nc.dram_tensor("Fd", [8, 512], F32).ap()
        self.dbg_out = {}

    def nm(self, name):
        self.uid = getattr(self, "uid", 0) + 1
        return "t%d_%s" % (self.uid, name)

    def dbg_tensor(self, name, shape, dt=F32):
        t = self.nc.dram_tensor("dbg_" + name, shape, dt, kind="ExternalOutput").ap()
        self.dbg_out[name] = t
        return t

    def norm_tile(self, S, T, x_ap, xkey, g_tile, hT_dst, hT_key, idx):
        self.norm_tile_a(S, T, x_ap, xkey, g_tile, idx)
        self.norm_tile_b(S, T, hT_dst, hT_key, idx)

    def norm_tile_a(self, S, T, x_ap, xkey, g_tile, idx):
        b = idx % 2
        junk, ss, h = T.get("junk"), T["ss"], T["h"]
        if junk is None:
            ACT(S, h[:, b, :], x_ap, AF.Square, r=[xkey], w=[("h", b), ("ss", b)], accum_out=ss[:, b, 0:1])
        else:
            ACT(S, junk[:, 0, :], x_ap, AF.Square, r=[xkey], w=["junk", ("ss", b)], accum_out=ss[:, b, 0:1])
        TS(S, ss[:, b, 1:2], ss[:, b, 0:1], 1.0 / DM, EPS, ALU.mult, ALU.add, r=[("ss", b)], w=[("ss1", b)])
        ACT(S, ss[:, b, 2:3], ss[:, b, 1:2], AF.Sqrt, r=[("ss1", b)], w=[("ss2", b)])
        S.op("dve", lambda e: e.reciprocal(out=ss[:, b, 3:4], in_=ss[:, b, 2:3]), [("ss2", b)], [("ss3", b)])
        STT(S, h[:, b, :], x_ap, ss[:, b, 3:4], g_tile[:], ALU.mult, ALU.mult, r=[xkey, ("ss3", b), "g"], w=[("h", b)])

    def norm_tile_b(self, S, T, hT_dst, hT_key, idx):
        b = idx % 2
        h, pst = T["h"], T["pst"]
        for kc in range(8):
            TR(S, pst[:, b, kc, :], h[:, b, kc * 128:(kc + 1) * 128], T["ident_bf"][:],
               r=[("h", b), "ident_bf"], w=[("pst", b)])
        if idx % 2 == 0:
            ACT(S, hT_dst, pst[:, b, :, :], AF.Copy, r=[("pst", b)], w=[hT_key])
        else:
            CP(S, hT_dst, pst[:, b, :, :], r=[("pst", b)], w=[hT_key])

    def alloc_norm(self, es, T, junk=True):
        nc = self.nc
        if junk:
            T["junk"] = es.enter_context(nc.sbuf_tensor(self.nm("junk"), [128, 1, DM], BF16))
        T["ss"] = es.enter_context(nc.sbuf_tensor(self.nm("ss"), [128, 2, 4], F32))
        T["h"] = es.enter_context(nc.sbuf_tensor(self.nm("h"), [128, 2, DM], BF16))
        T["pst"] = es.enter_context(nc.psum_tensor(self.nm("pst"), [128, 2, 8, 128], BF16))
        T["ident_bf"] = es.enter_context(nc.sbuf_tensor(self.nm("ident_bf"), [128, 128], BF16))

    def load_ident(self, S, T):
        S.dma("pool", T["ident_bf"][:], self.d["ident"], w=["ident_bf"])

    def block_B(self, l, x_in, x_out, final):
        nc, d = self.nc, self.d
        with ExitStack() as es:
            sb = lambda name, shape, dt=F32: es.enter_context(nc.sbuf_tensor(self.nm(name), shape, dt))
            T = {}
            self.alloc_norm(es, T)
            wd = sb("wd", [128, 32, DM], BF16)
            actall = sb("actall", [128, 32, 1024], BF16)
            h2T = sb("h2T", [128, 8, 1024], BF16)
            wg = sb("wg", [128, 2, 8, 256], BF16)
            wu = sb("wu", [128, 2, 8, 256], BF16)
            g2 = sb("g2", [128, DM])
            gf = sb("gf", [128, DM]) if final else None
            xt = sb("xt", [128, 2, DM])
            wr = sb("wr", [128, 8, 20], BF16)
            brb = sb("brb", [128, 20])
            esel = sb("esel", [32, 16, 128], BF16)
            combT = sb("combT", [32, 1024], BF16)
            rt = sb("rt", [128, 2, 96])
            hl = sb("hl", [128, 2, 32], BF16)
            cb = sb("cb", [128, 2, 512])
            sg = sb("sg", [128, 2, 512])
            fin = sb("fin", [128, 2, 4]) if final else None
            psA = [es.enter_context(nc.psum_tensor(self.nm("psA"), [128, 512], F32)) for i in range(2)]
            psB = [es.enter_context(nc.psum_tensor(self.nm("psB"), [128, 512], F32)) for i in range(2)]
            psC = es.enter_context(nc.psum_tensor(self.nm("psC"), [128, 512], F32))
            psL = es.enter_context(nc.psum_tensor(self.nm("psL"), [128, 512], F32))
            S = Sched(nc)
            self.load_ident(S, T)
            S.dma("sp", g2[:], d["g2b"][l], w=["g"])
            if final:
                S.dma("sp", gf[:], d["gfb"], w=["gf"])
            S.dma("sp", brb[:], d["brb"][l], w=["brb"])
            S.dma("pool", wr[:], d["wr"][l].rearrange("(kc p) n -> p kc n", p=128), w=["wr"])
            S.dma("pool", esel[:], d["esel"], w=["esel"])
            wdv = d["moe_w_down"][l].rearrange("e (ft p) d -> p e ft d", p=128)
            wd4 = wd[:].rearrange("p (e ft) d -> p e ft d", ft=2)

            def load_wd():
                for e4 in range(8):
                    S.dma("pool", wd4[:, 2 * e4:2 * e4 + 2], wdv[:, 2 * e4:2 * e4 + 2], w=[("wd", e4)])

            def load_w(e):
                b = e % 2
                S.dma("pool", wg[:, b], d["moe_w_gate"][l, e].rearrange("(kc p) f -> p kc f", p=128), w=[("wg", b)])
                S.dma("pool", wu[:, b], d["moe_w_up"][l, e].rearrange("(kc p) f -> p kc f", p=128), w=[("wu", b)])

            xtR = sb("xtR", [128, 2, DM])

            def R_a(sc, tt):
                b = tt % 2
                tok0 = sc * 1024 + tt * 128
                if tt == 0:
                    S.dma("sp", xtR[:, 0, :], x_in[tok0:tok0 + 128, :], w=[("xtR", 0)])
                if tt + 1 < 8:
                    S.dma("sp", xtR[:, 1 - b, :], x_in[tok0 + 128:tok0 + 256, :], w=[("xtR", 1 - b)])
                self.norm_tile_a(S, T, xtR[:, b, :], ("xtR", b), g2, tt)

            def R_b(sc, tt):
                b = tt % 2
                tok0 = sc * 1024 + tt * 128
                self.norm_tile_b(S, T, h2T[:, :, tt * 128:(tt + 1) * 128], ("h2T", tt), tt)
                lg = psL[:, b * 64:b * 64 + 20]
                for kc in range(8):
                    MM(S, lg, h2T[:, kc, tt * 128:(tt + 1) * 128], wr[:, kc, :], start=(kc == 0), stop=(kc == 7),
                       r=[("h2T", tt), "wr"], w=["psL"])
                R = rt[:, b, :]
                rk = ("rt", b)
                lgs = R[:, 0:20]
                TT(S, lgs, lg, brb[:], ALU.add, r=["psL", "brb"], w=[rk])
                S.op("dve", lambda e, R=R: e.reduce_max(out=R[:, 20:21], in_=R[:, 0:4], axis=AX.X), [rk], [rk])
                TS(S, R[:, 21:22], R[:, 20:21], -1.0, None, ALU.mult, r=[rk], w=[rk])
                ACT(S, R[:, 24:28], R[:, 0:4], AF.Exp, r=[rk], w=[rk], bias=R[:, 21:22], accum_out=R[:, 22:23])
                S.op("dve", lambda e, R=R: e.reciprocal(out=R[:, 23:24], in_=R[:, 22:23]), [rk], [rk])
                TS(S, R[:, 28:32], R[:, 0:4], R[:, 20:21], None, ALU.is_ge, r=[rk], w=[rk])
                TS(S, R[:, 28:32], R[:, 28:32], -1.0, 1e30, ALU.add, ALU.mult, r=[rk], w=[rk])
                em = R[:, 32:48]
                TT(S, em.rearrange("p (g e) -> p g e", e=4), R[:, 4:20].rearrange("p (g e) -> p g e", e=4),
                   R[:, 28:32].unsqueeze(2).to_broadcast([128, 4, 4]), ALU.add, r=[rk], w=[rk])
                S.op("dve", lambda e, R=R: e.max(out=R[:, 48:56], in_=R[:, 32:48]), [rk], [rk])
                TS(S, R[:, 56:57], R[:, 48:49], -1.0, None, ALU.mult, r=[rk], w=[rk])
                ACT(S, R[:, 64:80], em, AF.Exp, r=[rk], w=[rk], bias=R[:, 56:57])
                ACT(S, R[:, 57:58], R[:, 49:50], AF.Exp, r=[rk], w=[rk], bias=R[:, 56:57])
                TS(S, R[:, 57:58], R[:, 57:58], 1.0, None, ALU.add, r=[rk], w=[rk])
                S.op("dve", lambda e, R=R: e.reciprocal(out=R[:, 58:59], in_=R[:, 57:58]), [rk], [rk])
                TT(S, R[:, 59:60], R[:, 58:59], R[:, 23:24], ALU.mult, r=[rk], w=[rk])
                TS(S, R[:, 80:96], em, R[:, 49:50], None, ALU.is_ge, r=[rk], w=[rk])
                STT(S, R[:, 64:80], R[:, 64:80], R[:, 59:60], R[:, 80:96], ALU.mult, ALU.mult, r=[rk], w=[rk])
                CP(S, hl[:, b, 0:16], R[:, 64:80], r=[rk], w=[("hl", b)])
                TT(S, hl[:, b, 16:32], R[:, 64:80], hl[:, b, 0:16], ALU.subtract, r=[rk, ("hl", b)], w=[("hl", b)])
                if "comb" in self.dbg and l == 0:
                    S.dma("sp", self.dbg_out["comb"][tok0:tok0 + 128, :], R[:, 64:80], r=[rk])

            def R_c(sc, tt):
                b = tt % 2
                hlT = T["pst"][0:32, b, 0, :]
                TR(S, hlT, hl[:, b, :], T["ident_bf"][:], r=[("hl", b), "ident_bf"], w=[("pst", b)])
                CP(S, combT[:, tt * 128:(tt + 1) * 128], hlT, r=[("pst", b)], w=[("combT", tt // 4)])

            def D(sc, tt):
                b = tt % 2
                half = tt // 4
                tok0 = sc * 1024 + tt * 128
                if tt == 0:
                    S.dma("sp", xt[:, 0, :], x_in[tok0:tok0 + 128, :], w=[("xt", 0)])
                if tt + 1 < 8:
                    S.dma("sp", xt[:, 1 - b, :], x_in[tok0 + 128:tok0 + 256, :], w=[("xt", 1 - b)])
                for dh in range(2):
                    ps = psA[dh] if b == 0 else psB[dh]
                    pk = ("psA", dh) if b == 0 else ("psB", dh)
                    for ft in range(32):
                        MM(S, ps[:], actall[:, ft, tt * 128:(tt + 1) * 128], wd[:, ft, dh * 512:(dh + 1) * 512],
                           start=(ft == 0), stop=(ft == 31), r=[("actall", half), ("wd", ft // 4)], w=[pk])
                    TT(S, xt[:, b, dh * 512:(dh + 1) * 512], ps[:], xt[:, b, dh * 512:(dh + 1) * 512], ALU.add,
                       r=[pk, ("xt", b)], w=[("xt", b)])
                if final:
                    ACT(S, T["junk"][:, 0, :], xt[:, b, :], AF.Square, r=[("xt", b)], w=["junk", ("fin", b)],
                        accum_out=fin[:, b, 0:1])
                    TS(S, fin[:, b, 1:2], fin[:, b, 0:1], 1.0 / DM, EPS, ALU.mult, ALU.add, r=[("fin", b)], w=[("fin1", b)])
                    ACT(S, fin[:, b, 2:3], fin[:, b, 1:2], AF.Sqrt, r=[("fin1", b)], w=[("fin2", b)])
                    S.op("dve", lambda e, b=b: e.reciprocal(out=fin[:, b, 3:4], in_=fin[:, b, 2:3]), [("fin2", b)], [("fin3", b)])
                    STT(S, xt[:, b, :], xt[:, b, :], fin[:, b, 3:4], gf[:], ALU.mult, ALU.mult,
                        r=[("xt", b), ("fin3", b), "gf"], w=[("xt", b)])
                S.dma("sp", x_out[tok0:tok0 + 128, :], xt[:, b, :], r=[("xt", b)])

            def E(sc):
                for e in range(16):
                    wb = e % 2
                    if e + 1 < 16:
                        load_w(e + 1)
                    elif sc + 1 < 4:
                        load_w(0)
                    for half in range(2):
                        hk = [("h2T", 4 * half + i) for i in range(4)]
                        MM(S, psC[:], esel[:, e, :], combT[:, half * 512:(half + 1) * 512],
                           r=["esel", ("combT", half)], w=["psC"])
                        ACT(S, cb[:, half, :], psC[:], AF.Copy, r=["psC"], w=[("cb", half)])
                        for ft in range(2):
                            pb = (half * 2 + ft) % 2
                            for kc in range(8):
                                MM(S, psA[pb][:], wg[:, wb, kc, ft * 128:(ft + 1) * 128], h2T[:, kc, half * 512:(half + 1) * 512],
                                   start=(kc == 0), stop=(kc == 7), r=hk + [("wg", wb)], w=[("psA", pb)])
                            for kc in range(8):
                                MM(S, psB[pb][:], wu[:, wb, kc, ft * 128:(ft + 1) * 128], h2T[:, kc, half * 512:(half + 1) * 512],
                                   start=(kc == 0), stop=(kc == 7), r=hk + [("wu", wb)], w=[("psB", pb)])
                            ACT(S, sg[:, pb, :], psA[pb][:], AF.Silu, r=[("psA", pb)], w=[("sg", pb)])
                            TT(S, sg[:, pb, :], psB[pb][:], sg[:, pb, :], ALU.mult, r=[("psB", pb), ("sg", pb)], w=[("sg", pb)])
                            TT(S, actall[:, 2 * e + ft, half * 512:(half + 1) * 512], sg[:, pb, :], cb[:, half, :], ALU.mult,
                               r=[("sg", pb), ("cb", half)], w=[("actall", half)], eng="pool")

            R_a(0, 0)
            R_a(0, 1)
            for tt in range(8):
                R_b(0, tt)
                if tt + 2 < 8:
                    R_a(0, tt + 2)
                R_c(0, tt)
            load_w(0)
            load_wd()
            for sc in range(4):
                E(sc)
                nxt = sc + 1 < 4
                if nxt:
                    R_a(sc + 1, 0)
                    R_b(sc + 1, 0)
                    R_a(sc + 1, 1)
                for tt in range(8):
                    D(sc, tt)
                    if nxt:
                        R_c(sc + 1, tt)
                        if tt + 1 < 8:
                            R_b(sc + 1, tt + 1)
                        if tt + 2 < 8:
                            R_a(sc + 1, tt + 2)
            S.emit()


    def cmul(self, S, o_re, o_im, a_re, a_im, b_re, b_im, t1, t2, r, w, tk):
        TT(S, t1, a_re, b_re, ALU.mult, r=r, w=[tk + "1"])
        TT(S, t2, a_im, b_im, ALU.mult, r=r, w=[tk + "2"])
        TT(S, o_re, t1, t2, ALU.subtract, r=[tk + "1", tk + "2"], w=w)
        TT(S, t1, a_re, b_im, ALU.mult, r=r, w=[tk + "1"])
        TT(S, t2, a_im, b_re, ALU.mult, r=r, w=[tk + "2"])
        TT(S, o_im, t1, t2, ALU.add, r=[tk + "1", tk + "2"], w=w)

    def block_proj(self, l, x_in, mode, R):
        nc, d = self.nc, self.d
        ncols = 512 if mode == "up" else 1536
        c0 = 0 if mode == "up" else 512
        with ExitStack() as es:
            sb = lambda name, shape, dt=F32: es.enter_context(nc.sbuf_tensor(self.nm(name), shape, dt))
            ps = lambda name, shape, dt=F32: es.enter_context(nc.psum_tensor(self.nm(name), shape, dt))
            T = {}
            self.alloc_norm(es, T, junk=False)
            win = sb("win", [128, 8, ncols], BF16)
            g1 = sb("g1", [128, DM])
            NXB = 3
            xt = sb("xt", [128, NXB, DM])
            hT = sb("hT", [128, 2, 8, 512], BF16)
            psP = [ps("psP", [128, 512]) for _ in range(2)]
            S = Sched(nc)
            self.load_ident(S, T)
            S.dma("sp", g1[:], d["g1b"][l], w=["g"])
            wv = d["w_in"][l].rearrange("(kc p) n -> p kc n", p=128)
            for kc in range(8):
                S.dma("pool", win[:, kc, :], wv[:, kc, c0:c0 + ncols], w=[("win", kc)])
            wkeys = [("win", kc) for kc in range(8)]
            if mode == "up":
                pbuf = sb("pbuf", [128, 2, 2, 528])
                sA = sb("sA", [128, 2, 528])
                sB = sb("sB", [128, 2, 528])
                dlt = sb("dlt", [128, 2, 512], BF16)
                tfix = sb("tfix", [128, 16])
                poolw = sb("poolw", [128, 2, 128], BF16)
                winv = sb("winv", [128, 2])
                invc = sb("invc", [128, 2, 16])
                psQ = ps("psQ", [128, 2, 512])
                S.dma("pool", poolw[:], d["poolw"][l].rearrange("c k m -> k c m"), w=["poolw"])
                S.dma("sp", winv[:], d["winv"], w=["winv"])
                S.dma("sp", invc[:], d["invc"], w=["invc"])
                S.dma("sp", R["chp"][:], d["chp"][l], w=["chp"])
                MSET(S, pbuf[:, 0, :, 0:16], 0.0, w=[("pbuf", 0)])
                self.ssm_setup(S, l, es, R, T)
            else:
                qst = sb("qst", [128, 2, 4, 512], BF16)
                MSET(S, R["Vaug"][:, :, :, 64:65], 1.0, w=["vones"])
                MSET(S, R["ksum"][:], 0.0, w=["ksum"])
            def load_x(Tg):
                S.dma("sp", xt[:, Tg % NXB, :], x_in[Tg * 128:(Tg + 1) * 128, :], w=[("xt", Tg % NXB)])

            def norm_a(Tg):
                self.norm_tile_a(S, T, xt[:, Tg % NXB, :], ("xt", Tg % NXB), g1, Tg)

            for Tg in range(NXB):
                load_x(Tg)
            norm_a(0)
            norm_a(1)
            for c in range(8):
                hb = c % 2
                for tt in range(4):
                    Tg = c * 4 + tt
                    self.norm_tile_b(S, T, hT[:, hb, :, tt * 128:(tt + 1) * 128], ("hT", hb, tt), Tg)
                    if Tg + NXB < 32:
                        load_x(Tg + NXB)
                    if Tg + 2 < 32:
                        norm_a(Tg + 2)
                    if mode == "up" and tt == 2 and c > 0:
                        self.pool_chunk_back(S, l, c - 1, 1 - hb, pbuf, sA, sB, dlt, tfix, poolw, winv, invc, psQ, R)
                hk = [("hT", hb, tt) for tt in range(4)]
                nmt = ncols // 128 if mode == "up" else 8
                for mt in range(nmt):
                    pb = mt % 2
                    for kc in range(8):
                        MM(S, psP[pb][:], win[:, kc, mt * 128:(mt + 1) * 128], hT[:, hb, kc, :], start=(kc == 0),
                           stop=(kc == 7), r=hk + wkeys, w=[("psP", pb)])
                    if mode == "up":
                        if mt < 2:
                            ACT(S, R["u_sb"][:, mt, c * 512:(c + 1) * 512], psP[pb][:], AF.Copy, r=[("psP", pb)], w=[("u", mt)])
                        else:
                            ACT(S, pbuf[:, hb, mt - 2, 16:528], psP[pb][:], AF.Copy, r=[("psP", pb)], w=[("pbuf", hb)])
                    else:
                        if mt < 4:
                            ACT(S, qst[:, hb, mt, :], psP[pb][:], AF.Copy, r=[("psP", pb)], w=[("qst", hb)], scale=0.125)
                        else:
                            hp = mt - 4
                            for bl in range(2):
                                blk = 2 * c + bl
                                ACT(S, R["kT"][:, hp, blk * 256:(blk + 1) * 256], psP[pb][:, bl * 256:(bl + 1) * 256], AF.Copy,
                                    r=[("psP", pb)], w=["kT", "ksum"], accum_out=R["ksum"][:, hp, blk:blk + 1])
                if mode == "up":
                    self.pool_chunk_front(S, l, c, hb, pbuf, sA, sB)
                    if c == 7:
                        self.pool_chunk_back(S, l, c, hb, pbuf, sA, sB, dlt, tfix, poolw, winv, invc, psQ, R)
                else:
                    S.dma("sp", self.qT_d[:, :, c * 512:(c + 1) * 512].rearrange("hp p t -> p hp t"), qst[:, hb], r=[("qst", hb)])
                    for tt in range(4):
                        pb = tt % 2
                        for kc in range(8):
                            MM(S, psP[pb][:], hT[:, hb, kc, tt * 128:(tt + 1) * 128], win[:, kc, 1024:1536], start=(kc == 0),
                               stop=(kc == 7), r=hk + wkeys, w=[("psP", pb)])
                        CP(S, R["Vaug"][:, c * 4 + tt, :, 0:64], psP[pb][:].rearrange("p (h d) -> p h d", d=64),
                           r=[("psP", pb)], w=["Vaug"])
            if mode == "qkv":
                TS(S, R["kmeanT"][:], R["ksum"][:], 1.0 / 256.0, None, ALU.mult, r=["ksum"], w=["kmeanT"])
            S.emit()

    def pool_chunk_front(self, S, l, c, hb, pbuf, sA, sB):
        pk = ("pbuf", hb)
        if c + 1 < 8:
            CP(S, pbuf[:, 1 - hb, :, 0:16], pbuf[:, hb, :, 512:528], r=[pk], w=[("pbuf", 1 - hb)], eng="pool")
        add = lambda o, a, b_, r, w: TT(S, o, a, b_, ALU.add, r=r, w=w, eng="pool")
        for ct in range(2):
            p = pbuf[:, hb, ct, :]
            a_, b_ = sA[:, ct, :], sB[:, ct, :]
            ka, kb = ("sA", ct), ("sB", ct)
            add(a_[:, 1:528], p[:, 1:528], p[:, 0:527], [pk], [ka])
            if ct == 0:
                add(b_[64:128, 3:528], a_[64:128, 3:528], a_[64:128, 1:526], [ka], [kb])
            else:
                add(b_[:, 3:528], a_[:, 3:528], a_[:, 1:526], [ka], [kb])
                add(a_[:, 7:528], b_[:, 7:528], b_[:, 3:524], [kb], [ka])
                add(b_[64:128, 15:528], a_[64:128, 15:528], a_[64:128, 7:520], [ka], [kb])

    def pool_chunk_back(self, S, l, c, hb, pbuf, sA, sB, dlt, tfix, poolw, winv, invc, psQ, R):
        pk = ("pbuf", hb)
        for ct in range(2):
            p = pbuf[:, hb, ct, :]
            for half, src, sk in ((0, sA[:, ct, :], ("sA", ct)), (1, sB[:, ct, :], ("sB", ct))):
                rows = slice(64 * half, 64 * half + 64)
                STT(S, dlt[rows, ct, :], src[rows, 16:528], winv[rows, ct:ct + 1], p[rows, 16:528], ALU.mult, ALU.subtract,
                    r=[sk, pk, "winv"], w=[("dlt", ct)])
                if c == 0:
                    TT(S, tfix[rows, :], src[rows, 16:32], invc[rows, ct, :], ALU.mult, r=[sk, "invc"], w=["tfix"])
                    TT(S, dlt[rows, ct, 0:16], tfix[rows, :], p[rows, 16:32], ALU.subtract, r=["tfix", pk], w=[("dlt", ct)])
            MM(S, psQ[:, ct, :], poolw[:, ct, :], dlt[:, ct, :], r=["poolw", ("dlt", ct)], w=[("psQ", ct)])
        for ct in range(2):
            TS(S, R["ypT"][:, ct, c * 512:(c + 1) * 512], psQ[:, ct, :], R["chp"][:, ct, 2:3], R["chp"][:, ct, 3:4], ALU.add, ALU.mult,
               r=[("psQ", ct), "chp"], w=[("ypT", ct)])

    def ssm_setup(self, S, l, es, R, T):
        nc, d = self.nc, self.d
        sb = lambda name, shape, dt=F32: es.enter_context(nc.sbuf_tensor(self.nm(name), shape, dt))
        ps = lambda name, shape, dt=F32: es.enter_context(nc.psum_tensor(self.nm(name), shape, dt))
        sa = sb("sa", [128, 8, 3])
        sm = sb("sm", [128, 24, 8])
        Lre = sb("Lre", [128, 9, 8]); Lim = sb("Lim", [128, 9, 8])
        Fre = sb("Fre", [128, 8, 8]); Fim = sb("Fim", [128, 8, 8])
        tA = sb("tA", [128, 8, 8]); tB = sb("tB", [128, 8, 8])
        Bre = sb("Bre", [128, 8, 128]); Bim = sb("Bim", [128, 8, 128])
        Cre = sb("Cre", [128, 8, 128]); Cim = sb("Cim", [128, 8, 128]); nCim = sb("nCim", [128, 8, 128])
        scr = R["ysT"][:].bitcast(F32)
        carve = lambda i: scr[:, i // 2, (i % 2) * 1024:(i % 2) * 1024 + 1024].rearrange("p (a b) -> p a b", b=128)
        LBre, LBim, t1, t2 = carve(0), carve(1), carve(2), carve(3)
        LBb = sb("LBb", [128, 2, 8, 128], BF16)
        hpi = sb("hpi", [128, 1])
        psW = ps("psW", [128, 8, 128], BF16)
        psK = ps("psK", [128, 512])
        glu = R["glu_sb"]
        S.dma("pool", glu[:], d["ssm_glu_w"][l].rearrange("(kc p) n -> p kc n", p=128), w=["glu"])
        S.dma("sp", sa[:], d["ssmA"][l], w=["sa"])
        S.dma("sp", hpi[:], d["neghalfpi"], w=["hpi"])
        S.dma("sp", Bre[:], d["bexp"][l, 0], w=["Bre"])
        S.dma("sp", Bim[:], d["bexp"][l, 1], w=["Bim"])
        S.dma("sp", Cre[:], d["cexp"][l, 0], w=["Cre"])
        S.dma("sp", Cim[:], d["cexp"][l, 1], w=["Cim"])
        TS(S, nCim[:], Cim[:], -1.0, None, ALU.mult, r=["Cim"], w=["nCim"])
        a_re, a_im, ldt = sa[:, :, 0], sa[:, :, 1], sa[:, :, 2]
        k = lambda i: ("sm", i)
        sl = lambda i: sm[:, i, :]
        ACT(S, sl(0), ldt, AF.Exp, r=["sa"], w=[k(0)])
        TT(S, sl(1), a_re, sl(0), ALU.mult, r=["sa", k(0)], w=[k(1)])
        TT(S, sl(2), a_im, sl(0), ALU.mult, r=["sa", k(0)], w=[k(2)])
        ACT(S, sl(3), sl(1), AF.Exp, r=[k(1)], w=[k(3)], scale=1.0 / 32)
        ACT(S, sl(4), sl(2), AF.Sin, r=[k(2)], w=[k(4)], scale=1.0 / 32)
        ACT(S, sl(5), sl(2), AF.Sin, r=[k(2), "hpi"], w=[k(5)], scale=1.0 / 32, bias=hpi[:, 0:1])
        TT(S, sl(6), sl(3), sl(5), ALU.mult, r=[k(3), k(5)], w=[k(6)])
        TT(S, sl(7), sl(3), sl(4), ALU.mult, r=[k(3), k(4)], w=[k(7)])
        for _ in range(5):
            TT(S, sl(8), sl(6), sl(6), ALU.mult, r=[k(6)], w=[k(8)])
            TT(S, sl(9), sl(7), sl(7), ALU.mult, r=[k(7)], w=[k(9)])
            TT(S, sl(10), sl(6), sl(7), ALU.mult, r=[k(6), k(7)], w=[k(10)])
            TT(S, sl(6), sl(8), sl(9), ALU.subtract, r=[k(8), k(9)], w=[k(6)])
            TS(S, sl(7), sl(10), 2.0, None, ALU.mult, r=[k(10)], w=[k(7)])
        MSET(S, Lre[:, 0, :], 1.0, w=[("L", 0)], eng="dve")
        MSET(S, Lim[:, 0, :], 0.0, w=[("L", 0)], eng="dve")
        CP(S, Lre[:, 1, :], sl(6), r=[k(6)], w=[("L", 1)])
        CP(S, Lim[:, 1, :], sl(7), r=[k(7)], w=[("L", 1)])
        for j in range(1, 8):
            self.cmul(S, Lre[:, j + 1, :], Lim[:, j + 1, :], Lre[:, j, :], Lim[:, j, :], sl(6), sl(7), sl(8), sl(9),
                      r=[("L", j), k(6), k(7)], w=[("L", j + 1)], tk="smt")
        Ak = R["Ak"]
        CP(S, Ak[:, 0, :, 0], Lre[:, 8, :], r=[("L", 8)], w=[("Ak", 0)])
        CP(S, Ak[:, 0, :, 1], Lim[:, 8, :], r=[("L", 8)], w=[("Ak", 0)])
        for kk in range(8):
            self.cmul(S, Ak[:, kk + 1, :, 0], Ak[:, kk + 1, :, 1], Ak[:, kk, :, 0], Ak[:, kk, :, 1], Ak[:, kk, :, 0], Ak[:, kk, :, 1],
                      sl(8), sl(9), r=[("Ak", kk)], w=[("Ak", kk + 1)], tk="smt")
        for kk in range(9):
            TS(S, Ak[:, kk, :, 2], Ak[:, kk, :, 1], -1.0, None, ALU.mult, r=[("Ak", kk)], w=[("Akn", kk)])
        TT(S, sl(11), a_re, a_re, ALU.mult, r=["sa"], w=[k(11)])
        TT(S, sl(12), a_im, a_im, ALU.mult, r=["sa"], w=[k(12)])
        TT(S, sl(11), sl(11), sl(12), ALU.add, r=[k(11), k(12)], w=[k(11)])
        S.op("dve", lambda e: e.reciprocal(out=sm[:, 12, :], in_=sm[:, 11, :]), [k(11)], [k(12)])
        TS(S, sl(13), sl(6), -1.0, None, ALU.add, r=[k(6)], w=[k(13)])
        TT(S, sl(14), sl(13), a_re, ALU.mult, r=[k(13), "sa"], w=[k(14)])
        TT(S, sl(15), sl(7), a_im, ALU.mult, r=[k(7), "sa"], w=[k(15)])
        TT(S, sl(14), sl(14), sl(15), ALU.add, r=[k(14), k(15)], w=[k(14)])
        TT(S, sl(16), sl(14), sl(12), ALU.mult, r=[k(14), k(12)], w=[k(16)])
        TT(S, sl(14), sl(7), a_re, ALU.mult, r=[k(7), "sa"], w=[k(14)])
        TT(S, sl(15), sl(13), a_im, ALU.mult, r=[k(13), "sa"], w=[k(15)])
        TT(S, sl(14), sl(14), sl(15), ALU.subtract, r=[k(14), k(15)], w=[k(14)])
        TT(S, sl(17), sl(14), sl(12), ALU.mult, r=[k(14), k(12)], w=[k(17)])
        Lk = [("L", j) for j in range(9)]
        bc = lambda ap: ap.unsqueeze(1).to_broadcast([128, 8, 8])
        self.cmul(S, Fre[:], Fim[:], Lre[:, 0:8, :], Lim[:, 0:8, :], bc(sl(16)), bc(sl(17)), tA[:], tB[:],
                  r=Lk + [k(16), k(17)], w=["F"], tk="tAB")
        bc3 = lambda ap: ap.unsqueeze(2).to_broadcast([128, 8, 128])
        Wss, Kmat, Vm = R["Wss"], R["Kmat"], R["Vm"]
        for j in range(8):
            self.cmul(S, LBre, LBim, bc3(Fre[:, j, :]), bc3(Fim[:, j, :]), Bre[:], Bim[:], t1, t2,
                      r=["F", "Bre", "Bim"], w=["LB"], tk="t12")
            CP(S, LBb[:, 0], LBre[:], r=["LB"], w=["LBb"])
            CP(S, LBb[:, 1], LBim[:], r=["LB"], w=["LBb"])
            for ri in range(2):
                for P in range(8):
                    TR(S, psW[:, P, :], LBb[:, ri, P, :], T["ident_bf"][:], r=["LBb", "ident_bf"], w=["psW"])
                ACT(S, Wss[:, :, :, 7 - j, ri, :].rearrange("p a b s -> p (a b) s"), psW[:], AF.Copy, r=["psW"], w=["Wss"])
            for ct in range(2):
                reg = psK[:, ct * 128:(ct + 1) * 128]
                for i in range(4):
                    P = ct * 4 + i
                    MM(S, reg, LBre[:, P, :], Cre[:, P, :], start=(i == 0), stop=False, r=["LB", "Cre"], w=["psK"])
                for i in range(4):
                    P = ct * 4 + i
                    MM(S, reg, LBim[:, P, :], nCim[:, P, :], start=False, stop=(i == 3), r=["LB", "nCim"], w=["psK"])
            ACT(S, Kmat[:, :, j, :], psK[:, 0:256].rearrange("p (c m) -> p c m", m=128), AF.Copy, r=["psK"], w=["Kmat"])
        for tau in range(8):
            self.cmul(S, LBre, LBim, bc3(Lre[:, tau + 1, :]), bc3(Lim[:, tau + 1, :]), Cre[:], Cim[:], t1[:], t2[:],
                      r=Lk + ["Cre", "Cim"], w=["LB"], tk="t12")
            CP(S, Vm[:, :, 0, tau, :], LBre[:], r=["LB"], w=["Vm"])
            TS(S, Vm[:, :, 1, tau, :], LBim[:], -1.0, None, ALU.mult, r=["LB"], w=["Vm"])

    def block_ssm(self, l, R):
        nc = self.nc
        with ExitStack() as es:
            sb = lambda name, shape, dt=F32: es.enter_context(nc.sbuf_tensor(self.nm(name), shape, dt))
            ps = lambda name, shape, dt=F32: es.enter_context(nc.psum_tensor(self.nm(name), shape, dt))
            Xs = sb("Xs", [128, 2, 2, 768])
            Xprev = sb("Xprev", [128, 8, 2, 512], BF16)
            t1 = sb("t1s", [128, 4096])
            ygb = sb("ygb", [128, 2, 4096], BF16)
            sgl = sb("sgl", [128, 2, 512])
            psZ = [ps("psZ", [128, 512]) for _ in range(2)]
            psY = [ps("psY", [128, 512]) for _ in range(2)]
            psG = [ps("psG", [128, 512]) for _ in range(2)]
            S = Sched(nc)
            u_sb, Wss, Kmat, Vm, Ak, chp, glu, ysT = (R[k_] for k_ in ("u_sb", "Wss", "Kmat", "Vm", "Ak", "chp", "glu_sb", "ysT"))
            MSET(S, Xs[:, :, :, 0:256], 0.0, w=["Xs0", "Xs1"])
            MSET(S, Xprev[:, :, :, 0:1], 0.0, w=["Xprev"])
            u4 = [u_sb[:, ct, :].rearrange("p (s t) -> p t s", t=8) for ct in range(2)]
            for P in range(8):
                ct, pi = P // 4, P % 4
                for ri in range(2):
                    z = psZ[ri]
                    for sg in range(8):
                        MM(S, z[:], Wss[:, ct, pi, sg, ri, :], u4[ct][:, sg, :], start=(sg == 0), stop=(sg == 7),
                           r=["Wss", ("u", ct)], w=[("psZ", ri)])
                    CP(S, Xs[:, 0, ri, 256:768], z[:], r=[("psZ", ri)], w=["Xs0"])
                pp = 0
                for kk in range(9):
                    o = 1 << kk
                    src, dst = Xs[:, pp], Xs[:, 1 - pp]
                    sk, dk = "Xs%d" % pp, "Xs%d" % (1 - pp)
                    are, aim, naim = Ak[:, kk, P:P + 1, 0], Ak[:, kk, P:P + 1, 1], Ak[:, kk, P:P + 1, 2]
                    sh = slice(256 - o, 768 - o)
                    STT(S, dst[:, 0, 256:768], src[:, 0, sh], are, src[:, 0, 256:768], ALU.mult, ALU.add, r=[sk], w=[dk])
                    STT(S, dst[:, 0, 256:768], src[:, 1, sh], naim, dst[:, 0, 256:768], ALU.mult, ALU.add, r=[sk, dk], w=[dk])
                    STT(S, dst[:, 1, 256:768], src[:, 1, sh], are, src[:, 1, 256:768], ALU.mult, ALU.add, r=[sk], w=[dk])
                    STT(S, dst[:, 1, 256:768], src[:, 0, sh], aim, dst[:, 1, 256:768], ALU.mult, ALU.add, r=[sk, dk], w=[dk])
                    pp = 1 - pp
                CP(S, Xprev[:, P, :, 1:512], Xs[:, pp, :, 256:767], r=["Xs%d" % pp], w=["Xprev"])
            for ct in range(2):
                t1v = t1[:].rearrange("p (s t) -> p t s", t=8)
                for tau in range(8):
                    y = psY[tau % 2]
                    yk = ("psY", tau % 2)
                    n_mm = tau + 1 + 8
                    i = 0
                    for sg in range(tau + 1):
                        MM(S, y[:], Kmat[:, ct, tau - sg, :], u4[ct][:, sg, :], start=(i == 0), stop=(i == n_mm - 1), r=[], w=[yk])
                        i += 1
                    for pi in range(4):
                        for ri in range(2):
                            MM(S, y[:], Vm[:, ct * 4 + pi, ri, tau, :], Xprev[:, ct * 4 + pi, ri, :], start=(i == 0),
                               stop=(i == n_mm - 1), r=["Xprev"], w=[yk])
                            i += 1
                    STT(S, t1v[:, tau, :], u4[ct][:, tau, :], chp[:, ct, 0:1], y[:], ALU.mult, ALU.add, r=[yk], w=["t1"])
                for cc in range(8):
                    ACT(S, ygb[:, ct, cc * 512:(cc + 1) * 512], t1[:, cc * 512:(cc + 1) * 512], AF.Gelu_apprx_tanh,
                        r=["t1"], w=[("ygb", ct)])
            for cc in range(8):
                for mt in range(2):
                    g = psG[mt]
                    for kc in range(2):
                        MM(S, g[:], glu[:, kc, mt * 128:(mt + 1) * 128], ygb[:, kc, cc * 512:(cc + 1) * 512], start=(kc == 0),
                           stop=(kc == 1), r=[("ygb", 0), ("ygb", 1)], w=[("psG", mt)])
                    ACT(S, sgl[:, mt, :], g[:], AF.Sigmoid, r=[("psG", mt)], w=[("sgl", mt)], bias=chp[:, mt, 1:2])
                    TT(S, ysT[:, mt, cc * 512:(cc + 1) * 512], ygb[:, mt, cc * 512:(cc + 1) * 512], sgl[:, mt, :], ALU.mult,
                       r=[("sgl", mt), ("ygb", mt)], w=["ysT"])
            S.emit()

    def block_attn(self, l, x_in, x_out, R, nqb=16):
        nc, d = self.nc, self.d
        with ExitStack() as es:
            sb = lambda name, shape, dt=F32: es.enter_context(nc.sbuf_tensor(self.nm(name), shape, dt))
            ps = lambda name, shape, dt=F32: es.enter_context(nc.psum_tensor(self.nm(name), shape, dt))
            kT, Vaug, kmeanT, ysT, ypT = (R[k_] for k_ in ("kT", "Vaug", "kmeanT", "ysT", "ypT"))
            wout = sb("wout", [128, 8, DM], BF16)
            qT = sb("qT", [128, 2, 4, 2, 256], BF16)
            G = sb("G", [128, 8, 256])
            G2 = sb("G2", [128, 8, 256])
            HK = sb("HK", [128, 8, 256])
            c31 = sb("c31", [128, 8])
            negm = sb("negm", [128, 16, 16])
            PT = sb("PT", [128, 3, 2, 256], BF16)
            tmp = sb("tmp", [128, 3, 2, 256])
            gate = sb("gate", [128, 16, 16])
            top8 = sb("top8", [128, 16, 8])
            sel = sb("sel", [128, 2, 2, 8, 16])
            acc = sb("acc", [128, 2, 2, 8, 65])
            rec = sb("rec", [128, 2, 8])
            att = sb("att", [128, 2, 512], BF16)
            attT = sb("attT", [128, 2, 4, 256], BF16)
            xr = sb("xr", [128, 2, DM])
            ident = sb("identb", [128, 128], BF16)
            exch = sb("exch", [128, 128])
            rb = sb("rb", [128, 128]); ohm = sb("ohm", [128, 512])
            Fs = sb("Fs", [8, 512])
            pW = [ps("pW", [128, 512]) for _ in range(2)]
            pOb = [ps("pO", [128, 512]) for _ in range(2)]
            pO = [t_[:, 0:455].rearrange("p (s d) -> p s d", d=65) for t_ in pOb]
            pT = ps("pT", [128, 2, 4, 128], BF16)
            st = [ps("st", [128, 2, 256]) for _ in range(3)]
            S = Sched(nc)
            S.dma("pool", ident[:], d["ident"], w=["ident"])
            for nm_, t_ in (("exch", exch), ("rb", rb), ("ohm", ohm), ("negm", negm)):
                S.dma("sp", t_[:], d[nm_], w=[nm_])
            S.dma("sp", c31[:], d["c31b"], w=["c31"])
            wv = d["w_out"][l].rearrange("(kc p) n -> p kc n", p=128)
            for kc in range(8):
                S.dma("pool", wout[:, kc, :], wv[:, kc, :], w=[("wout", kc)])
            MM(S, pW[0][:], rb[:], ohm[:], r=["rb", "ohm"], w=[("pW", 0)])
            CP(S, Fs[:], pW[0][0:8, :], r=[("pW", 0)], w=["Fs"])
            fd = S.dma("sp", self.Fd, Fs[:], r=["Fs"], w=["Fd"])
            for off, dst, nm_ in ((0, G, "G"), (128, G2, "G2")):
                S.dma("sp", HK[:], bass.AP(tensor=self.Fd.tensor, offset=off, ap=[[1, 128], [512, 8], [1, 256]]), r=["Fd"], w=["HK"])
                for h in range(8):
                    pw = pW[h % 2]
                    MM(S, pw[:, 0:256], exch[:], HK[:, h, :], r=["exch", "HK"], w=[("pW", h % 2)])
                    CP(S, dst[:, h, :], pw[:, 0:256], r=[("pW", h % 2)], w=[nm_])
            MSET(S, qT[:], 0.0, w=[("qT", 0), ("qT", 1)])
            oslot = [0]

            def emit_head(qb):
                qbuf = qb % 2
                qk = ("qT", qbuf)
                for par_ in range(2):
                    rs = slice(64 * par_, 64 * par_ + 64)
                    S.dma("sp", qT[rs, qbuf, :, par_, :], self.qT_d[:, rs, qb * 256:(qb + 1) * 256].rearrange("hp p t -> p hp t"),
                          w=[qk])
                if qb >= 4:
                    pg = pW[0][:, 0:256].rearrange("p (i n) -> p i n", n=16)
                    for h in range(8):
                        hp = h // 2
                        for qt in range(2):
                            MM(S, pg[:, qt * 8 + h, :], qT[:, qbuf, hp, h % 2, qt * 128:(qt + 1) * 128], kmeanT[:, hp, :],
                               r=[qk, "kmeanT"], w=[("pW", 0)])
                    TT(S, gate[:], pg, negm[:, qb, :].unsqueeze(1).to_broadcast([128, 16, 16]), ALU.add,
                       r=[("pW", 0), "negm"], w=["gate"])
                    for i_ in range(16):
                        S.op("dve", lambda e, i_=i_: e.max(out=top8[:, i_, :], in_=gate[:, i_, :]), ["gate"], ["top8"])
                    TT(S, sel[:, qbuf].rearrange("p q h n -> p (q h) n"), gate[:],
                       top8[:, :, 2].unsqueeze(2).to_broadcast([128, 16, 16]), ALU.is_ge, r=["gate", "top8"], w=[("sel", qbuf)])

            def emit_S(it, idx):
                qb, n, h = it
                qbuf, hp, par, b = qb % 2, h // 2, h % 2, idx % 3
                own, prev = (n == qb), (n == qb - 1)
                stb, ptb = st[b], PT[:, b]
                sk, pk, tk, qk = ("st", b), ("PT", b), ("tmp", b), ("qT", qbuf)
                q_all = qT[:, qbuf, hp, par, :]
                MM(S, stb[:, 0, :], kT[:, hp, n * 256:n * 256 + 128], q_all, r=[qk, "kT"], w=[sk])
                if own:
                    MM(S, stb[:, 1, 128:256], kT[:, hp, n * 256 + 128:n * 256 + 256], q_all[:, 128:256], r=[qk, "kT"], w=[sk])
                    TT(S, tmp[:, b, 0, :], stb[:, 0, :], G[:, h, :], ALU.add, r=[sk, "G"], w=[tk])
                    TT(S, tmp[:, b, 1, 128:256], stb[:, 1, 128:256], G[:, h, 0:128], ALU.add, r=[sk, "G"], w=[tk])
                    ACT(S, ptb[:, 0, :], tmp[:, b, 0, :], AF.Exp, r=[tk], w=[pk])
                    ACT(S, ptb[:, 1, 128:256], tmp[:, b, 1, 128:256], AF.Exp, r=[tk], w=[pk])
                else:
                    MM(S, stb[:, 1, :], kT[:, hp, n * 256 + 128:n * 256 + 256], q_all, r=[qk, "kT"], w=[sk])
                    if prev:
                        TT(S, tmp[:, b, 1, :], stb[:, 1, :], G2[:, h, :], ALU.add, r=[sk, "G2"], w=[tk])
                        ACT(S, ptb[:, 0, :], stb[:, 0, :], AF.Exp, r=[sk, tk, "c31"], w=[pk], bias=c31[:, h:h + 1])
                        ACT(S, ptb[:, 1, :], tmp[:, b, 1, :], AF.Exp, r=[tk], w=[pk])
                    else:
                        ACT(S, ptb[:], stb[:], AF.Exp, r=[sk, "c31"], w=[pk], bias=c31[:, h:h + 1])

            def emit_PV(it, idx):
                qb, n, h = it
                qbuf, b = qb % 2, idx % 3
                own = (n == qb)
                ptb, pk = PT[:, b], ("PT", b)
                ok = ("pO", idx % 2)
                for qt in range(2):
                    o = pO[idx % 2][:, qt, :]
                    khs = [0] if (own and qt == 0) else [0, 1]
                    for kh in khs:
                        MM(S, o, ptb[:, kh, qt * 128:(qt + 1) * 128], Vaug[:, n * 2 + kh, h, :], start=(kh == khs[0]),
                           stop=(kh == khs[-1]), r=[pk, "Vaug", "vones"], w=[ok])
                for qt in range(2):
                    o = pO[idx % 2][:, qt, :]
                    ak = ("acc", qbuf, qt, h)
                    if own:
                        CP(S, acc[:, qbuf, qt, h, :], o, r=[ok], w=[ak])
                    elif qb < 4:
                        TT(S, acc[:, qbuf, qt, h, :], o, acc[:, qbuf, qt, h, :], ALU.add, r=[ok, ak], w=[ak])
                    else:
                        STT(S, acc[:, qbuf, qt, h, :], o, sel[:, qbuf, qt, h, n:n + 1], acc[:, qbuf, qt, h, :], ALU.mult, ALU.add,
                            r=[ok, ak, ("sel", qbuf)], w=[ak])

            def tail_norm(qb, qt):
                ab = qb % 2
                aks = [("acc", ab, qt, h) for h in range(8)]
                S.op("dve", lambda e: e.reciprocal(out=rec[:, qt, :], in_=acc[:, ab, qt, :, 64]), aks, [("rec", qt)])
                TT(S, att[:, qt, :].rearrange("p (h d) -> p h d", d=64), acc[:, ab, qt, :, 0:64],
                   rec[:, qt, :].unsqueeze(2).to_broadcast([128, 8, 64]), ALU.mult, r=aks + [("rec", qt)], w=[("att", qt)])
                for ck in range(4):
                    TR(S, pT[:, qt, ck, :], att[:, qt, ck * 128:(ck + 1) * 128], ident[:], r=[("att", qt), "ident"], w=["pT"])
                CP(S, attT[:, ab, :, qt * 128:(qt + 1) * 128], pT[:, qt], r=["pT"], w=[("attT", ab, qt)])

            def tail_out(qb, tt, dh):
                ab = qb % 2
                tok0 = qb * 256 + tt * 128
                if dh == 0:
                    S.dma("sp", xr[:, tt, :], x_in[tok0:tok0 + 128, :], w=[("xr", tt)])
                pw = pW[dh]
                for kc in range(8):
                    if kc < 2:
                        lhs, rk_ = ysT[:, kc, tok0:tok0 + 128], "ysT"
                    elif kc < 4:
                        lhs, rk_ = ypT[:, kc - 2, tok0:tok0 + 128], ("ypT", kc - 2)
                    else:
                        lhs, rk_ = attT[:, ab, kc - 4, tt * 128:(tt + 1) * 128], ("attT", ab, tt)
                    MM(S, pw[:], lhs, wout[:, kc, dh * 512:(dh + 1) * 512], start=(kc == 0), stop=(kc == 7),
                       r=[rk_, ("wout", kc)], w=[("pW", dh)])
                TT(S, xr[:, tt, dh * 512:(dh + 1) * 512], pw[:], xr[:, tt, dh * 512:(dh + 1) * 512], ALU.add,
                   r=[("pW", dh), ("xr", tt)], w=[("xr", tt)])
                if dh == 1:
                    S.dma("sp", x_out[tok0:tok0 + 128, :], xr[:, tt, :], r=[("xr", tt)])

            def tail_pieces(qb):
                return ([lambda qt=qt: tail_norm(qb, qt) for qt in range(2)]
                        + [lambda tt=tt, dh=dh: tail_out(qb, tt, dh) for tt in range(2) for dh in range(2)])

            LA = 2
            items = [(qb, n, h) for qb in range(nqb) for h in range(8) for n in ([qb] + list(range(qb)))]
            sched_at = {}
            emit_head(0)
            for j in range(LA):
                emit_S(items[j], j)
            for i, it in enumerate(items):
                j = i + LA
                if j < len(items):
                    nx = items[j]
                    if nx[0] != items[j - 1][0]:
                        emit_head(nx[0])
                    emit_S(nx, j)
                emit_PV(it, i)
                for fn in sched_at.pop(i, ()):
                    fn()
                if i + 1 == len(items):
                    for fn in tail_pieces(it[0]):
                        fn()
                elif items[i + 1][0] != it[0]:
                    n_next = (it[0] + 2) * 8
                    pcs = tail_pieces(it[0])
                    offs = [(k + 1) * n_next // (len(pcs) + 1) for k in range(len(pcs))]
                    for k, fn in enumerate(pcs):
                        sched_at.setdefault(i + 1 + offs[k], []).append(fn)
            assert not sched_at
            if "mix" in self.dbg and l == 0:
                S.dma("sp", self.dbg_out["ysT"], ysT[:], r=["ysT"])
                S.dma("sp", self.dbg_out["ypT"], ypT[:], r=[("ypT", 0), ("ypT", 1)])
                S.dma("sp", self.dbg_out["kT"], kT[:], r=["kT"])
            S.emit()

    def block_A(self, l, x_in, x_out):
        nc = self.nc
        with ExitStack() as es0:
            sb0 = lambda name, shape, dt=F32: es0.enter_context(nc.sbuf_tensor(self.nm(name), shape, dt))
            R = {}
            R["ysT"] = sb0("ysT", [128, 2, SEQ], BF16)
            R["ypT"] = sb0("ypT", [128, 2, SEQ], BF16)
            with ExitStack() as es1:
                sb1 = lambda name, shape, dt=F32: es1.enter_context(nc.sbuf_tensor(self.nm(name), shape, dt))
                R["u_sb"] = sb1("u_sb", [128, 2, SEQ], BF16)
                R["Wss"] = sb1("Wss", [128, 2, 4, 8, 2, 128], BF16)
                R["Vm"] = sb1("Vm", [128, 8, 2, 8, 128], BF16)
                R["Kmat"] = sb1("Kmat", [128, 2, 8, 128], BF16)
                R["Ak"] = sb1("Ak", [128, 9, 8, 3])
                R["chp"] = sb1("chp", [128, 2, 4])
                R["glu_sb"] = sb1("glu_sb", [128, 2, 256], BF16)
                self.block_proj(l, x_in, "up", R)
                if self.stop_after != ("A0", l):
                    self.block_ssm(l, R)
                if self.stop_after in (("AS", l), ("A0", l)):
                    S = Sched(nc)
                    S.dma("sp", self.dbg_out["ysT"], R["ysT"][:])
                    S.dma("sp", self.dbg_out["ypT"], R["ypT"][:])
                    S.dma("sp", self.dbg_out["u"], R["u_sb"][:])
                    S.dma("sp", self.dbg_out["Kmat"], R["Kmat"][:])
                    S.dma("sp", self.dbg_out["Ak"], R["Ak"][:])
                    S.emit()
                    return
            with ExitStack() as es2:
                sb2 = lambda name, shape, dt=F32: es2.enter_context(nc.sbuf_tensor(self.nm(name), shape, dt))
                R["kT"] = sb2("kT", [128, 4, SEQ], BF16)
                R["Vaug"] = sb2("Vaug", [128, 32, 8, 65], BF16)
                R["ksum"] = sb2("ksum", [128, 4, 16])
                R["kmeanT"] = sb2("kmeanT", [128, 4, 16], BF16)
                self.block_proj(l, x_in, "qkv", R)
                if self.stop_after == ("A1", l):
                    S = Sched(nc)
                    S.dma("sp", self.dbg_out["kT"], R["kT"][:])
                    S.dma("sp", self.dbg_out["Vaug"], R["Vaug"][:])
                    S.dma("sp", self.dbg_out["kmeanT"], R["kmeanT"][:])
                    S.emit()
                    return
                self.block_attn(l, x_in, x_out, R)

    def build_a2only(self, nqb):
        nc, d = self.nc, self.d
        ti = {}
        for name, shp in (("t_ysT", [128, 2, SEQ]), ("t_ypT", [128, 2, SEQ]), ("t_kT", [128, 4, SEQ]), ("t_V", [128, 32, 8, 64]),
                          ("t_kmean", [128, 4, 16]), ("t_q", [4, 128, SEQ])):
            ti[name] = nc.dram_tensor(name, shp, F32, kind="ExternalInput").ap()
        with ExitStack() as es:
            sb = lambda name, shape, dt=F32: es.enter_context(nc.sbuf_tensor(self.nm(name), shape, dt))
            R = {"ysT": sb("ysT", [128, 2, SEQ], BF16), "ypT": sb("ypT", [128, 2, SEQ], BF16), "kT": sb("kT", [128, 4, SEQ], BF16),
                 "Vaug": sb("Vaug", [128, 32, 8, 65], BF16), "ksum": sb("ksum", [128, 4, 16]), "kmeanT": sb("kmeanT", [128, 4, 16], BF16)}
            S = Sched(nc)
            S.dma("pool", R["ysT"][:], ti["t_ysT"]); S.dma("pool", R["ypT"][:], ti["t_ypT"]); S.dma("pool", R["kT"][:], ti["t_kT"])
            for kt4 in range(8):
                S.dma("pool", R["Vaug"][:, 4 * kt4:4 * kt4 + 4, :, 0:64], ti["t_V"][:, 4 * kt4:4 * kt4 + 4], w=["V"])
            S.dma("pool", R["kmeanT"][:], ti["t_kmean"])
            MSET(S, R["Vaug"][:, :, :, 64:65], 1.0, w=["vones"])
            S.dma("pool", self.qT_d, ti["t_q"])
            S.emit()
            self.block_attn(0, d["x"], self.out, R, nqb=nqb)
        return nc

    def build(self):
        d = self.d
        if isinstance(self.stop_after, tuple) and self.stop_after[0] == "A2only":
            return self.build_a2only(self.stop_after[1])
        for l in range(self.n_layers):
            x_in = d["x"] if l == 0 else self.x1
            last = (l == self.n_layers - 1)
            if self.stop_after == "Bonly":
                self.block_B(l, d["x"], self.out, False)
                return self.nc
            if self.stop_after in (("A", l), ("AS", l), ("A0", l), ("A1", l)):
                self.block_A(l, x_in, self.out)
                return self.nc
            self.block_A(l, x_in, self.xmid)
            self.block_B(l, self.xmid, self.out if last else self.x1, last and self.stop_after is None)
        return self.nc


_CONSTS = None


def _prep_inputs(inputs):
    global _CONSTS
    if _CONSTS is None:
        _CONSTS = _consts()
    inp = {k: np.asarray(v) for k, v in inputs.items()}
    lay = _host_layouts(inp)
    shared = {}
    for k in RAW_SHAPES:
        if k != "x":
            shared[k] = np.ascontiguousarray(inp[k], np.float32)
    shared.update(lay)
    shared.update(_CONSTS)
    maps = []
    for b in range(8):
        m = dict(shared)
        m["x"] = np.ascontiguousarray(inp["x"][b], np.float32)
        maps.append(m)
    return maps


def kernel(**inputs):
    maps = _prep_inputs(inputs)
    nc = Prog().build()
    res = run_bass_kernel_spmd(nc, maps, core_ids=list(range(8)))
    return np.stack([np.asarray(r["out"]) for r in res.results], axis=0).astype(np.float32)
```

```python
import math
from contextlib import ExitStack

import numpy as np
import concourse.bass as bass
import concourse.mybir as mybir
from concourse.bass_utils import run_bass_kernel_spmd

F32 = mybir.dt.float32
BF16 = mybir.dt.bfloat16
ALU = mybir.AluOpType
AF = mybir.ActivationFunctionType
AX = mybir.AxisListType

SEQ = 4096
DM = 1024
NL = 2
EPS = 1e-6
NEG = -30000.0

ENGINES = ("pe", "act", "dve", "pool", "sp")
N_DMA_SEMS = 24


class _Op:
    __slots__ = ("eng", "fn", "deps", "is_dma", "needs_inc", "inc_idx", "dsem", "dval", "gidx")

    def __init__(self, eng, fn, is_dma, gidx):
        self.eng = eng
        self.fn = fn
        self.deps = []
        self.is_dma = is_dma
        self.needs_inc = False
        self.inc_idx = 0
        self.dsem = None
        self.dval = 0
        self.gidx = gidx


class Sched:
    uid = 0

    def __init__(self, nc):
        self.nc = nc
        self.ops = {e: [] for e in ENGINES}
        self.last_write = {}
        self.readers = {}
        self.n = 0
        self.dma_count = 0
        self.dma_last = {}

    def _add(self, eng, fn, reads, writes, is_dma):
        op = _Op(eng, fn, is_dma, self.n)
        self.n += 1
        deps = {}
        raw = set()
        for k in reads:
            w = self.last_write.get(k)
            if w is not None:
                deps[w.gidx] = w
                raw.add(w.gidx)
        for k in writes:
            w = self.last_write.get(k)
            if w is not None:
                deps[w.gidx] = w
            for r in self.readers.get(k, ()):
                deps[r.gidx] = r
        if is_dma:
            j = self.dma_count % N_DMA_SEMS
            self.dma_count += 1
            prev = self.dma_last.get(j)
            if prev is not None:
                deps[prev.gidx] = prev
                op.dval = prev.dval + 16
            else:
                op.dval = 16
            op.dsem = j
            self.dma_last[j] = op
        deps.pop(op.gidx, None)
        op.deps = [(dd, (g in raw) and eng != "pe") for g, dd in deps.items()]
        for k in writes:
            self.last_write[k] = op
            self.readers[k] = []
        for k in reads:
            if k in writes:
                continue
            self.readers.setdefault(k, []).append(op)
        self.ops[eng].append(op)
        return op

    def op(self, eng, fn, reads=(), writes=()):
        return self._add(eng, fn, tuple(reads), tuple(writes), False)

    def dma(self, eng, out, in_, r=(), w=()):
        return self._add(eng, lambda e: e.dma_start(out=out, in_=in_), tuple(r), tuple(w), True)

    def emit(self):
        nc = self.nc
        for e in ENGINES:
            for op in self.ops[e]:
                for d, raw in op.deps:
                    if (not d.is_dma) and (d.eng != op.eng or raw):
                        d.needs_inc = True
        for e in ENGINES:
            c = 0
            for op in self.ops[e]:
                if op.needs_inc and not op.is_dma:
                    c += 1
                    op.inc_idx = c
        Sched.uid += 1
        esem = {e: nc.alloc_semaphore(name="s%d_%s" % (Sched.uid, e)) for e in ENGINES}
        dsem = [nc.alloc_semaphore(name="d%d_%d" % (Sched.uid, j)) for j in range(N_DMA_SEMS)]
        with ExitStack() as es:
            block = es.enter_context(nc.Block())
            engobj = {"pe": "tensor", "act": "scalar", "dve": "vector", "pool": "gpsimd", "sp": "sync"}

            def make(e):
                def body(eng):
                    waited = {}
                    for op in self.ops[e]:
                        for d, raw in op.deps:
                            if d.is_dma:
                                key = ("d", d.dsem)
                                val = d.dval
                                sem = dsem[d.dsem]
                            else:
                                if d.eng == e and not raw:
                                    continue
                                key = ("e", d.eng)
                                val = d.inc_idx
                                sem = esem[d.eng]
                            if waited.get(key, 0) >= val:
                                continue
                            waited[key] = val
                            eng.wait_ge(sem, val)
                        ins = op.fn(eng)
                        if op.is_dma:
                            ins.then_inc(dsem[op.dsem], 16)
                        elif op.needs_inc:
                            ins.then_inc(esem[e], 1)
                    if e == "sp":
                        for d in self.dma_last.values():
                            key = ("d", d.dsem)
                            if waited.get(key, 0) >= d.dval:
                                continue
                            waited[key] = d.dval
                            eng.wait_ge(dsem[d.dsem], d.dval)
                return body

            for e in ENGINES:
                getattr(block, engobj[e])(make(e))
        nc.clear_and_free_semaphores(list(esem.values()) + dsem)
        nc.all_engine_barrier()


def MM(S, out, lhsT, rhs, start=True, stop=True, r=(), w=()):
    S.op("pe", lambda e: e.matmul(out, lhsT, rhs, start=start, stop=stop), r, w)


def TR(S, out, in_, ident, r=(), w=()):
    S.op("pe", lambda e: e.transpose(out, in_, ident), r, w)


def ACT(S, out, in_, func, r=(), w=(), **kw):
    S.op("act", lambda e: e.activation(out=out, in_=in_, func=func, **kw), r, w)


def TT(S, out, in0, in1, op, r=(), w=(), eng="dve"):
    S.op(eng, lambda e: e.tensor_tensor(out=out, in0=in0, in1=in1, op=op), r, w)


def TS(S, out, in0, s1, s2, op0, op1=None, r=(), w=(), eng="dve"):
    if op1 is None:
        S.op(eng, lambda e: e.tensor_scalar(out=out, in0=in0, scalar1=s1, scalar2=None, op0=op0), r, w)
    else:
        S.op(eng, lambda e: e.tensor_scalar(out=out, in0=in0, scalar1=s1, scalar2=s2, op0=op0, op1=op1), r, w)


def STT(S, out, in0, scalar, in1, op0, op1, r=(), w=()):
    S.op("dve", lambda e: e.scalar_tensor_tensor(out=out, in0=in0, scalar=scalar, in1=in1, op0=op0, op1=op1), r, w)


def CP(S, out, in_, r=(), w=(), eng="dve"):
    S.op(eng, lambda e: e.tensor_copy(out=out, in_=in_), r, w)


def MSET(S, ap, val, w=(), eng="pool"):
    S.op(eng, lambda e: e.memset(ap, val), (), w)


def _t5_bucket_np(rel):
    rel = np.asarray(rel, dtype=np.int64)
    n = np.maximum(rel, 0)
    max_exact = 16
    nf = np.maximum(n, 1).astype(np.float32)
    large = max_exact + (np.log(nf / np.float32(max_exact)) / np.float32(math.log(128 / max_exact))
                         * np.float32(32 - max_exact)).astype(np.int32)
    large = np.minimum(large, 31)
    return np.where(n < max_exact, n, large)


def _consts():
    c = {}
    c["ident"] = np.eye(128, dtype=np.float32)
    c["exch"] = np.eye(128, dtype=np.float32)[::-1].copy()
    rel = np.arange(512) - 127
    b = _t5_bucket_np(rel)
    oh = np.zeros((32, 512), np.float32)
    oh[b[rel >= 0], np.arange(512)[rel >= 0]] = 1.0
    ohm = np.zeros((128, 512), np.float32)
    ohm[0:32] = oh
    ohm[32, rel < 0] = NEG
    c["ohm"] = ohm
    negm = np.zeros((128, 16, 16), np.float32)
    for qb in range(16):
        negm[:, qb, qb:] = -1e30
    c["negm"] = negm
    wins = (2, 4, 8, 16)
    winv = np.zeros((128, 2), np.float32)
    invc = np.zeros((128, 2, 16), np.float32)
    t = np.arange(16)
    for ct in range(2):
        for half in range(2):
            w = wins[2 * ct + half]
            winv[64 * half:64 * half + 64, ct] = 1.0 / w
            invc[64 * half:64 * half + 64, ct, :] = 1.0 / np.minimum(t + 1, w)
    c["winv"] = winv
    c["invc"] = invc
    esel = np.zeros((32, 16, 128), np.float32)
    for e in range(16):
        esel[e, e, :] = 1.0
        esel[16 + e, e, :] = 1.0
    c["esel"] = esel
    c["neghalfpi"] = np.full((128, 1), math.pi / 2, np.float32)
    return c


CONST_SHAPES = {"ident": [128, 128], "exch": [128, 128], "ohm": [128, 512],
                "negm": [128, 16, 16], "winv": [128, 2], "invc": [128, 2, 16], "esel": [32, 16, 128],
                "neghalfpi": [128, 1]}


def _host_layouts(inp):
    o = {}
    rep = lambda v: np.ascontiguousarray(np.broadcast_to(v[:, None, :], (v.shape[0], 128, v.shape[1])), np.float32)
    o["g1b"] = rep(inp["norm1_g"])
    o["g2b"] = rep(inp["norm2_g"])
    o["gfb"] = np.ascontiguousarray(np.broadcast_to(inp["final_norm_g"][None, :], (128, DM)), np.float32)
    rbp = np.zeros((128, 128), np.float32)
    rbp[0:32, 0:8] = inp["rel_bias"]
    rbp[32, 0:8] = 1.0
    o["rb"] = rbp
    o["c31b"] = np.ascontiguousarray(np.broadcast_to(inp["rel_bias"][31][None, :], (128, 8)), np.float32)
    def sm(a):
        return np.ascontiguousarray(a.reshape(NL, 8, 2, 64).transpose(0, 2, 3, 1).reshape(NL, 128, 8), np.float32)
    ldt = np.broadcast_to(inp["ssm_log_dt"][:, :, None], (NL, 16, 64))
    o["ssmA"] = np.ascontiguousarray(np.stack([sm(inp["ssm_a_re"]), sm(inp["ssm_a_im"]), sm(ldt)], axis=-1))
    def bexp(b):
        out = np.zeros((NL, 128, 8, 128), np.float32)
        for g in range(16):
            P, two = g // 2, g % 2
            cl = (g % 8) * 16
            out[:, two * 64:(two + 1) * 64, P, cl:cl + 16] = b[:, g]
        return out
    def cexp(cm):
        out = np.zeros((NL, 128, 8, 128), np.float32)
        for g in range(16):
            P, two = g // 2, g % 2
            cl = (g % 8) * 16
            out[:, two * 64:(two + 1) * 64, P, cl:cl + 16] = cm[:, g].transpose(0, 2, 1)
        return out
    o["bexp"] = np.stack([bexp(inp["ssm_b_re"]), bexp(inp["ssm_b_im"])], axis=1)
    o["cexp"] = np.stack([cexp(inp["ssm_c_re"]), cexp(inp["ssm_c_im"])], axis=1)
    chm = lambda v: np.ascontiguousarray(v.reshape(NL, 2, 128).transpose(0, 2, 1), np.float32)
    o["chp"] = np.ascontiguousarray(np.stack([chm(inp["ssm_d"]), chm(inp["ssm_glu_b"]), chm(inp["pool_b"]),
                                              chm(inp["pool_scale"])], axis=-1))
    pw = np.zeros((NL, 2, 128, 128), np.float32)
    for ct in range(2):
        for half in range(2):
            pw[:, ct, 64 * half:64 * half + 64, 64 * half:64 * half + 64] = inp["pool_w"][:, 2 * ct + half]
    o["poolw"] = pw
    wr = np.concatenate([inp["moe_group_w"], inp["moe_router_w"].transpose(0, 2, 1, 3).reshape(NL, DM, 16)], axis=-1)
    o["wr"] = np.ascontiguousarray(wr, np.float32)
    br = np.concatenate([inp["moe_group_b"], inp["moe_router_b"].reshape(NL, 16)], axis=-1)
    o["brb"] = np.ascontiguousarray(np.broadcast_to(br[:, None, :], (NL, 128, 20)), np.float32)
    return o


LAYOUT_SHAPES = {"g1b": [NL, 128, DM], "g2b": [NL, 128, DM], "gfb": [128, DM], "rb": [128, 128], "c31b": [128, 8],
                 "ssmA": [NL, 128, 8, 3], "bexp": [NL, 2, 128, 8, 128], "cexp": [NL, 2, 128, 8, 128],
                 "chp": [NL, 128, 2, 4], "poolw": [NL, 2, 128, 128], "wr": [NL, DM, 20], "brb": [NL, 128, 20]}
RAW_SHAPES = {"x": [SEQ, DM], "w_in": [NL, DM, 2048], "ssm_glu_w": [NL, 256, 256], "w_out": [NL, DM, DM],
              "moe_w_gate": [NL, 16, DM, 256], "moe_w_up": [NL, 16, DM, 256], "moe_w_down": [NL, 16, 256, DM]}


class Prog:
    def __init__(self, n_layers=NL, stop_after=None, dbg=()):
        self.nc = nc = bass.Bass("TRN2", target_bir_lowering=False)
        self.n_layers = n_layers
        self.stop_after = stop_after
        self.dbg = set(dbg)
        self.d = {}
        for name, shp in list(RAW_SHAPES.items()) + list(LAYOUT_SHAPES.items()) + list(CONST_SHAPES.items()):
            self.d[name] = nc.dram_tensor(name, shp, F32, kind="ExternalInput").ap()
        self.out = nc.dram_tensor("out", [SEQ, DM], F32, kind="ExternalOutput").ap()
        self.xmid = nc.dram_tensor("xmid", [SEQ, DM], F32).ap()
        self.x1 = nc.dram_tensor("x1", [SEQ, DM], F32).ap()
        self.qT_d = nc.dram_tensor("qT_d", [4, 128, SEQ], BF16).ap()
        self.Fd = nc.dram_tensor("Fd", [8, 512], F32).ap()
        self.dbg_out = {}

    def nm(self, name):
        self.uid = getattr(self, "uid", 0) + 1
        return "t%d_%s" % (self.uid, name)

    def dbg_tensor(self, name, shape, dt=F32):
        t = self.nc.dram_tensor("dbg_" + name, shape, dt, kind="ExternalOutput").ap()
        self.dbg_out[name] = t
        return t

    def norm_tile(self, S, T, x_ap, xkey, g_tile, hT_dst, hT_key, idx):
        self.norm_tile_a(S, T, x_ap, xkey, g_tile, idx)
        self.norm_tile_b(S, T, hT_dst, hT_key, idx)

    def norm_tile_a(self, S, T, x_ap, xkey, g_tile, idx):
        b = idx % 2
        junk, ss, h = T.get("junk"), T["ss"], T["h"]
        if junk is None:
            ACT(S, h[:, b, :], x_ap, AF.Square, r=[xkey], w=[("h", b), ("ss", b)], accum_out=ss[:, b, 0:1])
        else:
            ACT(S, junk[:, 0, :], x_ap, AF.Square, r=[xkey], w=["junk", ("ss", b)], accum_out=ss[:, b, 0:1])
        TS(S, ss[:, b, 1:2], ss[:, b, 0:1], 1.0 / DM, EPS, ALU.mult, ALU.add, r=[("ss", b)], w=[("ss1", b)])
        ACT(S, ss[:, b, 2:3], ss[:, b, 1:2], AF.Sqrt, r=[("ss1", b)], w=[("ss2", b)])
        S.op("dve", lambda e: e.reciprocal(out=ss[:, b, 3:4], in_=ss[:, b, 2:3]), [("ss2", b)], [("ss3", b)])
        STT(S, h[:, b, :], x_ap, ss[:, b, 3:4], g_tile[:], ALU.mult, ALU.mult, r=[xkey, ("ss3", b), "g"], w=[("h", b)])

    def norm_tile_b(self, S, T, hT_dst, hT_key, idx):
        b = idx % 2
        h, pst = T["h"], T["pst"]
        for kc in range(8):
            TR(S, pst[:, b, kc, :], h[:, b, kc * 128:(kc + 1) * 128], T["ident_bf"][:],
               r=[("h", b), "ident_bf"], w=[("pst", b)])
        if idx % 2 == 0:
            ACT(S, hT_dst, pst[:, b, :, :], AF.Copy, r=[("pst", b)], w=[hT_key])
        else:
            CP(S, hT_dst, pst[:, b, :, :], r=[("pst", b)], w=[hT_key])

    def alloc_norm(self, es, T, junk=True):
        nc = self.nc
        if junk:
            T["junk"] = es.enter_context(nc.sbuf_tensor(self.nm("junk"), [128, 1, DM], BF16))
        T["ss"] = es.enter_context(nc.sbuf_tensor(self.nm("ss"), [128, 2, 4], F32))
        T["h"] = es.enter_context(nc.sbuf_tensor(self.nm("h"), [128, 2, DM], BF16))
        T["pst"] = es.enter_context(nc.psum_tensor(self.nm("pst"), [128, 2, 8, 128], BF16))
        T["ident_bf"] = es.enter_context(nc.sbuf_tensor(self.nm("ident_bf"), [128, 128], BF16))

    def load_ident(self, S, T):
        S.dma("pool", T["ident_bf"][:], self.d["ident"], w=["ident_bf"])

    def block_B(self, l, x_in, x_out, final):
        nc, d = self.nc, self.d
        with ExitStack() as es:
            sb = lambda name, shape, dt=F32: es.enter_context(nc.sbuf_tensor(self.nm(name), shape, dt))
            T = {}
            self.alloc_norm(es, T)
            wd = sb("wd", [128, 32, DM], BF16)
            actall = sb("actall", [128, 32, 1024], BF16)
            h2T = sb("h2T", [128, 8, 1024], BF16)
            wg = sb("wg", [128, 2, 8, 256], BF16)
            wu = sb("wu", [128, 2, 8, 256], BF16)
            g2 = sb("g2", [128, DM])
            gf = sb("gf", [128, DM]) if final else None
            xt = sb("xt", [128, 2, DM])
            wr = sb("wr", [128, 8, 20], BF16)
            brb = sb("brb", [128, 20])
            esel = sb("esel", [32, 16, 128], BF16)
            combT = sb("combT", [32, 1024], BF16)
            rt = sb("rt", [128, 2, 96])
            hl = sb("hl", [128, 2, 32], BF16)
            cb = sb("cb", [128, 2, 512])
            sg = sb("sg", [128, 2, 512])
            fin = sb("fin", [128, 2, 4]) if final else None
            psA = [es.enter_context(nc.psum_tensor(self.nm("psA"), [128, 512], F32)) for i in range(2)]
            psB = [es.enter_context(nc.psum_tensor(self.nm("psB"), [128, 512], F32)) for i in range(2)]
            psC = es.enter_context(nc.psum_tensor(self.nm("psC"), [128, 512], F32))
            psL = es.enter_context(nc.psum_tensor(self.nm("psL"), [128, 512], F32))
            S = Sched(nc)
            self.load_ident(S, T)
            S.dma("sp", g2[:], d["g2b"][l], w=["g"])
            if final:
                S.dma("sp", gf[:], d["gfb"], w=["gf"])
            S.dma("sp", brb[:], d["brb"][l], w=["brb"])
            S.dma("pool", wr[:], d["wr"][l].rearrange("(kc p) n -> p kc n", p=128), w=["wr"])
            S.dma("pool", esel[:], d["esel"], w=["esel"])
            wdv = d["moe_w_down"][l].rearrange("e (ft p) d -> p e ft d", p=128)
            wd4 = wd[:].rearrange("p (e ft) d -> p e ft d", ft=2)

            def load_wd():
                for e4 in range(8):
                    S.dma("pool", wd4[:, 2 * e4:2 * e4 + 2], wdv[:, 2 * e4:2 * e4 + 2], w=[("wd", e4)])

            def load_w(e):
                b = e % 2
                S.dma("pool", wg[:, b], d["moe_w_gate"][l, e].rearrange("(kc p) f -> p kc f", p=128), w=[("wg", b)])
                S.dma("pool", wu[:, b], d["moe_w_up"][l, e].rearrange("(kc p) f -> p kc f", p=128), w=[("wu", b)])

            xtR = sb("xtR", [128, 2, DM])

            def R_a(sc, tt):
                b = tt % 2
                tok0 = sc * 1024 + tt * 128
                if tt == 0:
                    S.dma("sp", xtR[:, 0, :], x_in[tok0:tok0 + 128, :], w=[("xtR", 0)])
                if tt + 1 < 8:
                    S.dma("sp", xtR[:, 1 - b, :], x_in[tok0 + 128:tok0 + 256, :], w=[("xtR", 1 - b)])
                self.norm_tile_a(S, T, xtR[:, b, :], ("xtR", b), g2, tt)

            def R_b(sc, tt):
                b = tt % 2
                tok0 = sc * 1024 + tt * 128
                self.norm_tile_b(S, T, h2T[:, :, tt * 128:(tt + 1) * 128], ("h2T", tt), tt)
                lg = psL[:, b * 64:b * 64 + 20]
                for kc in range(8):
                    MM(S, lg, h2T[:, kc, tt * 128:(tt + 1) * 128], wr[:, kc, :], start=(kc == 0), stop=(kc == 7),
                       r=[("h2T", tt), "wr"], w=["psL"])
                R = rt[:, b, :]
                rk = ("rt", b)
                lgs = R[:, 0:20]
                TT(S, lgs, lg, brb[:], ALU.add, r=["psL", "brb"], w=[rk])
                S.op("dve", lambda e, R=R: e.reduce_max(out=R[:, 20:21], in_=R[:, 0:4], axis=AX.X), [rk], [rk])
                TS(S, R[:, 21:22], R[:, 20:21], -1.0, None, ALU.mult, r=[rk], w=[rk])
                ACT(S, R[:, 24:28], R[:, 0:4], AF.Exp, r=[rk], w=[rk], bias=R[:, 21:22], accum_out=R[:, 22:23])
                S.op("dve", lambda e, R=R: e.reciprocal(out=R[:, 23:24], in_=R[:, 22:23]), [rk], [rk])
                TS(S, R[:, 28:32], R[:, 0:4], R[:, 20:21], None, ALU.is_ge, r=[rk], w=[rk])
                TS(S, R[:, 28:32], R[:, 28:32], -1.0, 1e30, ALU.add, ALU.mult, r=[rk], w=[rk])
                em = R[:, 32:48]
                TT(S, em.rearrange("p (g e) -> p g e", e=4), R[:, 4:20].rearrange("p (g e) -> p g e", e=4),
                   R[:, 28:32].unsqueeze(2).to_broadcast([128, 4, 4]), ALU.add, r=[rk], w=[rk])
                S.op("dve", lambda e, R=R: e.max(out=R[:, 48:56], in_=R[:, 32:48]), [rk], [rk])
                TS(S, R[:, 56:57], R[:, 48:49], -1.0, None, ALU.mult, r=[rk], w=[rk])
                ACT(S, R[:, 64:80], em, AF.Exp, r=[rk], w=[rk], bias=R[:, 56:57])
                ACT(S, R[:, 57:58], R[:, 49:50], AF.Exp, r=[rk], w=[rk], bias=R[:, 56:57])
                TS(S, R[:, 57:58], R[:, 57:58], 1.0, None, ALU.add, r=[rk], w=[rk])
                S.op("dve", lambda e, R=R: e.reciprocal(out=R[:, 58:59], in_=R[:, 57:58]), [rk], [rk])
                TT(S, R[:, 59:60], R[:, 58:59], R[:, 23:24], ALU.mult, r=[rk], w=[rk])
                TS(S, R[:, 80:96], em, R[:, 49:50], None, ALU.is_ge, r=[rk], w=[rk])
                STT(S, R[:, 64:80], R[:, 64:80], R[:, 59:60], R[:, 80:96], ALU.mult, ALU.mult, r=[rk], w=[rk])
                CP(S, hl[:, b, 0:16], R[:, 64:80], r=[rk], w=[("hl", b)])
                TT(S, hl[:, b, 16:32], R[:, 64:80], hl[:, b, 0:16], ALU.subtract, r=[rk, ("hl", b)], w=[("hl", b)])
                if "comb" in self.dbg and l == 0:
                    S.dma("sp", self.dbg_out["comb"][tok0:tok0 + 128, :], R[:, 64:80], r=[rk])

            def R_c(sc, tt):
                b = tt % 2
                hlT = T["pst"][0:32, b, 0, :]
                TR(S, hlT, hl[:, b, :], T["ident_bf"][:], r=[("hl", b), "ident_bf"], w=[("pst", b)])
                CP(S, combT[:, tt * 128:(tt + 1) * 128], hlT, r=[("pst", b)], w=[("combT", tt // 4)])

            def D(sc, tt):
                b = tt % 2
                half = tt // 4
                tok0 = sc * 1024 + tt * 128
                if tt == 0:
                    S.dma("sp", xt[:, 0, :], x_in[tok0:tok0 + 128, :], w=[("xt", 0)])
                if tt + 1 < 8:
                    S.dma("sp", xt[:, 1 - b, :], x_in[tok0 + 128:tok0 + 256, :], w=[("xt", 1 - b)])
                for dh in range(2):
                    ps = psA[dh] if b == 0 else psB[dh]
                    pk = ("psA", dh) if b == 0 else ("psB", dh)
                    for ft in range(32):
                        MM(S, ps[:], actall[:, ft, tt * 128:(tt + 1) * 128], wd[:, ft, dh * 512:(dh + 1) * 512],
                           start=(ft == 0), stop=(ft == 31), r=[("actall", half), ("wd", ft // 4)], w=[pk])
                    TT(S, xt[:, b, dh * 512:(dh + 1) * 512], ps[:], xt[:, b, dh * 512:(dh + 1) * 512], ALU.add,
                       r=[pk, ("xt", b)], w=[("xt", b)])
                if final:
                    ACT(S, T["junk"][:, 0, :], xt[:, b, :], AF.Square, r=[("xt", b)], w=["junk", ("fin", b)],
                        accum_out=fin[:, b, 0:1])
                    TS(S, fin[:, b, 1:2], fin[:, b, 0:1], 1.0 / DM, EPS, ALU.mult, ALU.add, r=[("fin", b)], w=[("fin1", b)])
                    ACT(S, fin[:, b, 2:3], fin[:, b, 1:2], AF.Sqrt, r=[("fin1", b)], w=[("fin2", b)])
                    S.op("dve", lambda e, b=b: e.reciprocal(out=fin[:, b, 3:4], in_=fin[:, b, 2:3]), [("fin2", b)], [("fin3", b)])
                    STT(S, xt[:, b, :], xt[:, b, :], fin[:, b, 3:4], gf[:], ALU.mult, ALU.mult,
                        r=[("xt", b), ("fin3", b), "gf"], w=[("xt", b)])
                S.dma("sp", x_out[tok0:tok0 + 128, :], xt[:, b, :], r=[("xt", b)])

            def E(sc):
                for e in range(16):
                    wb = e % 2
                    if e + 1 < 16:
                        load_w(e + 1)
                    elif sc + 1 < 4:
                        load_w(0)
                    for half in range(2):
                        hk = [("h2T", 4 * half + i) for i in range(4)]
                        MM(S, psC[:], esel[:, e, :], combT[:, half * 512:(half + 1) * 512],
                           r=["esel", ("combT", half)], w=["psC"])
                        ACT(S, cb[:, half, :], psC[:], AF.Copy, r=["psC"], w=[("cb", half)])
                        for ft in range(2):
                            pb = (half * 2 + ft) % 2
                            for kc in range(8):
                                MM(S, psA[pb][:], wg[:, wb, kc, ft * 128:(ft + 1) * 128], h2T[:, kc, half * 512:(half + 1) * 512],
                                   start=(kc == 0), stop=(kc == 7), r=hk + [("wg", wb)], w=[("psA", pb)])
                            for kc in range(8):
                                MM(S, psB[pb][:], wu[:, wb, kc, ft * 128:(ft + 1) * 128], h2T[:, kc, half * 512:(half + 1) * 512],
                                   start=(kc == 0), stop=(kc == 7), r=hk + [("wu", wb)], w=[("psB", pb)])
                            ACT(S, sg[:, pb, :], psA[pb][:], AF.Silu, r=[("psA", pb)], w=[("sg", pb)])
                            TT(S, sg[:, pb, :], psB[pb][:], sg[:, pb, :], ALU.mult, r=[("psB", pb), ("sg", pb)], w=[("sg", pb)])
                            TT(S, actall[:, 2 * e + ft, half * 512:(half + 1) * 512], sg[:, pb, :], cb[:, half, :], ALU.mult,
                               r=[("sg", pb), ("cb", half)], w=[("actall", half)], eng="pool")

            R_a(0, 0)
            R_a(0, 1)
            for tt in range(8):
                R_b(0, tt)
                if tt + 2 < 8:
                    R_a(0, tt + 2)
                R_c(0, tt)
            load_w(0)
            load_wd()
            for sc in range(4):
                E(sc)
                nxt = sc + 1 < 4
                if nxt:
                    R_a(sc + 1, 0)
                    R_b(sc + 1, 0)
                    R_a(sc + 1, 1)
                for tt in range(8):
                    D(sc, tt)
                    if nxt:
                        R_c(sc + 1, tt)
                        if tt + 1 < 8:
                            R_b(sc + 1, tt + 1)
                        if tt + 2 < 8:
                            R_a(sc + 1, tt + 2)
            S.emit()


    def cmul(self, S, o_re, o_im, a_re, a_im, b_re, b_im, t1, t2, r, w, tk):
        TT(S, t1, a_re, b_re, ALU.mult, r=r, w=[tk + "1"])
        TT(S, t2, a_im, b_im, ALU.mult, r=r, w=[tk + "2"])
        TT(S, o_re, t1, t2, ALU.subtract, r=[tk + "1", tk + "2"], w=w)
        TT(S, t1, a_re, b_im, ALU.mult, r=r, w=[tk + "1"])
        TT(S, t2, a_im, b_re, ALU.mult, r=r, w=[tk + "2"])
        TT(S, o_im, t1, t2, ALU.add, r=[tk + "1", tk + "2"], w=w)

    def block_proj(self, l, x_in, mode, R):
        nc, d = self.nc, self.d
        ncols = 512 if mode == "up" else 1536
        c0 = 0 if mode == "up" else 512
        with ExitStack() as es:
            sb = lambda name, shape, dt=F32: es.enter_context(nc.sbuf_tensor(self.nm(name), shape, dt))
            ps = lambda name, shape, dt=F32: es.enter_context(nc.psum_tensor(self.nm(name), shape, dt))
            T = {}
            self.alloc_norm(es, T, junk=False)
            win = sb("win", [128, 8, ncols], BF16)
            g1 = sb("g1", [128, DM])
            NXB = 3
            xt = sb("xt", [128, NXB, DM])
            hT = sb("hT", [128, 2, 8, 512], BF16)
            psP = [ps("psP", [128, 512]) for _ in range(2)]
            S = Sched(nc)
            self.load_ident(S, T)
            S.dma("sp", g1[:], d["g1b"][l], w=["g"])
            wv = d["w_in"][l].rearrange("(kc p) n -> p kc n", p=128)
            for kc in range(8):
                S.dma("pool", win[:, kc, :], wv[:, kc, c0:c0 + ncols], w=[("win", kc)])
            wkeys = [("win", kc) for kc in range(8)]
            if mode == "up":
                pbuf = sb("pbuf", [128, 2, 2, 528])
                sA = sb("sA", [128, 2, 528])
                sB = sb("sB", [128, 2, 528])
                dlt = sb("dlt", [128, 2, 512], BF16)
                tfix = sb("tfix", [128, 16])
                poolw = sb("poolw", [128, 2, 128], BF16)
                winv = sb("winv", [128, 2])
                invc = sb("invc", [128, 2, 16])
                psQ = ps("psQ", [128, 2, 512])
                S.dma("pool", poolw[:], d["poolw"][l].rearrange("c k m -> k c m"), w=["poolw"])
                S.dma("sp", winv[:], d["winv"], w=["winv"])
                S.dma("sp", invc[:], d["invc"], w=["invc"])
                S.dma("sp", R["chp"][:], d["chp"][l], w=["chp"])
                MSET(S, pbuf[:, 0, :, 0:16], 0.0, w=[("pbuf", 0)])
                self.ssm_setup(S, l, es, R, T)
            else:
                qst = sb("qst", [128, 2, 4, 512], BF16)
                MSET(S, R["Vaug"][:, :, :, 64:65], 1.0, w=["vones"])
                MSET(S, R["ksum"][:], 0.0, w=["ksum"])
            def load_x(Tg):
                S.dma("sp", xt[:, Tg % NXB, :], x_in[Tg * 128:(Tg + 1) * 128, :], w=[("xt", Tg % NXB)])

            def norm_a(Tg):
                self.norm_tile_a(S, T, xt[:, Tg % NXB, :], ("xt", Tg % NXB), g1, Tg)

            for Tg in range(NXB):
                load_x(Tg)
            norm_a(0)
            norm_a(1)
            for c in range(8):
                hb = c % 2
                for tt in range(4):
                    Tg = c * 4 + tt
                    self.norm_tile_b(S, T, hT[:, hb, :, tt * 128:(tt + 1) * 128], ("hT", hb, tt), Tg)
                    if Tg + NXB < 32:
                        load_x(Tg + NXB)
                    if Tg + 2 < 32:
                        norm_a(Tg + 2)
                    if mode == "up" and tt == 2 and c > 0:
                        self.pool_chunk_back(S, l, c - 1, 1 - hb, pbuf, sA, sB, dlt, tfix, poolw, winv, invc, psQ, R)
                hk = [("hT", hb, tt) for tt in range(4)]
                nmt = ncols // 128 if mode == "up" else 8
                for mt in range(nmt):
                    pb = mt % 2
                    for kc in range(8):
                        MM(S, psP[pb][:], win[:, kc, mt * 128:(mt + 1) * 128], hT[:, hb, kc, :], start=(kc == 0),
                           stop=(kc == 7), r=hk + wkeys, w=[("psP", pb)])
                    if mode == "up":
                        if mt < 2:
                            ACT(S, R["u_sb"][:, mt, c * 512:(c + 1) * 512], psP[pb][:], AF.Copy, r=[("psP", pb)], w=[("u", mt)])
                        else:
                            ACT(S, pbuf[:, hb, mt - 2, 16:528], psP[pb][:], AF.Copy, r=[("psP", pb)], w=[("pbuf", hb)])
                    else:
                        if mt < 4:
                            ACT(S, qst[:, hb, mt, :], psP[pb][:], AF.Copy, r=[("psP", pb)], w=[("qst", hb)], scale=0.125)
                        else:
                            hp = mt - 4
                            for bl in range(2):
                                blk = 2 * c + bl
                                ACT(S, R["kT"][:, hp, blk * 256:(blk + 1) * 256], psP[pb][:, bl * 256:(bl + 1) * 256], AF.Copy,
                                    r=[("psP", pb)], w=["kT", "ksum"], accum_out=R["ksum"][:, hp, blk:blk + 1])
                if mode == "up":
                    self.pool_chunk_front(S, l, c, hb, pbuf, sA, sB)
                    if c == 7:
                        self.pool_chunk_back(S, l, c, hb, pbuf, sA, sB, dlt, tfix, poolw, winv, invc, psQ, R)
                else:
                    S.dma("sp", self.qT_d[:, :, c * 512:(c + 1) * 512].rearrange("hp p t -> p hp t"), qst[:, hb], r=[("qst", hb)])
                    for tt in range(4):
                        pb = tt % 2
                        for kc in range(8):
                            MM(S, psP[pb][:], hT[:, hb, kc, tt * 128:(tt + 1) * 128], win[:, kc, 1024:1536], start=(kc == 0),
                               stop=(kc == 7), r=hk + wkeys, w=[("psP", pb)])
                        CP(S, R["Vaug"][:, c * 4 + tt, :, 0:64], psP[pb][:].rearrange("p (h d) -> p h d", d=64),
                           r=[("psP", pb)], w=["Vaug"])
            if mode == "qkv":
                TS(S, R["kmeanT"][:], R["ksum"][:], 1.0 / 256.0, None, ALU.mult, r=["ksum"], w=["kmeanT"])
            S.emit()

    def pool_chunk_front(self, S, l, c, hb, pbuf, sA, sB):
        pk = ("pbuf", hb)
        if c + 1 < 8:
            CP(S, pbuf[:, 1 - hb, :, 0:16], pbuf[:, hb, :, 512:528], r=[pk], w=[("pbuf", 1 - hb)], eng="pool")
        add = lambda o, a, b_, r, w: TT(S, o, a, b_, ALU.add, r=r, w=w, eng="pool")
        for ct in range(2):
            p = pbuf[:, hb, ct, :]
            a_, b_ = sA[:, ct, :], sB[:, ct, :]
            ka, kb = ("sA", ct), ("sB", ct)
            add(a_[:, 1:528], p[:, 1:528], p[:, 0:527], [pk], [ka])
            if ct == 0:
                add(b_[64:128, 3:528], a_[64:128, 3:528], a_[64:128, 1:526], [ka], [kb])
            else:
                add(b_[:, 3:528], a_[:, 3:528], a_[:, 1:526], [ka], [kb])
                add(a_[:, 7:528], b_[:, 7:528], b_[:, 3:524], [kb], [ka])
                add(b_[64:128, 15:528], a_[64:128, 15:528], a_[64:128, 7:520], [ka], [kb])

    def pool_chunk_back(self, S, l, c, hb, pbuf, sA, sB, dlt, tfix, poolw, winv, invc, psQ, R):
        pk = ("pbuf", hb)
        for ct in range(2):
            p = pbuf[:, hb, ct, :]
            for half, src, sk in ((0, sA[:, ct, :], ("sA", ct)), (1, sB[:, ct, :], ("sB", ct))):
                rows = slice(64 * half, 64 * half + 64)
                STT(S, dlt[rows, ct, :], src[rows, 16:528], winv[rows, ct:ct + 1], p[rows, 16:528], ALU.mult, ALU.subtract,
                    r=[sk, pk, "winv"], w=[("dlt", ct)])
                if c == 0:
                    TT(S, tfix[rows, :], src[rows, 16:32], invc[rows, ct, :], ALU.mult, r=[sk, "invc"], w=["tfix"])
                    TT(S, dlt[rows, ct, 0:16], tfix[rows, :], p[rows, 16:32], ALU.subtract, r=["tfix", pk], w=[("dlt", ct)])
            MM(S, psQ[:, ct, :], poolw[:, ct, :], dlt[:, ct, :], r=["poolw", ("dlt", ct)], w=[("psQ", ct)])
        for ct in range(2):
            TS(S, R["ypT"][:, ct, c * 512:(c + 1) * 512], psQ[:, ct, :], R["chp"][:, ct, 2:3], R["chp"][:, ct, 3:4], ALU.add, ALU.mult,
               r=[("psQ", ct), "chp"], w=[("ypT", ct)])

    def ssm_setup(self, S, l, es, R, T):
        nc, d = self.nc, self.d
        sb = lambda name, shape, dt=F32: es.enter_context(nc.sbuf_tensor(self.nm(name), shape, dt))
        ps = lambda name, shape, dt=F32: es.enter_context(nc.psum_tensor(self.nm(name), shape, dt))
        sa = sb("sa", [128, 8, 3])
        sm = sb("sm", [128, 24, 8])
        Lre = sb("Lre", [128, 9, 8]); Lim = sb("Lim", [128, 9, 8])
        Fre = sb("Fre", [128, 8, 8]); Fim = sb("Fim", [128, 8, 8])
        tA = sb("tA", [128, 8, 8]); tB = sb("tB", [128, 8, 8])
        Bre = sb("Bre", [128, 8, 128]); Bim = sb("Bim", [128, 8, 128])
        Cre = sb("Cre", [128, 8, 128]); Cim = sb("Cim", [128, 8, 128]); nCim = sb("nCim", [128, 8, 128])
        scr = R["ysT"][:].bitcast(F32)
        carve = lambda i: scr[:, i // 2, (i % 2) * 1024:(i % 2) * 1024 + 1024].rearrange("p (a b) -> p a b", b=128)
        LBre, LBim, t1, t2 = carve(0), carve(1), carve(2), carve(3)
        LBb = sb("LBb", [128, 2, 8, 128], BF16)
        hpi = sb("hpi", [128, 1])
        psW = ps("psW", [128, 8, 128], BF16)
        psK = ps("psK", [128, 512])
        glu = R["glu_sb"]
        S.dma("pool", glu[:], d["ssm_glu_w"][l].rearrange("(kc p) n -> p kc n", p=128), w=["glu"])
        S.dma("sp", sa[:], d["ssmA"][l], w=["sa"])
        S.dma("sp", hpi[:], d["neghalfpi"], w=["hpi"])
        S.dma("sp", Bre[:], d["bexp"][l, 0], w=["Bre"])
        S.dma("sp", Bim[:], d["bexp"][l, 1], w=["Bim"])
        S.dma("sp", Cre[:], d["cexp"][l, 0], w=["Cre"])
        S.dma("sp", Cim[:], d["cexp"][l, 1], w=["Cim"])
        TS(S, nCim[:], Cim[:], -1.0, None, ALU.mult, r=["Cim"], w=["nCim"])
        a_re, a_im, ldt = sa[:, :, 0], sa[:, :, 1], sa[:, :, 2]
        k = lambda i: ("sm", i)
        sl = lambda i: sm[:, i, :]
        ACT(S, sl(0), ldt, AF.Exp, r=["sa"], w=[k(0)])
        TT(S, sl(1), a_re, sl(0), ALU.mult, r=["sa", k(0)], w=[k(1)])
        TT(S, sl(2), a_im, sl(0), ALU.mult, r=["sa", k(0)], w=[k(2)])
        ACT(S, sl(3), sl(1), AF.Exp, r=[k(1)], w=[k(3)], scale=1.0 / 32)
        ACT(S, sl(4), sl(2), AF.Sin, r=[k(2)], w=[k(4)], scale=1.0 / 32)
        ACT(S, sl(5), sl(2), AF.Sin, r=[k(2), "hpi"], w=[k(5)], scale=1.0 / 32, bias=hpi[:, 0:1])
        TT(S, sl(6), sl(3), sl(5), ALU.mult, r=[k(3), k(5)], w=[k(6)])
        TT(S, sl(7), sl(3), sl(4), ALU.mult, r=[k(3), k(4)], w=[k(7)])
        for _ in range(5):
            TT(S, sl(8), sl(6), sl(6), ALU.mult, r=[k(6)], w=[k(8)])
            TT(S, sl(9), sl(7), sl(7), ALU.mult, r=[k(7)], w=[k(9)])
            TT(S, sl(10), sl(6), sl(7), ALU.mult, r=[k(6), k(7)], w=[k(10)])
            TT(S, sl(6), sl(8), sl(9), ALU.subtract, r=[k(8), k(9)], w=[k(6)])
            TS(S, sl(7), sl(10), 2.0, None, ALU.mult, r=[k(10)], w=[k(7)])
        MSET(S, Lre[:, 0, :], 1.0, w=[("L", 0)], eng="dve")
        MSET(S, Lim[:, 0, :], 0.0, w=[("L", 0)], eng="dve")
        CP(S, Lre[:, 1, :], sl(6), r=[k(6)], w=[("L", 1)])
        CP(S, Lim[:, 1, :], sl(7), r=[k(7)], w=[("L", 1)])
        for j in range(1, 8):
            self.cmul(S, Lre[:, j + 1, :], Lim[:, j + 1, :], Lre[:, j, :], Lim[:, j, :], sl(6), sl(7), sl(8), sl(9),
                      r=[("L", j), k(6), k(7)], w=[("L", j + 1)], tk="smt")
        Ak = R["Ak"]
        CP(S, Ak[:, 0, :, 0], Lre[:, 8, :], r=[("L", 8)], w=[("Ak", 0)])
        CP(S, Ak[:, 0, :, 1], Lim[:, 8, :], r=[("L", 8)], w=[("Ak", 0)])
        for kk in range(8):
            self.cmul(S, Ak[:, kk + 1, :, 0], Ak[:, kk + 1, :, 1], Ak[:, kk, :, 0], Ak[:, kk, :, 1], Ak[:, kk, :, 0], Ak[:, kk, :, 1],
                      sl(8), sl(9), r=[("Ak", kk)], w=[("Ak", kk + 1)], tk="smt")
        for kk in range(9):
            TS(S, Ak[:, kk, :, 2], Ak[:, kk, :, 1], -1.0, None, ALU.mult, r=[("Ak", kk)], w=[("Akn", kk)])
        TT(S, sl(11), a_re, a_re, ALU.mult, r=["sa"], w=[k(11)])
        TT(S, sl(12), a_im, a_im, ALU.mult, r=["sa"], w=[k(12)])
        TT(S, sl(11), sl(11), sl(12), ALU.add, r=[k(11), k(12)], w=[k(11)])
        S.op("dve", lambda e: e.reciprocal(out=sm[:, 12, :], in_=sm[:, 11, :]), [k(11)], [k(12)])
        TS(S, sl(13), sl(6), -1.0, None, ALU.add, r=[k(6)], w=[k(13)])
        TT(S, sl(14), sl(13), a_re, ALU.mult, r=[k(13), "sa"], w=[k(14)])
        TT(S, sl(15), sl(7), a_im, ALU.mult, r=[k(7), "sa"], w=[k(15)])
        TT(S, sl(14), sl(14), sl(15), ALU.add, r=[k(14), k(15)], w=[k(14)])
        TT(S, sl(16), sl(14), sl(12), ALU.mult, r=[k(14), k(12)], w=[k(16)])
        TT(S, sl(14), sl(7), a_re, ALU.mult, r=[k(7), "sa"], w=[k(14)])
        TT(S, sl(15), sl(13), a_im, ALU.mult, r=[k(13), "sa"], w=[k(15)])
        TT(S, sl(14), sl(14), sl(15), ALU.subtract, r=[k(14), k(15)], w=[k(14)])
        TT(S, sl(17), sl(14), sl(12), ALU.mult, r=[k(14), k(12)], w=[k(17)])
        Lk = [("L", j) for j in range(9)]
        bc = lambda ap: ap.unsqueeze(1).to_broadcast([128, 8, 8])
        self.cmul(S, Fre[:], Fim[:], Lre[:, 0:8, :], Lim[:, 0:8, :], bc(sl(16)), bc(sl(17)), tA[:], tB[:],
                  r=Lk + [k(16), k(17)], w=["F"], tk="tAB")
        bc3 = lambda ap: ap.unsqueeze(2).to_broadcast([128, 8, 128])
        Wss, Kmat, Vm = R["Wss"], R["Kmat"], R["Vm"]
        for j in range(8):
            self.cmul(S, LBre, LBim, bc3(Fre[:, j, :]), bc3(Fim[:, j, :]), Bre[:], Bim[:], t1, t2,
                      r=["F", "Bre", "Bim"], w=["LB"], tk="t12")
            CP(S, LBb[:, 0], LBre[:], r=["LB"], w=["LBb"])
            CP(S, LBb[:, 1], LBim[:], r=["LB"], w=["LBb"])
            for ri in range(2):
                for P in range(8):
                    TR(S, psW[:, P, :], LBb[:, ri, P, :], T["ident_bf"][:], r=["LBb", "ident_bf"], w=["psW"])
                ACT(S, Wss[:, :, :, 7 - j, ri, :].rearrange("p a b s -> p (a b) s"), psW[:], AF.Copy, r=["psW"], w=["Wss"])
            for ct in range(2):
                reg = psK[:, ct * 128:(ct + 1) * 128]
                for i in range(4):
                    P = ct * 4 + i
                    MM(S, reg, LBre[:, P, :], Cre[:, P, :], start=(i == 0), stop=False, r=["LB", "Cre"], w=["psK"])
                for i in range(4):
                    P = ct * 4 + i
                    MM(S, reg, LBim[:, P, :], nCim[:, P, :], start=False, stop=(i == 3), r=["LB", "nCim"], w=["psK"])
            ACT(S, Kmat[:, :, j, :], psK[:, 0:256].rearrange("p (c m) -> p c m", m=128), AF.Copy, r=["psK"], w=["Kmat"])
        for tau in range(8):
            self.cmul(S, LBre, LBim, bc3(Lre[:, tau + 1, :]), bc3(Lim[:, tau + 1, :]), Cre[:], Cim[:], t1[:], t2[:],
                      r=Lk + ["Cre", "Cim"], w=["LB"], tk="t12")
            CP(S, Vm[:, :, 0, tau, :], LBre[:], r=["LB"], w=["Vm"])
            TS(S, Vm[:, :, 1, tau, :], LBim[:], -1.0, None, ALU.mult, r=["LB"], w=["Vm"])

    def block_ssm(self, l, R):
        nc = self.nc
        with ExitStack() as es:
            sb = lambda name, shape, dt=F32: es.enter_context(nc.sbuf_tensor(self.nm(name), shape, dt))
            ps = lambda name, shape, dt=F32: es.enter_context(nc.psum_tensor(self.nm(name), shape, dt))
            Xs = sb("Xs", [128, 2, 2, 768])
            Xprev = sb("Xprev", [128, 8, 2, 512], BF16)
            t1 = sb("t1s", [128, 4096])
            ygb = sb("ygb", [128, 2, 4096], BF16)
            sgl = sb("sgl", [128, 2, 512])
            psZ = [ps("psZ", [128, 512]) for _ in range(2)]
            psY = [ps("psY", [128, 512]) for _ in range(2)]
            psG = [ps("psG", [128, 512]) for _ in range(2)]
            S = Sched(nc)
            u_sb, Wss, Kmat, Vm, Ak, chp, glu, ysT = (R[k_] for k_ in ("u_sb", "Wss", "Kmat", "Vm", "Ak", "chp", "glu_sb", "ysT"))
            MSET(S, Xs[:, :, :, 0:256], 0.0, w=["Xs0", "Xs1"])
            MSET(S, Xprev[:, :, :, 0:1], 0.0, w=["Xprev"])
            u4 = [u_sb[:, ct, :].rearrange("p (s t) -> p t s", t=8) for ct in range(2)]
            for P in range(8):
                ct, pi = P // 4, P % 4
                for ri in range(2):
                    z = psZ[ri]
                    for sg in range(8):
                        MM(S, z[:], Wss[:, ct, pi, sg, ri, :], u4[ct][:, sg, :], start=(sg == 0), stop=(sg == 7),
                           r=["Wss", ("u", ct)], w=[("psZ", ri)])
                    CP(S, Xs[:, 0, ri, 256:768], z[:], r=[("psZ", ri)], w=["Xs0"])
                pp = 0
                for kk in range(9):
                    o = 1 << kk
                    src, dst = Xs[:, pp], Xs[:, 1 - pp]
                    sk, dk = "Xs%d" % pp, "Xs%d" % (1 - pp)
                    are, aim, naim = Ak[:, kk, P:P + 1, 0], Ak[:, kk, P:P + 1, 1], Ak[:, kk, P:P + 1, 2]
                    sh = slice(256 - o, 768 - o)
                    STT(S, dst[:, 0, 256:768], src[:, 0, sh], are, src[:, 0, 256:768], ALU.mult, ALU.add, r=[sk], w=[dk])
                    STT(S, dst[:, 0, 256:768], src[:, 1, sh], naim, dst[:, 0, 256:768], ALU.mult, ALU.add, r=[sk, dk], w=[dk])
                    STT(S, dst[:, 1, 256:768], src[:, 1, sh], are, src[:, 1, 256:768], ALU.mult, ALU.add, r=[sk], w=[dk])
                    STT(S, dst[:, 1, 256:768], src[:, 0, sh], aim, dst[:, 1, 256:768], ALU.mult, ALU.add, r=[sk, dk], w=[dk])
                    pp = 1 - pp
                CP(S, Xprev[:, P, :, 1:512], Xs[:, pp, :, 256:767], r=["Xs%d" % pp], w=["Xprev"])
            for ct in range(2):
                t1v = t1[:].rearrange("p (s t) -> p t s", t=8)
                for tau in range(8):
                    y = psY[tau % 2]
                    yk = ("psY", tau % 2)
                    n_mm = tau + 1 + 8
                    i = 0
                    for sg in range(tau + 1):
                        MM(S, y[:], Kmat[:, ct, tau - sg, :], u4[ct][:, sg, :], start=(i == 0), stop=(i == n_mm - 1), r=[], w=[yk])
                        i += 1
                    for pi in range(4):
                        for ri in range(2):
                            MM(S, y[:], Vm[:, ct * 4 + pi, ri, tau, :], Xprev[:, ct * 4 + pi, ri, :], start=(i == 0),
                               stop=(i == n_mm - 1), r=["Xprev"], w=[yk])
                            i += 1
                    STT(S, t1v[:, tau, :], u4[ct][:, tau, :], chp[:, ct, 0:1], y[:], ALU.mult, ALU.add, r=[yk], w=["t1"])
                for cc in range(8):
                    ACT(S, ygb[:, ct, cc * 512:(cc + 1) * 512], t1[:, cc * 512:(cc + 1) * 512], AF.Gelu_apprx_tanh,
                        r=["t1"], w=[("ygb", ct)])
            for cc in range(8):
                for mt in range(2):
                    g = psG[mt]
                    for kc in range(2):
                        MM(S, g[:], glu[:, kc, mt * 128:(mt + 1) * 128], ygb[:, kc, cc * 512:(cc + 1) * 512], start=(kc == 0),
                           stop=(kc == 1), r=[("ygb", 0), ("ygb", 1)], w=[("psG", mt)])
                    ACT(S, sgl[:, mt, :], g[:], AF.Sigmoid, r=[("psG", mt)], w=[("sgl", mt)], bias=chp[:, mt, 1:2])
                    TT(S, ysT[:, mt, cc * 512:(cc + 1) * 512], ygb[:, mt, cc * 512:(cc + 1) * 512], sgl[:, mt, :], ALU.mult,
                       r=[("sgl", mt), ("ygb", mt)], w=["ysT"])
            S.emit()

    def block_attn(self, l, x_in, x_out, R, nqb=16):
        nc, d = self.nc, self.d
        with ExitStack() as es:
            sb = lambda name, shape, dt=F32: es.enter_context(nc.sbuf_tensor(self.nm(name), shape, dt))
            ps = lambda name, shape, dt=F32: es.enter_context(nc.psum_tensor(self.nm(name), shape, dt))
            kT, Vaug, kmeanT, ysT, ypT = (R[k_] for k_ in ("kT", "Vaug", "kmeanT", "ysT", "ypT"))
            wout = sb("wout", [128, 8, DM], BF16)
            qT = sb("qT", [128, 2, 4, 2, 256], BF16)
            G = sb("G", [128, 8, 256])
            G2 = sb("G2", [128, 8, 256])
            HK = sb("HK", [128, 8, 256])
            c31 = sb("c31", [128, 8])
            negm = sb("negm", [128, 16, 16])
            PT = sb("PT", [128, 3, 2, 256], BF16)
            tmp = sb("tmp", [128, 3, 2, 256])
            gate = sb("gate", [128, 16, 16])
            top8 = sb("top8", [128, 16, 8])
            sel = sb("sel", [128, 2, 2, 8, 16])
            acc = sb("acc", [128, 2, 2, 8, 65])
            rec = sb("rec", [128, 2, 8])
            att = sb("att", [128, 2, 512], BF16)
            attT = sb("attT", [128, 2, 4, 256], BF16)
            xr = sb("xr", [128, 2, DM])
            ident = sb("identb", [128, 128], BF16)
            exch = sb("exch", [128, 128])
            rb = sb("rb", [128, 128]); ohm = sb("ohm", [128, 512])
            Fs = sb("Fs", [8, 512])
            pW = [ps("pW", [128, 512]) for _ in range(2)]
            pOb = [ps("pO", [128, 512]) for _ in range(2)]
            pO = [t_[:, 0:455].rearrange("p (s d) -> p s d", d=65) for t_ in pOb]
            pT = ps("pT", [128, 2, 4, 128], BF16)
            st = [ps("st", [128, 2, 256]) for _ in range(3)]
            S = Sched(nc)
            S.dma("pool", ident[:], d["ident"], w=["ident"])
            for nm_, t_ in (("exch", exch), ("rb", rb), ("ohm", ohm), ("negm", negm)):
                S.dma("sp", t_[:], d[nm_], w=[nm_])
            S.dma("sp", c31[:], d["c31b"], w=["c31"])
            wv = d["w_out"][l].rearrange("(kc p) n -> p kc n", p=128)
            for kc in range(8):
                S.dma("pool", wout[:, kc, :], wv[:, kc, :], w=[("wout", kc)])
            MM(S, pW[0][:], rb[:], ohm[:], r=["rb", "ohm"], w=[("pW", 0)])
            CP(S, Fs[:], pW[0][0:8, :], r=[("pW", 0)], w=["Fs"])
            fd = S.dma("sp", self.Fd, Fs[:], r=["Fs"], w=["Fd"])
            for off, dst, nm_ in ((0, G, "G"), (128, G2, "G2")):
                S.dma("sp", HK[:], bass.AP(tensor=self.Fd.tensor, offset=off, ap=[[1, 128], [512, 8], [1, 256]]), r=["Fd"], w=["HK"])
                for h in range(8):
                    pw = pW[h % 2]
                    MM(S, pw[:, 0:256], exch[:], HK[:, h, :], r=["exch", "HK"], w=[("pW", h % 2)])
                    CP(S, dst[:, h, :], pw[:, 0:256], r=[("pW", h % 2)], w=[nm_])
            MSET(S, qT[:], 0.0, w=[("qT", 0), ("qT", 1)])
            oslot = [0]

            def emit_qload(qb):
                qbuf = qb % 2
                qk = ("qT", qbuf)
                for par_ in range(2):
                    rs = slice(64 * par_, 64 * par_ + 64)
                    S.dma("sp", qT[rs, qbuf, :, par_, :], self.qT_d[:, rs, qb * 256:(qb + 1) * 256].rearrange("hp p t -> p hp t"),
                          w=[qk])

            def emit_head(qb):
                qbuf = qb % 2
                qk = ("qT", qbuf)
                if qb + 1 < nqb:
                    emit_qload(qb + 1)
                if qb >= 4:
                    pg = pW[0][:, 0:256].rearrange("p (i n) -> p i n", n=16)
                    for h in range(8):
                        hp = h // 2
                        for qt in range(2):
                            MM(S, pg[:, qt * 8 + h, :], qT[:, qbuf, hp, h % 2, qt * 128:(qt + 1) * 128], kmeanT[:, hp, :],
                               r=[qk, "kmeanT"], w=[("pW", 0)])
                    TT(S, gate[:], pg, negm[:, qb, :].unsqueeze(1).to_broadcast([128, 16, 16]), ALU.add,
                       r=[("pW", 0), "negm"], w=["gate"])
                    for i_ in range(16):
                        S.op("dve", lambda e, i_=i_: e.max(out=top8[:, i_, :], in_=gate[:, i_, :]), ["gate"], ["top8"])
                    TT(S, sel[:, qbuf].rearrange("p q h n -> p (q h) n"), gate[:],
                       top8[:, :, 2].unsqueeze(2).to_broadcast([128, 16, 16]), ALU.is_ge, r=["gate", "top8"], w=[("sel", qbuf)])

            def emit_S(it, idx):
                qb, n, h = it
                qbuf, hp, par, b = qb % 2, h // 2, h % 2, idx % 3
                own, prev = (n == qb), (n == qb - 1)
                stb, ptb = st[b], PT[:, b]
                sk, pk, tk, qk = ("st", b), ("PT", b), ("tmp", b), ("qT", qbuf)
                q_all = qT[:, qbuf, hp, par, :]
                MM(S, stb[:, 0, :], kT[:, hp, n * 256:n * 256 + 128], q_all, r=[qk, "kT"], w=[sk])
                if own:
                    MM(S, stb[:, 1, 128:256], kT[:, hp, n * 256 + 128:n * 256 + 256], q_all[:, 128:256], r=[qk, "kT"], w=[sk])
                    TT(S, tmp[:, b, 0, :], stb[:, 0, :], G[:, h, :], ALU.add, r=[sk, "G"], w=[tk])
                    TT(S, tmp[:, b, 1, 128:256], stb[:, 1, 128:256], G[:, h, 0:128], ALU.add, r=[sk, "G"], w=[tk])
                    ACT(S, ptb[:, 0, :], tmp[:, b, 0, :], AF.Exp, r=[tk], w=[pk])
                    ACT(S, ptb[:, 1, 128:256], tmp[:, b, 1, 128:256], AF.Exp, r=[tk], w=[pk])
                else:
                    MM(S, stb[:, 1, :], kT[:, hp, n * 256 + 128:n * 256 + 256], q_all, r=[qk, "kT"], w=[sk])
                    if prev:
                        TT(S, tmp[:, b, 1, :], stb[:, 1, :], G2[:, h, :], ALU.add, r=[sk, "G2"], w=[tk])
                        ACT(S, ptb[:, 0, :], stb[:, 0, :], AF.Exp, r=[sk, tk, "c31"], w=[pk], bias=c31[:, h:h + 1])
                        ACT(S, ptb[:, 1, :], tmp[:, b, 1, :], AF.Exp, r=[tk], w=[pk])
                    else:
                        ACT(S, ptb[:], stb[:], AF.Exp, r=[sk, "c31"], w=[pk], bias=c31[:, h:h + 1])

            def emit_PV(it, idx):
                qb, n, h = it
                qbuf, b = qb % 2, idx % 3
                own = (n == qb)
                ptb, pk = PT[:, b], ("PT", b)
                ok = ("pO", idx % 2)
                for qt in range(2):
                    o = pO[idx % 2][:, qt, :]
                    khs = [0] if (own and qt == 0) else [0, 1]
                    for kh in khs:
                        MM(S, o, ptb[:, kh, qt * 128:(qt + 1) * 128], Vaug[:, n * 2 + kh, h, :], start=(kh == khs[0]),
                           stop=(kh == khs[-1]), r=[pk, "Vaug", "vones"], w=[ok])
                for qt in range(2):
                    o = pO[idx % 2][:, qt, :]
                    ak = ("acc", qbuf, qt, h)
                    if it in first_items:
                        if own or qb < 4:
                            CP(S, acc[:, qbuf, qt, h, :], o, r=[ok], w=[ak])
                        else:
                            TS(S, acc[:, qbuf, qt, h, :], o, sel[:, qbuf, qt, h, n:n + 1], None, ALU.mult,
                               r=[ok, ("sel", qbuf)], w=[ak])
                    elif own or qb < 4:
                        TT(S, acc[:, qbuf, qt, h, :], o, acc[:, qbuf, qt, h, :], ALU.add, r=[ok, ak], w=[ak])
                    else:
                        STT(S, acc[:, qbuf, qt, h, :], o, sel[:, qbuf, qt, h, n:n + 1], acc[:, qbuf, qt, h, :], ALU.mult, ALU.add,
                            r=[ok, ak, ("sel", qbuf)], w=[ak])

            def tail_norm(qb, qt):
                ab = qb % 2
                aks = [("acc", ab, qt, h) for h in range(8)]
                S.op("dve", lambda e: e.reciprocal(out=rec[:, qt, :], in_=acc[:, ab, qt, :, 64]), aks, [("rec", qt)])
                TT(S, att[:, qt, :].rearrange("p (h d) -> p h d", d=64), acc[:, ab, qt, :, 0:64],
                   rec[:, qt, :].unsqueeze(2).to_broadcast([128, 8, 64]), ALU.mult, r=aks + [("rec", qt)], w=[("att", qt)])
                for ck in range(4):
                    TR(S, pT[:, qt, ck, :], att[:, qt, ck * 128:(ck + 1) * 128], ident[:], r=[("att", qt), "ident"], w=["pT"])
                CP(S, attT[:, ab, :, qt * 128:(qt + 1) * 128], pT[:, qt], r=["pT"], w=[("attT", ab, qt)])

            def tail_out(qb, tt, dh):
                ab = qb % 2
                tok0 = qb * 256 + tt * 128
                if dh == 0:
                    S.dma("sp", xr[:, tt, :], x_in[tok0:tok0 + 128, :], w=[("xr", tt)])
                pw = pW[dh]
                for kc in range(8):
                    if kc < 2:
                        lhs, rk_ = ysT[:, kc, tok0:tok0 + 128], "ysT"
                    elif kc < 4:
                        lhs, rk_ = ypT[:, kc - 2, tok0:tok0 + 128], ("ypT", kc - 2)
                    else:
                        lhs, rk_ = attT[:, ab, kc - 4, tt * 128:(tt + 1) * 128], ("attT", ab, tt)
                    MM(S, pw[:], lhs, wout[:, kc, dh * 512:(dh + 1) * 512], start=(kc == 0), stop=(kc == 7),
                       r=[rk_, ("wout", kc)], w=[("pW", dh)])
                TT(S, xr[:, tt, dh * 512:(dh + 1) * 512], pw[:], xr[:, tt, dh * 512:(dh + 1) * 512], ALU.add,
                   r=[("pW", dh), ("xr", tt)], w=[("xr", tt)])
                if dh == 1:
                    S.dma("sp", x_out[tok0:tok0 + 128, :], xr[:, tt, :], r=[("xr", tt)])

            def tail_pieces(qb):
                return ([lambda qt=qt: tail_norm(qb, qt) for qt in range(2)]
                        + [lambda tt=tt, dh=dh: tail_out(qb, tt, dh) for tt in range(2) for dh in range(2)])

            LA = 2
            items = []
            for qb in range(nqb):
                base = [(qb, n, h) for n in range(qb) for h in range(8)]
                for k in range(8):
                    base.insert(min(len(base), (2 * k + 1) * qb // 2 + k), (qb, qb, k))
                items += base
            first_items = set()
            seen_heads = set()
            for it_ in items:
                if (it_[0], it_[2]) not in seen_heads:
                    seen_heads.add((it_[0], it_[2]))
                    first_items.add(it_)
            sched_at = {}
            emit_qload(0)
            emit_head(0)
            for j in range(LA):
                emit_S(items[j], j)
            for i, it in enumerate(items):
                j = i + LA
                if j < len(items):
                    nx = items[j]
                    if nx[0] != items[j - 1][0]:
                        emit_head(nx[0])
                    emit_S(nx, j)
                emit_PV(it, i)
                for fn in sched_at.pop(i, ()):
                    fn()
                if i + 1 == len(items):
                    for fn in tail_pieces(it[0]):
                        fn()
                elif items[i + 1][0] != it[0]:
                    n_next = (it[0] + 2) * 8
                    pcs = tail_pieces(it[0])
                    offs = [(k + 1) * n_next // (len(pcs) + 1) for k in range(len(pcs))]
                    for k, fn in enumerate(pcs):
                        sched_at.setdefault(i + 1 + offs[k], []).append(fn)
            assert not sched_at
            if "mix" in self.dbg and l == 0:
                S.dma("sp", self.dbg_out["ysT"], ysT[:], r=["ysT"])
                S.dma("sp", self.dbg_out["ypT"], ypT[:], r=[("ypT", 0), ("ypT", 1)])
                S.dma("sp", self.dbg_out["kT"], kT[:], r=["kT"])
            S.emit()

    def block_A(self, l, x_in, x_out):
        nc = self.nc
        with ExitStack() as es0:
            sb0 = lambda name, shape, dt=F32: es0.enter_context(nc.sbuf_tensor(self.nm(name), shape, dt))
            R = {}
            R["ysT"] = sb0("ysT", [128, 2, SEQ], BF16)
            R["ypT"] = sb0("ypT", [128, 2, SEQ], BF16)
            with ExitStack() as es1:
                sb1 = lambda name, shape, dt=F32: es1.enter_context(nc.sbuf_tensor(self.nm(name), shape, dt))
                R["u_sb"] = sb1("u_sb", [128, 2, SEQ], BF16)
                R["Wss"] = sb1("Wss", [128, 2, 4, 8, 2, 128], BF16)
                R["Vm"] = sb1("Vm", [128, 8, 2, 8, 128], BF16)
                R["Kmat"] = sb1("Kmat", [128, 2, 8, 128], BF16)
                R["Ak"] = sb1("Ak", [128, 9, 8, 3])
                R["chp"] = sb1("chp", [128, 2, 4])
                R["glu_sb"] = sb1("glu_sb", [128, 2, 256], BF16)
                self.block_proj(l, x_in, "up", R)
                if self.stop_after != ("A0", l):
                    self.block_ssm(l, R)
                if self.stop_after in (("AS", l), ("A0", l)):
                    S = Sched(nc)
                    S.dma("sp", self.dbg_out["ysT"], R["ysT"][:])
                    S.dma("sp", self.dbg_out["ypT"], R["ypT"][:])
                    S.dma("sp", self.dbg_out["u"], R["u_sb"][:])
                    S.dma("sp", self.dbg_out["Kmat"], R["Kmat"][:])
                    S.dma("sp", self.dbg_out["Ak"], R["Ak"][:])
                    S.emit()
                    return
            with ExitStack() as es2:
                sb2 = lambda name, shape, dt=F32: es2.enter_context(nc.sbuf_tensor(self.nm(name), shape, dt))
                R["kT"] = sb2("kT", [128, 4, SEQ], BF16)
                R["Vaug"] = sb2("Vaug", [128, 32, 8, 65], BF16)
                R["ksum"] = sb2("ksum", [128, 4, 16])
                R["kmeanT"] = sb2("kmeanT", [128, 4, 16], BF16)
                self.block_proj(l, x_in, "qkv", R)
                if self.stop_after == ("A1", l):
                    S = Sched(nc)
                    S.dma("sp", self.dbg_out["kT"], R["kT"][:])
                    S.dma("sp", self.dbg_out["Vaug"], R["Vaug"][:])
                    S.dma("sp", self.dbg_out["kmeanT"], R["kmeanT"][:])
                    S.emit()
                    return
                self.block_attn(l, x_in, x_out, R)

    def build_a2only(self, nqb):
        nc, d = self.nc, self.d
        ti = {}
        for name, shp in (("t_ysT", [128, 2, SEQ]), ("t_ypT", [128, 2, SEQ]), ("t_kT", [128, 4, SEQ]), ("t_V", [128, 32, 8, 64]),
                          ("t_kmean", [128, 4, 16]), ("t_q", [4, 128, SEQ])):
            ti[name] = nc.dram_tensor(name, shp, F32, kind="ExternalInput").ap()
        with ExitStack() as es:
            sb = lambda name, shape, dt=F32: es.enter_context(nc.sbuf_tensor(self.nm(name), shape, dt))
            R = {"ysT": sb("ysT", [128, 2, SEQ], BF16), "ypT": sb("ypT", [128, 2, SEQ], BF16), "kT": sb("kT", [128, 4, SEQ], BF16),
                 "Vaug": sb("Vaug", [128, 32, 8, 65], BF16), "ksum": sb("ksum", [128, 4, 16]), "kmeanT": sb("kmeanT", [128, 4, 16], BF16)}
            S = Sched(nc)
            S.dma("pool", R["ysT"][:], ti["t_ysT"]); S.dma("pool", R["ypT"][:], ti["t_ypT"]); S.dma("pool", R["kT"][:], ti["t_kT"])
            for kt4 in range(8):
                S.dma("pool", R["Vaug"][:, 4 * kt4:4 * kt4 + 4, :, 0:64], ti["t_V"][:, 4 * kt4:4 * kt4 + 4], w=["V"])
            S.dma("pool", R["kmeanT"][:], ti["t_kmean"])
            MSET(S, R["Vaug"][:, :, :, 64:65], 1.0, w=["vones"])
            S.dma("pool", self.qT_d, ti["t_q"])
            S.emit()
            self.block_attn(0, d["x"], self.out, R, nqb=nqb)
        return nc

    def build(self):
        d = self.d
        if isinstance(self.stop_after, tuple) and self.stop_after[0] == "A2only":
            return self.build_a2only(self.stop_after[1])
        for l in range(self.n_layers):
            x_in = d["x"] if l == 0 else self.x1
            last = (l == self.n_layers - 1)
            if self.stop_after == "Bonly":
                self.block_B(l, d["x"], self.out, False)
                return self.nc
            if self.stop_after in (("A", l), ("AS", l), ("A0", l), ("A1", l)):
                self.block_A(l, x_in, self.out)
                return self.nc
            self.block_A(l, x_in, self.xmid)
            self.block_B(l, self.xmid, self.out if last else self.x1, last and self.stop_after is None)
        return self.nc


_CONSTS = None


def _prep_inputs(inputs):
    global _CONSTS
    if _CONSTS is None:
        _CONSTS = _consts()
    inp = {k: np.asarray(v) for k, v in inputs.items()}
    lay = _host_layouts(inp)
    shared = {}
    for k in RAW_SHAPES:
        if k != "x":
            shared[k] = np.ascontiguousarray(inp[k], np.float32)
    shared.update(lay)
    shared.update(_CONSTS)
    maps = []
    for b in range(8):
        m = dict(shared)
        m["x"] = np.ascontiguousarray(inp["x"][b], np.float32)
        maps.append(m)
    return maps


def kernel(**inputs):
    maps = _prep_inputs(inputs)
    nc = Prog().build()
    res = run_bass_kernel_spmd(nc, maps, core_ids=list(range(8)))
    return np.stack([np.asarray(r["out"]) for r in res.results], axis=0).astype(np.float32)
```

```python
import math
from contextlib import ExitStack

import numpy as np
import concourse.bass as bass
import concourse.mybir as mybir
from concourse.bass_utils import run_bass_kernel_spmd

F32 = mybir.dt.float32
BF16 = mybir.dt.bfloat16
ALU = mybir.AluOpType
AF = mybir.ActivationFunctionType
AX = mybir.AxisListType

SEQ = 4096
DM = 1024
NL = 2
EPS = 1e-6
NEG = -30000.0

ENGINES = ("pe", "act", "dve", "pool", "sp")
SYNC_ALL_SAME_ENGINE = False
N_DMA_SEMS = 24


class _Op:
    __slots__ = ("eng", "fn", "deps", "is_dma", "needs_inc", "inc_idx", "dsem", "dval", "gidx")

    def __init__(self, eng, fn, is_dma, gidx):
        self.eng = eng
        self.fn = fn
        self.deps = []
        self.is_dma = is_dma
        self.needs_inc = False
        self.inc_idx = 0
        self.dsem = None
        self.dval = 0
        self.gidx = gidx


class Sched:
    uid = 0

    def __init__(self, nc):
        self.nc = nc
        self.ops = {e: [] for e in ENGINES}
        self.last_write = {}
        self.readers = {}
        self.n = 0
        self.dma_count = 0
        self.dma_last = {}

    def _add(self, eng, fn, reads, writes, is_dma):
        op = _Op(eng, fn, is_dma, self.n)
        self.n += 1
        deps = {}
        raw = set()
        for k in reads:
            w = self.last_write.get(k)
            if w is not None:
                deps[w.gidx] = w
                raw.add(w.gidx)
        for k in writes:
            w = self.last_write.get(k)
            if w is not None:
                deps[w.gidx] = w
            for r in self.readers.get(k, ()):
                deps[r.gidx] = r
        if is_dma:
            j = self.dma_count % N_DMA_SEMS
            self.dma_count += 1
            prev = self.dma_last.get(j)
            if prev is not None:
                deps[prev.gidx] = prev
                op.dval = prev.dval + 16
            else:
                op.dval = 16
            op.dsem = j
            self.dma_last[j] = op
        deps.pop(op.gidx, None)
        op.deps = [(dd, ((g in raw) or SYNC_ALL_SAME_ENGINE) and eng != "pe") for g, dd in deps.items()]
        for k in writes:
            self.last_write[k] = op
            self.readers[k] = []
        for k in reads:
            if k in writes:
                continue
            self.readers.setdefault(k, []).append(op)
        self.ops[eng].append(op)
        return op

    def op(self, eng, fn, reads=(), writes=()):
        return self._add(eng, fn, tuple(reads), tuple(writes), False)

    def dma(self, eng, out, in_, r=(), w=()):
        return self._add(eng, lambda e: e.dma_start(out=out, in_=in_), tuple(r), tuple(w), True)

    def emit(self):
        nc = self.nc
        for e in ENGINES:
            for op in self.ops[e]:
                for d, raw in op.deps:
                    if (not d.is_dma) and (d.eng != op.eng or raw):
                        d.needs_inc = True
        for e in ENGINES:
            c = 0
            for op in self.ops[e]:
                if op.needs_inc and not op.is_dma:
                    c += 1
                    op.inc_idx = c
        Sched.uid += 1
        esem = {e: nc.alloc_semaphore(name="s%d_%s" % (Sched.uid, e)) for e in ENGINES}
        dsem = [nc.alloc_semaphore(name="d%d_%d" % (Sched.uid, j)) for j in range(N_DMA_SEMS)]
        with ExitStack() as es:
            block = es.enter_context(nc.Block())
            engobj = {"pe": "tensor", "act": "scalar", "dve": "vector", "pool": "gpsimd", "sp": "sync"}

            def make(e):
                def body(eng):
                    waited = {}
                    for op in self.ops[e]:
                        for d, raw in op.deps:
                            if d.is_dma:
                                key = ("d", d.dsem)
                                val = d.dval
                                sem = dsem[d.dsem]
                            else:
                                if d.eng == e and not raw:
                                    continue
                                key = ("e", d.eng)
                                val = d.inc_idx
                                sem = esem[d.eng]
                            if waited.get(key, 0) >= val:
                                continue
                            waited[key] = val
                            eng.wait_ge(sem, val)
                        ins = op.fn(eng)
                        if op.is_dma:
                            ins.then_inc(dsem[op.dsem], 16)
                        elif op.needs_inc:
                            ins.then_inc(esem[e], 1)
                    if e == "sp":
                        for d in self.dma_last.values():
                            key = ("d", d.dsem)
                            if waited.get(key, 0) >= d.dval:
                                continue
                            waited[key] = d.dval
                            eng.wait_ge(dsem[d.dsem], d.dval)
                return body

            for e in ENGINES:
                getattr(block, engobj[e])(make(e))
        nc.clear_and_free_semaphores(list(esem.values()) + dsem)
        nc.all_engine_barrier()


def MM(S, out, lhsT, rhs, start=True, stop=True, r=(), w=()):
    S.op("pe", lambda e: e.matmul(out, lhsT, rhs, start=start, stop=stop), r, w)


def TR(S, out, in_, ident, r=(), w=()):
    S.op("pe", lambda e: e.transpose(out, in_, ident), r, w)


def ACT(S, out, in_, func, r=(), w=(), **kw):
    S.op("act", lambda e: e.activation(out=out, in_=in_, func=func, **kw), r, w)


def TT(S, out, in0, in1, op, r=(), w=(), eng="dve"):
    S.op(eng, lambda e: e.tensor_tensor(out=out, in0=in0, in1=in1, op=op), r, w)


def TS(S, out, in0, s1, s2, op0, op1=None, r=(), w=(), eng="dve"):
    if op1 is None:
        S.op(eng, lambda e: e.tensor_scalar(out=out, in0=in0, scalar1=s1, scalar2=None, op0=op0), r, w)
    else:
        S.op(eng, lambda e: e.tensor_scalar(out=out, in0=in0, scalar1=s1, scalar2=s2, op0=op0, op1=op1), r, w)


def STT(S, out, in0, scalar, in1, op0, op1, r=(), w=(), eng="dve"):
    S.op(eng, lambda e: e.scalar_tensor_tensor(out=out, in0=in0, scalar=scalar, in1=in1, op0=op0, op1=op1), r, w)


def CP(S, out, in_, r=(), w=(), eng="dve"):
    S.op(eng, lambda e: e.tensor_copy(out=out, in_=in_), r, w)


def MSET(S, ap, val, w=(), eng="pool"):
    S.op(eng, lambda e: e.memset(ap, val), (), w)


def _t5_bucket_np(rel):
    rel = np.asarray(rel, dtype=np.int64)
    n = np.maximum(rel, 0)
    max_exact = 16
    nf = np.maximum(n, 1).astype(np.float32)
    large = max_exact + (np.log(nf / np.float32(max_exact)) / np.float32(math.log(128 / max_exact))
                         * np.float32(32 - max_exact)).astype(np.int32)
    large = np.minimum(large, 31)
    return np.where(n < max_exact, n, large)


def _consts():
    c = {}
    c["ident"] = np.eye(128, dtype=np.float32)
    c["exch"] = np.eye(128, dtype=np.float32)[::-1].copy()
    rel = np.arange(512) - 127
    b = _t5_bucket_np(rel)
    oh = np.zeros((32, 512), np.float32)
    oh[b[rel >= 0], np.arange(512)[rel >= 0]] = 1.0
    ohm = np.zeros((128, 512), np.float32)
    ohm[0:32] = oh
    ohm[32, rel < 0] = NEG
    c["ohm"] = ohm
    negm = np.zeros((128, 16, 16), np.float32)
    for qb in range(16):
        negm[:, qb, qb:] = -1e30
    c["negm"] = negm
    wins = (2, 4, 8, 16)
    winv = np.zeros((128, 2), np.float32)
    invc = np.zeros((128, 2, 16), np.float32)
    t = np.arange(16)
    for ct in range(2):
        for half in range(2):
            w = wins[2 * ct + half]
            winv[64 * half:64 * half + 64, ct] = 1.0 / w
            invc[64 * half:64 * half + 64, ct, :] = 1.0 / np.minimum(t + 1, w)
    c["winv"] = winv
    c["invc"] = invc
    esel = np.zeros((32, 16, 128), np.float32)
    for e in range(16):
        esel[e, e, :] = 1.0
        esel[16 + e, e, :] = 1.0
    c["esel"] = esel
    c["neghalfpi"] = np.full((128, 1), math.pi / 2, np.float32)
    return c


CONST_SHAPES = {"ident": [128, 128], "exch": [128, 128], "ohm": [128, 512],
                "negm": [128, 16, 16], "winv": [128, 2], "invc": [128, 2, 16], "esel": [32, 16, 128],
                "neghalfpi": [128, 1]}


def _host_layouts(inp):
    o = {}
    rep = lambda v: np.ascontiguousarray(np.broadcast_to(v[:, None, :], (v.shape[0], 128, v.shape[1])), np.float32)
    o["g1b"] = rep(inp["norm1_g"])
    o["g2b"] = rep(inp["norm2_g"])
    o["gfb"] = np.ascontiguousarray(np.broadcast_to(inp["final_norm_g"][None, :], (128, DM)), np.float32)
    rbp = np.zeros((128, 128), np.float32)
    rbp[0:32, 0:8] = inp["rel_bias"]
    rbp[32, 0:8] = 1.0
    o["rb"] = rbp
    o["c31b"] = np.ascontiguousarray(np.broadcast_to(inp["rel_bias"][31][None, :], (128, 8)), np.float32)
    def sm(a):
        return np.ascontiguousarray(a.reshape(NL, 8, 2, 64).transpose(0, 2, 3, 1).reshape(NL, 128, 8), np.float32)
    ldt = np.broadcast_to(inp["ssm_log_dt"][:, :, None], (NL, 16, 64))
    o["ssmA"] = np.ascontiguousarray(np.stack([sm(inp["ssm_a_re"]), sm(inp["ssm_a_im"]), sm(ldt)], axis=-1))
    def bexp(b):
        out = np.zeros((NL, 128, 8, 128), np.float32)
        for g in range(16):
            P, two = g // 2, g % 2
            cl = (g % 8) * 16
            out[:, two * 64:(two + 1) * 64, P, cl:cl + 16] = b[:, g]
        return out
    def cexp(cm):
        out = np.zeros((NL, 128, 8, 128), np.float32)
        for g in range(16):
            P, two = g // 2, g % 2
            cl = (g % 8) * 16
            out[:, two * 64:(two + 1) * 64, P, cl:cl + 16] = cm[:, g].transpose(0, 2, 1)
        return out
    o["bexp"] = np.stack([bexp(inp["ssm_b_re"]), bexp(inp["ssm_b_im"])], axis=1)
    o["cexp"] = np.stack([cexp(inp["ssm_c_re"]), cexp(inp["ssm_c_im"])], axis=1)
    chm = lambda v: np.ascontiguousarray(v.reshape(NL, 2, 128).transpose(0, 2, 1), np.float32)
    o["chp"] = np.ascontiguousarray(np.stack([chm(inp["ssm_d"]), chm(inp["ssm_glu_b"]), chm(inp["pool_b"]),
                                              chm(inp["pool_scale"])], axis=-1))
    pw = np.zeros((NL, 2, 128, 128), np.float32)
    for ct in range(2):
        for half in range(2):
            pw[:, ct, 64 * half:64 * half + 64, 64 * half:64 * half + 64] = inp["pool_w"][:, 2 * ct + half]
    o["poolw"] = pw
    wr = np.concatenate([inp["moe_group_w"], inp["moe_router_w"].transpose(0, 2, 1, 3).reshape(NL, DM, 16)], axis=-1)
    o["wr"] = np.ascontiguousarray(wr, np.float32)
    br = np.concatenate([inp["moe_group_b"], inp["moe_router_b"].reshape(NL, 16)], axis=-1)
    o["brb"] = np.ascontiguousarray(np.broadcast_to(br[:, None, :], (NL, 128, 20)), np.float32)
    return o


LAYOUT_SHAPES = {"g1b": [NL, 128, DM], "g2b": [NL, 128, DM], "gfb": [128, DM], "rb": [128, 128], "c31b": [128, 8],
                 "ssmA": [NL, 128, 8, 3], "bexp": [NL, 2, 128, 8, 128], "cexp": [NL, 2, 128, 8, 128],
                 "chp": [NL, 128, 2, 4], "poolw": [NL, 2, 128, 128], "wr": [NL, DM, 20], "brb": [NL, 128, 20]}
RAW_SHAPES = {"x": [SEQ, DM], "w_in": [NL, DM, 2048], "ssm_glu_w": [NL, 256, 256], "w_out": [NL, DM, DM],
              "moe_w_gate": [NL, 16, DM, 256], "moe_w_up": [NL, 16, DM, 256], "moe_w_down": [NL, 16, 256, DM]}


class Prog:
    def __init__(self, n_layers=NL, stop_after=None, dbg=()):
        self.nc = nc = bass.Bass("TRN2", target_bir_lowering=False)
        self.n_layers = n_layers
        self.stop_after = stop_after
        self.dbg = set(dbg)
        self.d = {}
        for name, shp in list(RAW_SHAPES.items()) + list(LAYOUT_SHAPES.items()) + list(CONST_SHAPES.items()):
            self.d[name] = nc.dram_tensor(name, shp, F32, kind="ExternalInput").ap()
        self.out = nc.dram_tensor("out", [SEQ, DM], F32, kind="ExternalOutput").ap()
        self.xmid = nc.dram_tensor("xmid", [SEQ, DM], F32).ap()
        self.x1 = nc.dram_tensor("x1", [SEQ, DM], F32).ap()
        self.qT_d = nc.dram_tensor("qT_d", [4, 128, SEQ], BF16).ap()
        self.Fd = nc.dram_tensor("Fd", [8, 512], F32).ap()
        self.dbg_out = {}

    def nm(self, name):
        self.uid = getattr(self, "uid", 0) + 1
        return "t%d_%s" % (self.uid, name)

    def dbg_tensor(self, name, shape, dt=F32):
        t = self.nc.dram_tensor("dbg_" + name, shape, dt, kind="ExternalOutput").ap()
        self.dbg_out[name] = t
        return t

    def norm_tile(self, S, T, x_ap, xkey, g_tile, hT_dst, hT_key, idx):
        self.norm_tile_a(S, T, x_ap, xkey, g_tile, idx)
        self.norm_tile_b(S, T, hT_dst, hT_key, idx)

    def norm_tile_a(self, S, T, x_ap, xkey, g_tile, idx):
        b = idx % 2
        junk, ss, h = T.get("junk"), T["ss"], T["h"]
        if junk is None:
            ACT(S, h[:, b, :], x_ap, AF.Square, r=[xkey], w=[("h", b), ("ss", b)], accum_out=ss[:, b, 0:1])
        else:
            ACT(S, junk[:, 0, :], x_ap, AF.Square, r=[xkey], w=["junk", ("ss", b)], accum_out=ss[:, b, 0:1])
        TS(S, ss[:, b, 1:2], ss[:, b, 0:1], 1.0 / DM, EPS, ALU.mult, ALU.add, r=[("ss", b)], w=[("ss1", b)])
        ACT(S, ss[:, b, 2:3], ss[:, b, 1:2], AF.Sqrt, r=[("ss1", b)], w=[("ss2", b)])
        S.op("dve", lambda e: e.reciprocal(out=ss[:, b, 3:4], in_=ss[:, b, 2:3]), [("ss2", b)], [("ss3", b)])
        STT(S, h[:, b, :], x_ap, ss[:, b, 3:4], g_tile[:], ALU.mult, ALU.mult, r=[xkey, ("ss3", b), "g"], w=[("h", b)])

    def norm_tile_b(self, S, T, hT_dst, hT_key, idx):
        b = idx % 2
        h, pst = T["h"], T["pst"]
        for kc in range(8):
            TR(S, pst[:, b, kc, :], h[:, b, kc * 128:(kc + 1) * 128], T["ident_bf"][:],
               r=[("h", b), "ident_bf"], w=[("pst", b)])
        if idx % 2 == 0:
            ACT(S, hT_dst, pst[:, b, :, :], AF.Copy, r=[("pst", b)], w=[hT_key])
        else:
            CP(S, hT_dst, pst[:, b, :, :], r=[("pst", b)], w=[hT_key])

    def alloc_norm(self, es, T, junk=True):
        nc = self.nc
        if junk:
            T["junk"] = es.enter_context(nc.sbuf_tensor(self.nm("junk"), [128, 1, DM], BF16))
        T["ss"] = es.enter_context(nc.sbuf_tensor(self.nm("ss"), [128, 2, 4], F32))
        T["h"] = es.enter_context(nc.sbuf_tensor(self.nm("h"), [128, 2, DM], BF16))
        T["pst"] = es.enter_context(nc.psum_tensor(self.nm("pst"), [128, 2, 8, 128], BF16))
        T["ident_bf"] = es.enter_context(nc.sbuf_tensor(self.nm("ident_bf"), [128, 128], BF16))

    def load_ident(self, S, T):
        S.dma("pool", T["ident_bf"][:], self.d["ident"], w=["ident_bf"])

    def block_B(self, l, x_in, x_out, final):
        nc, d = self.nc, self.d
        with ExitStack() as es:
            sb = lambda name, shape, dt=F32: es.enter_context(nc.sbuf_tensor(self.nm(name), shape, dt))
            T = {}
            self.alloc_norm(es, T)
            wd = sb("wd", [128, 32, DM], BF16)
            actall = sb("actall", [128, 32, 1024], BF16)
            h2T = sb("h2T", [128, 8, 1024], BF16)
            wg = sb("wg", [128, 2, 8, 256], BF16)
            wu = sb("wu", [128, 2, 8, 256], BF16)
            g2 = sb("g2", [128, DM])
            gf = sb("gf", [128, DM]) if final else None
            xt = sb("xt", [128, 2, DM])
            wr = sb("wr", [128, 8, 20], BF16)
            brb = sb("brb", [128, 20])
            esel = sb("esel", [32, 16, 128], BF16)
            combT = sb("combT", [32, 1024], BF16)
            rt = sb("rt", [128, 2, 96])
            hl = sb("hl", [128, 2, 32], BF16)
            cb = sb("cb", [128, 2, 512])
            sg = sb("sg", [128, 2, 512])
            fin = sb("fin", [128, 2, 4]) if final else None
            psA = [es.enter_context(nc.psum_tensor(self.nm("psA"), [128, 512], F32)) for i in range(2)]
            psB = [es.enter_context(nc.psum_tensor(self.nm("psB"), [128, 512], F32)) for i in range(2)]
            psC = es.enter_context(nc.psum_tensor(self.nm("psC"), [128, 512], F32))
            psL = es.enter_context(nc.psum_tensor(self.nm("psL"), [128, 512], F32))
            S = Sched(nc)
            self.load_ident(S, T)
            S.dma("sp", g2[:], d["g2b"][l], w=["g"])
            if final:
                S.dma("sp", gf[:], d["gfb"], w=["gf"])
            S.dma("sp", brb[:], d["brb"][l], w=["brb"])
            S.dma("pool", wr[:], d["wr"][l].rearrange("(kc p) n -> p kc n", p=128), w=["wr"])
            S.dma("pool", esel[:], d["esel"], w=["esel"])
            wdv = d["moe_w_down"][l].rearrange("e (ft p) d -> p e ft d", p=128)
            wd4 = wd[:].rearrange("p (e ft) d -> p e ft d", ft=2)

            def load_wd():
                for e4 in range(8):
                    S.dma("pool", wd4[:, 2 * e4:2 * e4 + 2], wdv[:, 2 * e4:2 * e4 + 2], w=[("wd", e4)])

            def load_w(e):
                b = e % 2
                S.dma("pool", wg[:, b], d["moe_w_gate"][l, e].rearrange("(kc p) f -> p kc f", p=128), w=[("wg", b)])
                S.dma("pool", wu[:, b], d["moe_w_up"][l, e].rearrange("(kc p) f -> p kc f", p=128), w=[("wu", b)])

            xtR = sb("xtR", [128, 2, DM])

            def R_a(sc, tt):
                b = tt % 2
                tok0 = sc * 1024 + tt * 128
                if tt == 0:
                    S.dma("sp", xtR[:, 0, :], x_in[tok0:tok0 + 128, :], w=[("xtR", 0)])
                if tt + 1 < 8:
                    S.dma("sp", xtR[:, 1 - b, :], x_in[tok0 + 128:tok0 + 256, :], w=[("xtR", 1 - b)])
                self.norm_tile_a(S, T, xtR[:, b, :], ("xtR", b), g2, tt)

            def R_b(sc, tt):
                b = tt % 2
                tok0 = sc * 1024 + tt * 128
                self.norm_tile_b(S, T, h2T[:, :, tt * 128:(tt + 1) * 128], ("h2T", tt), tt)
                lg = psL[:, b * 64:b * 64 + 20]
                for kc in range(8):
                    MM(S, lg, h2T[:, kc, tt * 128:(tt + 1) * 128], wr[:, kc, :], start=(kc == 0), stop=(kc == 7),
                       r=[("h2T", tt), "wr"], w=["psL"])
                R = rt[:, b, :]
                rk = ("rt", b)
                lgs = R[:, 0:20]
                TT(S, lgs, lg, brb[:], ALU.add, r=["psL", "brb"], w=[rk])
                S.op("dve", lambda e, R=R: e.reduce_max(out=R[:, 20:21], in_=R[:, 0:4], axis=AX.X), [rk], [rk])
                TS(S, R[:, 21:22], R[:, 20:21], -1.0, None, ALU.mult, r=[rk], w=[rk])
                ACT(S, R[:, 24:28], R[:, 0:4], AF.Exp, r=[rk], w=[rk], bias=R[:, 21:22], accum_out=R[:, 22:23])
                S.op("dve", lambda e, R=R: e.reciprocal(out=R[:, 23:24], in_=R[:, 22:23]), [rk], [rk])
                TS(S, R[:, 28:32], R[:, 0:4], R[:, 20:21], None, ALU.is_ge, r=[rk], w=[rk])
                TS(S, R[:, 28:32], R[:, 28:32], -1.0, 1e30, ALU.add, ALU.mult, r=[rk], w=[rk])
                em = R[:, 32:48]
                TT(S, em.rearrange("p (g e) -> p g e", e=4), R[:, 4:20].rearrange("p (g e) -> p g e", e=4),
                   R[:, 28:32].unsqueeze(2).to_broadcast([128, 4, 4]), ALU.add, r=[rk], w=[rk])
                S.op("dve", lambda e, R=R: e.max(out=R[:, 48:56], in_=R[:, 32:48]), [rk], [rk])
                TS(S, R[:, 56:57], R[:, 48:49], -1.0, None, ALU.mult, r=[rk], w=[rk])
                ACT(S, R[:, 64:80], em, AF.Exp, r=[rk], w=[rk], bias=R[:, 56:57])
                ACT(S, R[:, 57:58], R[:, 49:50], AF.Exp, r=[rk], w=[rk], bias=R[:, 56:57])
                TS(S, R[:, 57:58], R[:, 57:58], 1.0, None, ALU.add, r=[rk], w=[rk])
                S.op("dve", lambda e, R=R: e.reciprocal(out=R[:, 58:59], in_=R[:, 57:58]), [rk], [rk])
                TT(S, R[:, 59:60], R[:, 58:59], R[:, 23:24], ALU.mult, r=[rk], w=[rk])
                TS(S, R[:, 80:96], em, R[:, 49:50], None, ALU.is_ge, r=[rk], w=[rk])
                STT(S, R[:, 64:80], R[:, 64:80], R[:, 59:60], R[:, 80:96], ALU.mult, ALU.mult, r=[rk], w=[rk])
                CP(S, hl[:, b, 0:16], R[:, 64:80], r=[rk], w=[("hl", b)])
                TT(S, hl[:, b, 16:32], R[:, 64:80], hl[:, b, 0:16], ALU.subtract, r=[rk, ("hl", b)], w=[("hl", b)])
                if "comb" in self.dbg and l == 0:
                    S.dma("sp", self.dbg_out["comb"][tok0:tok0 + 128, :], R[:, 64:80], r=[rk])

            def R_c(sc, tt):
                b = tt % 2
                hlT = T["pst"][0:32, b, 0, :]
                TR(S, hlT, hl[:, b, :], T["ident_bf"][:], r=[("hl", b), "ident_bf"], w=[("pst", b)])
                CP(S, combT[:, tt * 128:(tt + 1) * 128], hlT, r=[("pst", b)], w=[("combT", tt // 4)])

            def D(sc, tt):
                b = tt % 2
                half = tt // 4
                tok0 = sc * 1024 + tt * 128
                if tt == 0:
                    S.dma("sp", xt[:, 0, :], x_in[tok0:tok0 + 128, :], w=[("xt", 0)])
                if tt + 1 < 8:
                    S.dma("sp", xt[:, 1 - b, :], x_in[tok0 + 128:tok0 + 256, :], w=[("xt", 1 - b)])
                for dh in range(2):
                    ps = psA[dh] if b == 0 else psB[dh]
                    pk = ("psA", dh) if b == 0 else ("psB", dh)
                    for ft in range(32):
                        MM(S, ps[:], actall[:, ft, tt * 128:(tt + 1) * 128], wd[:, ft, dh * 512:(dh + 1) * 512],
                           start=(ft == 0), stop=(ft == 31), r=[("actall", half), ("wd", ft // 4)], w=[pk])
                    TT(S, xt[:, b, dh * 512:(dh + 1) * 512], ps[:], xt[:, b, dh * 512:(dh + 1) * 512], ALU.add,
                       r=[pk, ("xt", b)], w=[("xt", b)])
                if final:
                    ACT(S, T["junk"][:, 0, :], xt[:, b, :], AF.Square, r=[("xt", b)], w=["junk", ("fin", b)],
                        accum_out=fin[:, b, 0:1])
                    TS(S, fin[:, b, 1:2], fin[:, b, 0:1], 1.0 / DM, EPS, ALU.mult, ALU.add, r=[("fin", b)], w=[("fin1", b)])
                    ACT(S, fin[:, b, 2:3], fin[:, b, 1:2], AF.Sqrt, r=[("fin1", b)], w=[("fin2", b)])
                    S.op("dve", lambda e, b=b: e.reciprocal(out=fin[:, b, 3:4], in_=fin[:, b, 2:3]), [("fin2", b)], [("fin3", b)])
                    STT(S, xt[:, b, :], xt[:, b, :], fin[:, b, 3:4], gf[:], ALU.mult, ALU.mult,
                        r=[("xt", b), ("fin3", b), "gf"], w=[("xt", b)])
                S.dma("sp", x_out[tok0:tok0 + 128, :], xt[:, b, :], r=[("xt", b)])

            def E(sc):
                for e in range(16):
                    wb = e % 2
                    if e + 1 < 16:
                        load_w(e + 1)
                    elif sc + 1 < 4:
                        load_w(0)
                    for half in range(2):
                        hk = [("h2T", 4 * half + i) for i in range(4)]
                        MM(S, psC[:], esel[:, e, :], combT[:, half * 512:(half + 1) * 512],
                           r=["esel", ("combT", half)], w=["psC"])
                        ACT(S, cb[:, half, :], psC[:], AF.Copy, r=["psC"], w=[("cb", half)])
                        for ft in range(2):
                            pb = (half * 2 + ft) % 2
                            for kc in range(8):
                                MM(S, psA[pb][:], wg[:, wb, kc, ft * 128:(ft + 1) * 128], h2T[:, kc, half * 512:(half + 1) * 512],
                                   start=(kc == 0), stop=(kc == 7), r=hk + [("wg", wb)], w=[("psA", pb)])
                            for kc in range(8):
                                MM(S, psB[pb][:], wu[:, wb, kc, ft * 128:(ft + 1) * 128], h2T[:, kc, half * 512:(half + 1) * 512],
                                   start=(kc == 0), stop=(kc == 7), r=hk + [("wu", wb)], w=[("psB", pb)])
                            ACT(S, sg[:, pb, :], psA[pb][:], AF.Silu, r=[("psA", pb)], w=[("sg", pb)])
                            TT(S, sg[:, pb, :], psB[pb][:], sg[:, pb, :], ALU.mult, r=[("psB", pb), ("sg", pb)], w=[("sg", pb)])
                            TT(S, actall[:, 2 * e + ft, half * 512:(half + 1) * 512], sg[:, pb, :], cb[:, half, :], ALU.mult,
                               r=[("sg", pb), ("cb", half)], w=[("actall", half)], eng="pool")

            R_a(0, 0)
            R_a(0, 1)
            for tt in range(8):
                R_b(0, tt)
                if tt + 2 < 8:
                    R_a(0, tt + 2)
                R_c(0, tt)
            load_w(0)
            load_wd()
            for sc in range(4):
                E(sc)
                nxt = sc + 1 < 4
                if nxt:
                    R_a(sc + 1, 0)
                    R_b(sc + 1, 0)
                    R_a(sc + 1, 1)
                for tt in range(8):
                    D(sc, tt)
                    if nxt:
                        R_c(sc + 1, tt)
                        if tt + 1 < 8:
                            R_b(sc + 1, tt + 1)
                        if tt + 2 < 8:
                            R_a(sc + 1, tt + 2)
            S.emit()


    def cmul(self, S, o_re, o_im, a_re, a_im, b_re, b_im, t1, t2, r, w, tk):
        TT(S, t1, a_re, b_re, ALU.mult, r=r, w=[tk + "1"])
        TT(S, t2, a_im, b_im, ALU.mult, r=r, w=[tk + "2"])
        TT(S, o_re, t1, t2, ALU.subtract, r=[tk + "1", tk + "2"], w=w)
        TT(S, t1, a_re, b_im, ALU.mult, r=r, w=[tk + "1"])
        TT(S, t2, a_im, b_re, ALU.mult, r=r, w=[tk + "2"])
        TT(S, o_im, t1, t2, ALU.add, r=[tk + "1", tk + "2"], w=w)

    def block_proj(self, l, x_in, mode, R):
        nc, d = self.nc, self.d
        ncols = 512 if mode == "up" else 1536
        c0 = 0 if mode == "up" else 512
        with ExitStack() as es:
            sb = lambda name, shape, dt=F32: es.enter_context(nc.sbuf_tensor(self.nm(name), shape, dt))
            ps = lambda name, shape, dt=F32: es.enter_context(nc.psum_tensor(self.nm(name), shape, dt))
            T = {}
            self.alloc_norm(es, T, junk=False)
            win = sb("win", [128, 8, ncols], BF16)
            g1 = sb("g1", [128, DM])
            NXB = 3
            xt = sb("xt", [128, NXB, DM])
            hT = sb("hT", [128, 2, 8, 512], BF16)
            psP = [ps("psP", [128, 512]) for _ in range(2)]
            S = Sched(nc)
            self.load_ident(S, T)
            S.dma("sp", g1[:], d["g1b"][l], w=["g"])
            wv = d["w_in"][l].rearrange("(kc p) n -> p kc n", p=128)
            for kc in range(8):
                S.dma("pool", win[:, kc, :], wv[:, kc, c0:c0 + ncols], w=[("win", kc)])
            wkeys = [("win", kc) for kc in range(8)]
            if mode == "up":
                pbuf = sb("pbuf", [128, 2, 2, 528])
                sA = sb("sA", [128, 2, 528])
                sB = sb("sB", [128, 2, 528])
                dlt = sb("dlt", [128, 2, 512], BF16)
                tfix = sb("tfix", [128, 16])
                poolw = sb("poolw", [128, 2, 128], BF16)
                winv = sb("winv", [128, 2])
                invc = sb("invc", [128, 2, 16])
                psQ = ps("psQ", [128, 2, 512])
                S.dma("pool", poolw[:], d["poolw"][l].rearrange("c k m -> k c m"), w=["poolw"])
                S.dma("sp", winv[:], d["winv"], w=["winv"])
                S.dma("sp", invc[:], d["invc"], w=["invc"])
                S.dma("sp", R["chp"][:], d["chp"][l], w=["chp"])
                MSET(S, pbuf[:, 0, :, 0:16], 0.0, w=[("pbuf", 0)])
                self.ssm_setup(S, l, es, R, T)
            else:
                qst = sb("qst", [128, 2, 4, 512], BF16)
                MSET(S, R["Vaug"][:, :, :, 64:65], 1.0, w=["vones"])
                MSET(S, R["ksum"][:], 0.0, w=["ksum"])
            def load_x(Tg):
                S.dma("sp", xt[:, Tg % NXB, :], x_in[Tg * 128:(Tg + 1) * 128, :], w=[("xt", Tg % NXB)])

            def norm_a(Tg):
                self.norm_tile_a(S, T, xt[:, Tg % NXB, :], ("xt", Tg % NXB), g1, Tg)

            for Tg in range(NXB):
                load_x(Tg)
            norm_a(0)
            norm_a(1)
            for c in range(8):
                hb = c % 2
                for tt in range(4):
                    Tg = c * 4 + tt
                    self.norm_tile_b(S, T, hT[:, hb, :, tt * 128:(tt + 1) * 128], ("hT", hb, tt), Tg)
                    if Tg + NXB < 32:
                        load_x(Tg + NXB)
                    if Tg + 2 < 32:
                        norm_a(Tg + 2)
                    if mode == "up" and tt == 2 and c > 0:
                        self.pool_chunk_back(S, l, c - 1, 1 - hb, pbuf, sA, sB, dlt, tfix, poolw, winv, invc, psQ, R)
                hk = [("hT", hb, tt) for tt in range(4)]
                nmt = ncols // 128 if mode == "up" else 8
                for mt in range(nmt):
                    pb = mt % 2
                    for kc in range(8):
                        MM(S, psP[pb][:], win[:, kc, mt * 128:(mt + 1) * 128], hT[:, hb, kc, :], start=(kc == 0),
                           stop=(kc == 7), r=hk + wkeys, w=[("psP", pb)])
                    if mode == "up":
                        if mt < 2:
                            ACT(S, R["u_sb"][:, mt, :].rearrange("p (t s) -> p t s", t=8)[:, :, c * 64:(c + 1) * 64],
                                psP[pb][:].rearrange("p (s t) -> p t s", t=8), AF.Copy, r=[("psP", pb)], w=[("u", mt)])
                        else:
                            ACT(S, pbuf[:, hb, mt - 2, 16:528], psP[pb][:], AF.Copy, r=[("psP", pb)], w=[("pbuf", hb)])
                    else:
                        if mt < 4:
                            ACT(S, qst[:, hb, mt, :], psP[pb][:], AF.Copy, r=[("psP", pb)], w=[("qst", hb)], scale=0.125)
                        else:
                            hp = mt - 4
                            for bl in range(2):
                                blk = 2 * c + bl
                                ACT(S, R["kT"][:, hp, blk * 256:(blk + 1) * 256], psP[pb][:, bl * 256:(bl + 1) * 256], AF.Copy,
                                    r=[("psP", pb)], w=["kT", "ksum"], accum_out=R["ksum"][:, hp, blk:blk + 1])
                if mode == "up":
                    self.pool_chunk_front(S, l, c, hb, pbuf, sA, sB)
                    if c == 7:
                        self.pool_chunk_back(S, l, c, hb, pbuf, sA, sB, dlt, tfix, poolw, winv, invc, psQ, R)
                else:
                    S.dma("sp", self.qT_d[:, :, c * 512:(c + 1) * 512].rearrange("hp p t -> p hp t"), qst[:, hb], r=[("qst", hb)])
                    for tt in range(4):
                        pb = tt % 2
                        for kc in range(8):
                            MM(S, psP[pb][:], hT[:, hb, kc, tt * 128:(tt + 1) * 128], win[:, kc, 1024:1536], start=(kc == 0),
                               stop=(kc == 7), r=hk + wkeys, w=[("psP", pb)])
                        CP(S, R["Vaug"][:, c * 4 + tt, :, 0:64], psP[pb][:].rearrange("p (h d) -> p h d", d=64),
                           r=[("psP", pb)], w=["Vaug"])
            if mode == "qkv":
                TS(S, R["kmeanT"][:], R["ksum"][:], 1.0 / 256.0, None, ALU.mult, r=["ksum"], w=["kmeanT"])
            S.emit()

    def pool_chunk_front(self, S, l, c, hb, pbuf, sA, sB):
        pk = ("pbuf", hb)
        if c + 1 < 8:
            CP(S, pbuf[:, 1 - hb, :, 0:16], pbuf[:, hb, :, 512:528], r=[pk], w=[("pbuf", 1 - hb)], eng="pool")
        add = lambda o, a, b_, r, w: TT(S, o, a, b_, ALU.add, r=r, w=w, eng="pool")
        for ct in range(2):
            p = pbuf[:, hb, ct, :]
            a_, b_ = sA[:, ct, :], sB[:, ct, :]
            ka, kb = ("sA", ct), ("sB", ct)
            add(a_[:, 1:528], p[:, 1:528], p[:, 0:527], [pk], [ka])
            if ct == 0:
                add(b_[64:128, 3:528], a_[64:128, 3:528], a_[64:128, 1:526], [ka], [kb])
            else:
                add(b_[:, 3:528], a_[:, 3:528], a_[:, 1:526], [ka], [kb])
                add(a_[:, 7:528], b_[:, 7:528], b_[:, 3:524], [kb], [ka])
                add(b_[64:128, 15:528], a_[64:128, 15:528], a_[64:128, 7:520], [ka], [kb])

    def pool_chunk_back(self, S, l, c, hb, pbuf, sA, sB, dlt, tfix, poolw, winv, invc, psQ, R):
        pk = ("pbuf", hb)
        for ct in range(2):
            p = pbuf[:, hb, ct, :]
            for half, src, sk in ((0, sA[:, ct, :], ("sA", ct)), (1, sB[:, ct, :], ("sB", ct))):
                rows = slice(64 * half, 64 * half + 64)
                STT(S, dlt[rows, ct, :], src[rows, 16:528], winv[rows, ct:ct + 1], p[rows, 16:528], ALU.mult, ALU.subtract,
                    r=[sk, pk, "winv"], w=[("dlt", ct)])
                if c == 0:
                    TT(S, tfix[rows, :], src[rows, 16:32], invc[rows, ct, :], ALU.mult, r=[sk, "invc"], w=["tfix"])
                    TT(S, dlt[rows, ct, 0:16], tfix[rows, :], p[rows, 16:32], ALU.subtract, r=["tfix", pk], w=[("dlt", ct)])
            MM(S, psQ[:, ct, :], poolw[:, ct, :], dlt[:, ct, :], r=["poolw", ("dlt", ct)], w=[("psQ", ct)])
        for ct in range(2):
            TS(S, R["ypT"][:, ct, c * 512:(c + 1) * 512], psQ[:, ct, :], R["chp"][:, ct, 2:3], R["chp"][:, ct, 3:4], ALU.add, ALU.mult,
               r=[("psQ", ct), "chp"], w=[("ypT", ct)])

    def ssm_setup(self, S, l, es, R, T):
        nc, d = self.nc, self.d
        sb = lambda name, shape, dt=F32: es.enter_context(nc.sbuf_tensor(self.nm(name), shape, dt))
        ps = lambda name, shape, dt=F32: es.enter_context(nc.psum_tensor(self.nm(name), shape, dt))
        sa = sb("sa", [128, 8, 3])
        sm = sb("sm", [128, 24, 8])
        Lre = sb("Lre", [128, 9, 8]); Lim = sb("Lim", [128, 9, 8])
        Fre = sb("Fre", [128, 8, 8]); Fim = sb("Fim", [128, 8, 8])
        tA = sb("tA", [128, 8, 8]); tB = sb("tB", [128, 8, 8])
        Bre = sb("Bre", [128, 8, 128]); Bim = sb("Bim", [128, 8, 128])
        Cre = sb("Cre", [128, 8, 128]); Cim = sb("Cim", [128, 8, 128]); nCim = sb("nCim", [128, 8, 128])
        scr = R["ysT"][:].bitcast(F32)
        carve = lambda i: scr[:, i // 2, (i % 2) * 1024:(i % 2) * 1024 + 1024].rearrange("p (a b) -> p a b", b=128)
        LBre, LBim, t1, t2 = carve(0), carve(1), carve(2), carve(3)
        LBb = sb("LBb", [128, 2, 8, 128], BF16)
        hpi = sb("hpi", [128, 1])
        psW = ps("psW", [128, 8, 128], BF16)
        psK = ps("psK", [128, 512])
        glu = R["glu_sb"]
        S.dma("pool", glu[:], d["ssm_glu_w"][l].rearrange("(kc p) n -> p kc n", p=128), w=["glu"])
        S.dma("sp", sa[:], d["ssmA"][l], w=["sa"])
        S.dma("sp", hpi[:], d["neghalfpi"], w=["hpi"])
        S.dma("sp", Bre[:], d["bexp"][l, 0], w=["Bre"])
        S.dma("sp", Bim[:], d["bexp"][l, 1], w=["Bim"])
        S.dma("sp", Cre[:], d["cexp"][l, 0], w=["Cre"])
        S.dma("sp", Cim[:], d["cexp"][l, 1], w=["Cim"])
        TS(S, nCim[:], Cim[:], -1.0, None, ALU.mult, r=["Cim"], w=["nCim"])
        a_re, a_im, ldt = sa[:, :, 0], sa[:, :, 1], sa[:, :, 2]
        k = lambda i: ("sm", i)
        sl = lambda i: sm[:, i, :]
        ACT(S, sl(0), ldt, AF.Exp, r=["sa"], w=[k(0)])
        TT(S, sl(1), a_re, sl(0), ALU.mult, r=["sa", k(0)], w=[k(1)])
        TT(S, sl(2), a_im, sl(0), ALU.mult, r=["sa", k(0)], w=[k(2)])
        ACT(S, sl(3), sl(1), AF.Exp, r=[k(1)], w=[k(3)], scale=1.0 / 32)
        ACT(S, sl(4), sl(2), AF.Sin, r=[k(2)], w=[k(4)], scale=1.0 / 32)
        ACT(S, sl(5), sl(2), AF.Sin, r=[k(2), "hpi"], w=[k(5)], scale=1.0 / 32, bias=hpi[:, 0:1])
        TT(S, sl(6), sl(3), sl(5), ALU.mult, r=[k(3), k(5)], w=[k(6)])
        TT(S, sl(7), sl(3), sl(4), ALU.mult, r=[k(3), k(4)], w=[k(7)])
        for _ in range(5):
            TT(S, sl(8), sl(6), sl(6), ALU.mult, r=[k(6)], w=[k(8)])
            TT(S, sl(9), sl(7), sl(7), ALU.mult, r=[k(7)], w=[k(9)])
            TT(S, sl(10), sl(6), sl(7), ALU.mult, r=[k(6), k(7)], w=[k(10)])
            TT(S, sl(6), sl(8), sl(9), ALU.subtract, r=[k(8), k(9)], w=[k(6)])
            TS(S, sl(7), sl(10), 2.0, None, ALU.mult, r=[k(10)], w=[k(7)])
        MSET(S, Lre[:, 0, :], 1.0, w=[("L", 0)], eng="dve")
        MSET(S, Lim[:, 0, :], 0.0, w=[("L", 0)], eng="dve")
        CP(S, Lre[:, 1, :], sl(6), r=[k(6)], w=[("L", 1)])
        CP(S, Lim[:, 1, :], sl(7), r=[k(7)], w=[("L", 1)])
        for j in range(1, 8):
            self.cmul(S, Lre[:, j + 1, :], Lim[:, j + 1, :], Lre[:, j, :], Lim[:, j, :], sl(6), sl(7), sl(8), sl(9),
                      r=[("L", j), k(6), k(7)], w=[("L", j + 1)], tk="smt")
        Ak = R["Ak"]
        CP(S, Ak[:, 0, :, 0], Lre[:, 8, :], r=[("L", 8)], w=[("Ak", 0)])
        CP(S, Ak[:, 0, :, 1], Lim[:, 8, :], r=[("L", 8)], w=[("Ak", 0)])
        for kk in range(8):
            self.cmul(S, Ak[:, kk + 1, :, 0], Ak[:, kk + 1, :, 1], Ak[:, kk, :, 0], Ak[:, kk, :, 1], Ak[:, kk, :, 0], Ak[:, kk, :, 1],
                      sl(8), sl(9), r=[("Ak", kk)], w=[("Ak", kk + 1)], tk="smt")
        for kk in range(9):
            TS(S, Ak[:, kk, :, 2], Ak[:, kk, :, 1], -1.0, None, ALU.mult, r=[("Ak", kk)], w=[("Akn", kk)])
        TT(S, sl(11), a_re, a_re, ALU.mult, r=["sa"], w=[k(11)])
        TT(S, sl(12), a_im, a_im, ALU.mult, r=["sa"], w=[k(12)])
        TT(S, sl(11), sl(11), sl(12), ALU.add, r=[k(11), k(12)], w=[k(11)])
        S.op("dve", lambda e: e.reciprocal(out=sm[:, 12, :], in_=sm[:, 11, :]), [k(11)], [k(12)])
        TS(S, sl(13), sl(6), -1.0, None, ALU.add, r=[k(6)], w=[k(13)])
        TT(S, sl(14), sl(13), a_re, ALU.mult, r=[k(13), "sa"], w=[k(14)])
        TT(S, sl(15), sl(7), a_im, ALU.mult, r=[k(7), "sa"], w=[k(15)])
        TT(S, sl(14), sl(14), sl(15), ALU.add, r=[k(14), k(15)], w=[k(14)])
        TT(S, sl(16), sl(14), sl(12), ALU.mult, r=[k(14), k(12)], w=[k(16)])
        TT(S, sl(14), sl(7), a_re, ALU.mult, r=[k(7), "sa"], w=[k(14)])
        TT(S, sl(15), sl(13), a_im, ALU.mult, r=[k(13), "sa"], w=[k(15)])
        TT(S, sl(14), sl(14), sl(15), ALU.subtract, r=[k(14), k(15)], w=[k(14)])
        TT(S, sl(17), sl(14), sl(12), ALU.mult, r=[k(14), k(12)], w=[k(17)])
        Lk = [("L", j) for j in range(9)]
        bc = lambda ap: ap.unsqueeze(1).to_broadcast([128, 8, 8])
        self.cmul(S, Fre[:], Fim[:], Lre[:, 0:8, :], Lim[:, 0:8, :], bc(sl(16)), bc(sl(17)), tA[:], tB[:],
                  r=Lk + [k(16), k(17)], w=["F"], tk="tAB")
        bc3 = lambda ap: ap.unsqueeze(2).to_broadcast([128, 8, 128])
        Wss, Kmat, Vm = R["Wss"], R["Kmat"], R["Vm"]
        for j in range(8):
            self.cmul(S, LBre, LBim, bc3(Fre[:, j, :]), bc3(Fim[:, j, :]), Bre[:], Bim[:], t1, t2,
                      r=["F", "Bre", "Bim"], w=["LB"], tk="t12")
            CP(S, LBb[:, 0], LBre[:], r=["LB"], w=["LBb"])
            CP(S, LBb[:, 1], LBim[:], r=["LB"], w=["LBb"])
            for ri in range(2):
                for P in range(8):
                    TR(S, psW[:, P, :], LBb[:, ri, P, :], T["ident_bf"][:], r=["LBb", "ident_bf"], w=["psW"])
                ACT(S, Wss[:, :, :, 7 - j, ri, :].rearrange("p a b s -> p (a b) s"), psW[:], AF.Copy, r=["psW"], w=["Wss"])
            for ct in range(2):
                reg = psK[:, ct * 128:(ct + 1) * 128]
                for i in range(4):
                    P = ct * 4 + i
                    MM(S, reg, LBre[:, P, :], Cre[:, P, :], start=(i == 0), stop=False, r=["LB", "Cre"], w=["psK"])
                for i in range(4):
                    P = ct * 4 + i
                    MM(S, reg, LBim[:, P, :], nCim[:, P, :], start=False, stop=(i == 3), r=["LB", "nCim"], w=["psK"])
            ACT(S, Kmat[:, :, j, :], psK[:, 0:256].rearrange("p (c m) -> p c m", m=128), AF.Copy, r=["psK"], w=["Kmat"])
        for tau in range(8):
            self.cmul(S, LBre, LBim, bc3(Lre[:, tau + 1, :]), bc3(Lim[:, tau + 1, :]), Cre[:], Cim[:], t1[:], t2[:],
                      r=Lk + ["Cre", "Cim"], w=["LB"], tk="t12")
            CP(S, Vm[:, :, 0, tau, :], LBre[:], r=["LB"], w=["Vm"])
            TS(S, Vm[:, :, 1, tau, :], LBim[:], -1.0, None, ALU.mult, r=["LB"], w=["Vm"])

    def block_ssm(self, l, R):
        nc = self.nc
        with ExitStack() as es:
            sb = lambda name, shape, dt=F32: es.enter_context(nc.sbuf_tensor(self.nm(name), shape, dt))
            ps = lambda name, shape, dt=F32: es.enter_context(nc.psum_tensor(self.nm(name), shape, dt))
            Xs = sb("Xs", [128, 2, 2, 768])
            XsP = None
            Xprev = sb("Xprev", [128, 8, 2, 512], BF16)
            t1 = sb("t1s", [128, 4096])
            ygb = sb("ygb", [128, 2, 4096], BF16)
            sgl = sb("sgl", [128, 2, 512])
            psZ = [ps("psZ", [128, 512]) for _ in range(2)]
            psZP = None
            psY = [ps("psY", [128, 512]) for _ in range(2)]
            psG = [ps("psG", [128, 512]) for _ in range(2)]
            S = Sched(nc)
            u_sb, Wss, Kmat, Vm, Ak, chp, glu, ysT = (R[k_] for k_ in ("u_sb", "Wss", "Kmat", "Vm", "Ak", "chp", "glu_sb", "ysT"))
            allk = lambda nm_: [(nm_, pp_, ri_) for pp_ in range(2) for ri_ in range(2)]
            MSET(S, Xs[:, :, :, 0:256], 0.0, w=allk("Xs"))
            MSET(S, Xprev[:, :, :, 0:1], 0.0, w=["Xprev"])
            u4 = [u_sb[:, ct, :].rearrange("p (t s) -> p t s", t=8) for ct in range(2)]

            def scan_chain(P, eng):
                ct, pi = P // 4, P % 4
                X, nm_, zb = (Xs, "Xs", psZ) if eng == "dve" else (XsP, "XsP", psZP)
                zn = "psZ" if eng == "dve" else "psZP"
                for ri in range(2):
                    z = zb[ri]
                    for sg in range(8):
                        MM(S, z[:], Wss[:, ct, pi, sg, ri, :], u4[ct][:, sg, :], start=(sg == 0), stop=(sg == 7),
                           r=["Wss", ("u", ct)], w=[(zn, ri)])
                    if eng == "dve":
                        CP(S, X[:, 0, ri, 256:768], z[:], r=[(zn, ri)], w=[(nm_, 0, ri)])
                    else:
                        ACT(S, X[:, 0, ri, 256:768], z[:], AF.Copy, r=[(zn, ri)], w=[(nm_, 0, ri)])
                pp = 0
                for kk in range(9):
                    o = 1 << kk
                    src, dst = X[:, pp], X[:, 1 - pp]
                    sre, sim, dre, dim = (nm_, pp, 0), (nm_, pp, 1), (nm_, 1 - pp, 0), (nm_, 1 - pp, 1)
                    are, aim, naim = Ak[:, kk, P:P + 1, 0], Ak[:, kk, P:P + 1, 1], Ak[:, kk, P:P + 1, 2]
                    sh = slice(256 - o, 768 - o)
                    STT(S, dst[:, 0, 256:768], src[:, 0, sh], are, src[:, 0, 256:768], ALU.mult, ALU.add, r=[sre], w=[dre], eng=eng)
                    STT(S, dst[:, 1, 256:768], src[:, 1, sh], are, src[:, 1, 256:768], ALU.mult, ALU.add, r=[sim], w=[dim], eng=eng)
                    STT(S, dst[:, 0, 256:768], src[:, 1, sh], naim, dst[:, 0, 256:768], ALU.mult, ALU.add, r=[sim, dre], w=[dre], eng=eng)
                    STT(S, dst[:, 1, 256:768], src[:, 0, sh], aim, dst[:, 1, 256:768], ALU.mult, ALU.add, r=[sre, dim], w=[dim], eng=eng)
                    pp = 1 - pp
                CP(S, Xprev[:, P, :, 1:512], X[:, pp, :, 256:767], r=[(nm_, pp, 0), (nm_, pp, 1)], w=[("Xprev", P)], eng=eng)

            for P in range(8):
                scan_chain(P, "dve")
            for ct in range(2):
                t1v = t1[:].rearrange("p (s t) -> p t s", t=8)
                for tau in range(8):
                    y = psY[tau % 2]
                    yk = ("psY", tau % 2)
                    n_mm = tau + 1 + 8
                    i = 0
                    for sg in range(tau + 1):
                        MM(S, y[:], Kmat[:, ct, tau - sg, :], u4[ct][:, sg, :], start=(i == 0), stop=(i == n_mm - 1), r=[], w=[yk])
                        i += 1
                    for pi in range(4):
                        for ri in range(2):
                            MM(S, y[:], Vm[:, ct * 4 + pi, ri, tau, :], Xprev[:, ct * 4 + pi, ri, :], start=(i == 0),
                               stop=(i == n_mm - 1), r=["Xprev", ("Xprev", ct * 4 + pi)], w=[yk])
                            i += 1
                    STT(S, t1v[:, tau, :], u4[ct][:, tau, :], chp[:, ct, 0:1], y[:], ALU.mult, ALU.add, r=[yk], w=["t1"])
                for cc in range(8):
                    ACT(S, ygb[:, ct, cc * 512:(cc + 1) * 512], t1[:, cc * 512:(cc + 1) * 512], AF.Gelu_apprx_tanh,
                        r=["t1"], w=[("ygb", ct)])
            for cc in range(8):
                for mt in range(2):
                    g = psG[mt]
                    for kc in range(2):
                        MM(S, g[:], glu[:, kc, mt * 128:(mt + 1) * 128], ygb[:, kc, cc * 512:(cc + 1) * 512], start=(kc == 0),
                           stop=(kc == 1), r=[("ygb", 0), ("ygb", 1)], w=[("psG", mt)])
                    ACT(S, sgl[:, mt, :], g[:], AF.Sigmoid, r=[("psG", mt)], w=[("sgl", mt)], bias=chp[:, mt, 1:2])
                    TT(S, ysT[:, mt, cc * 512:(cc + 1) * 512], ygb[:, mt, cc * 512:(cc + 1) * 512], sgl[:, mt, :], ALU.mult,
                       r=[("sgl", mt), ("ygb", mt)], w=["ysT"])
            S.emit()

    def block_attn(self, l, x_in, x_out, R, nqb=16):
        nc, d = self.nc, self.d
        with ExitStack() as es:
            sb = lambda name, shape, dt=F32: es.enter_context(nc.sbuf_tensor(self.nm(name), shape, dt))
            ps = lambda name, shape, dt=F32: es.enter_context(nc.psum_tensor(self.nm(name), shape, dt))
            kT, Vaug, kmeanT, ysT, ypT = (R[k_] for k_ in ("kT", "Vaug", "kmeanT", "ysT", "ypT"))
            wout = sb("wout", [128, 8, DM], BF16)
            qT = sb("qT", [128, 2, 4, 2, 256], BF16)
            G = sb("G", [128, 8, 256])
            G2 = sb("G2", [128, 8, 256])
            HK = sb("HK", [128, 8, 256])
            c31 = sb("c31", [128, 8])
            negm = sb("negm", [128, 16, 16])
            PT = sb("PT", [128, 3, 2, 256], BF16)
            tmp = sb("tmp", [128, 3, 2, 256])
            gate = sb("gate", [128, 16, 16])
            top8 = sb("top8", [128, 16, 8])
            sel = sb("sel", [128, 2, 2, 8, 16])
            acc = sb("acc", [128, 2, 2, 8, 65])
            rec = sb("rec", [128, 2, 8])
            att = sb("att", [128, 2, 512], BF16)
            attT = sb("attT", [128, 2, 4, 256], BF16)
            xr = sb("xr", [128, 2, DM])
            ident = sb("identb", [128, 128], BF16)
            exch = sb("exch", [128, 128])
            rb = sb("rb", [128, 128]); ohm = sb("ohm", [128, 512])
            Fs = sb("Fs", [8, 512])
            pW = [ps("pW", [128, 512]) for _ in range(2)]
            pOb = [ps("pO", [128, 512]) for _ in range(2)]
            pO = [t_[:, 0:455].rearrange("p (s d) -> p s d", d=65) for t_ in pOb]
            pT = ps("pT", [128, 2, 4, 128], BF16)
            st = [ps("st", [128, 2, 256]) for _ in range(3)]
            S = Sched(nc)
            S.dma("pool", ident[:], d["ident"], w=["ident"])
            for nm_, t_ in (("exch", exch), ("rb", rb), ("ohm", ohm), ("negm", negm)):
                S.dma("sp", t_[:], d[nm_], w=[nm_])
            S.dma("sp", c31[:], d["c31b"], w=["c31"])
            wv = d["w_out"][l].rearrange("(kc p) n -> p kc n", p=128)
            for kc in range(8):
                S.dma("pool", wout[:, kc, :], wv[:, kc, :], w=[("wout", kc)])
            MM(S, pW[0][:], rb[:], ohm[:], r=["rb", "ohm"], w=[("pW", 0)])
            CP(S, Fs[:], pW[0][0:8, :], r=[("pW", 0)], w=["Fs"])
            fd = S.dma("sp", self.Fd, Fs[:], r=["Fs"], w=["Fd"])
            for off, dst, nm_ in ((0, G, "G"), (128, G2, "G2")):
                S.dma("sp", HK[:], bass.AP(tensor=self.Fd.tensor, offset=off, ap=[[1, 128], [512, 8], [1, 256]]), r=["Fd"], w=["HK"])
                for h in range(8):
                    pw = pW[h % 2]
                    MM(S, pw[:, 0:256], exch[:], HK[:, h, :], r=["exch", "HK"], w=[("pW", h % 2)])
                    CP(S, dst[:, h, :], pw[:, 0:256], r=[("pW", h % 2)], w=[nm_])
            MSET(S, qT[:], 0.0, w=[("qT", 0), ("qT", 1)])
            oslot = [0]

            def emit_qload(qb):
                qbuf = qb % 2
                qk = ("qT", qbuf)
                for par_ in range(2):
                    rs = slice(64 * par_, 64 * par_ + 64)
                    S.dma("sp", qT[rs, qbuf, :, par_, :], self.qT_d[:, rs, qb * 256:(qb + 1) * 256].rearrange("hp p t -> p hp t"),
                          w=[qk])

            def emit_head(qb):
                qbuf = qb % 2
                qk = ("qT", qbuf)
                if qb + 1 < nqb:
                    emit_qload(qb + 1)
                if qb >= 4:
                    pg = pW[0][:, 0:256].rearrange("p (i n) -> p i n", n=16)
                    for h in range(8):
                        hp = h // 2
                        for qt in range(2):
                            MM(S, pg[:, qt * 8 + h, :], qT[:, qbuf, hp, h % 2, qt * 128:(qt + 1) * 128], kmeanT[:, hp, :],
                               r=[qk, "kmeanT"], w=[("pW", 0)])
                    TT(S, gate[:], pg, negm[:, qb, :].unsqueeze(1).to_broadcast([128, 16, 16]), ALU.add,
                       r=[("pW", 0), "negm"], w=["gate"])
                    for i_ in range(16):
                        S.op("dve", lambda e, i_=i_: e.max(out=top8[:, i_, :], in_=gate[:, i_, :]), ["gate"], ["top8"])
                    TT(S, sel[:, qbuf].rearrange("p q h n -> p (q h) n"), gate[:],
                       top8[:, :, 2].unsqueeze(2).to_broadcast([128, 16, 16]), ALU.is_ge, r=["gate", "top8"], w=[("sel", qbuf)])

            def emit_S(it, idx):
                qb, n, h = it
                qbuf, hp, par, b = qb % 2, h // 2, h % 2, idx % 3
                own, prev = (n == qb), (n == qb - 1)
                stb, ptb = st[b], PT[:, b]
                sk, pk, tk, qk = ("st", b), ("PT", b), ("tmp", b), ("qT", qbuf)
                q_all = qT[:, qbuf, hp, par, :]
                MM(S, stb[:, 0, :], kT[:, hp, n * 256:n * 256 + 128], q_all, r=[qk, "kT"], w=[sk])
                if own:
                    MM(S, stb[:, 1, 128:256], kT[:, hp, n * 256 + 128:n * 256 + 256], q_all[:, 128:256], r=[qk, "kT"], w=[sk])
                    TT(S, tmp[:, b, 0, :], stb[:, 0, :], G[:, h, :], ALU.add, r=[sk, "G"], w=[tk])
                    TT(S, tmp[:, b, 1, 128:256], stb[:, 1, 128:256], G[:, h, 0:128], ALU.add, r=[sk, "G"], w=[tk])
                    ACT(S, ptb[:, 0, :], tmp[:, b, 0, :], AF.Exp, r=[tk], w=[pk])
                    ACT(S, ptb[:, 1, 128:256], tmp[:, b, 1, 128:256], AF.Exp, r=[tk], w=[pk])
                else:
                    MM(S, stb[:, 1, :], kT[:, hp, n * 256 + 128:n * 256 + 256], q_all, r=[qk, "kT"], w=[sk])
                    if prev:
                        TT(S, tmp[:, b, 1, :], stb[:, 1, :], G2[:, h, :], ALU.add, r=[sk, "G2"], w=[tk])
                        ACT(S, ptb[:, 0, :], stb[:, 0, :], AF.Exp, r=[sk, tk, "c31"], w=[pk], bias=c31[:, h:h + 1])
                        ACT(S, ptb[:, 1, :], tmp[:, b, 1, :], AF.Exp, r=[tk], w=[pk])
                    else:
                        ACT(S, ptb[:], stb[:], AF.Exp, r=[sk, "c31"], w=[pk], bias=c31[:, h:h + 1])

            def emit_PV(it, idx):
                qb, n, h = it
                qbuf, b = qb % 2, idx % 3
                own = (n == qb)
                ptb, pk = PT[:, b], ("PT", b)
                ok = ("pO", idx % 2)
                for qt in range(2):
                    o = pO[idx % 2][:, qt, :]
                    khs = [0] if (own and qt == 0) else [0, 1]
                    for kh in khs:
                        MM(S, o, ptb[:, kh, qt * 128:(qt + 1) * 128], Vaug[:, n * 2 + kh, h, :], start=(kh == khs[0]),
                           stop=(kh == khs[-1]), r=[pk, "Vaug", "vones"], w=[ok])
                for qt in range(2):
                    o = pO[idx % 2][:, qt, :]
                    ak = ("acc", qbuf, qt, h)
                    if it in first_items:
                        if own or qb < 4:
                            CP(S, acc[:, qbuf, qt, h, :], o, r=[ok], w=[ak])
                        else:
                            TS(S, acc[:, qbuf, qt, h, :], o, sel[:, qbuf, qt, h, n:n + 1], None, ALU.mult,
                               r=[ok, ("sel", qbuf)], w=[ak])
                    elif own or qb < 4:
                        TT(S, acc[:, qbuf, qt, h, :], o, acc[:, qbuf, qt, h, :], ALU.add, r=[ok, ak], w=[ak])
                    else:
                        STT(S, acc[:, qbuf, qt, h, :], o, sel[:, qbuf, qt, h, n:n + 1], acc[:, qbuf, qt, h, :], ALU.mult, ALU.add,
                            r=[ok, ak, ("sel", qbuf)], w=[ak])

            def tail_norm(qb, qt):
                ab = qb % 2
                aks = [("acc", ab, qt, h) for h in range(8)]
                S.op("dve", lambda e: e.reciprocal(out=rec[:, qt, :], in_=acc[:, ab, qt, :, 64]), aks, [("rec", qt)])
                TT(S, att[:, qt, :].rearrange("p (h d) -> p h d", d=64), acc[:, ab, qt, :, 0:64],
                   rec[:, qt, :].unsqueeze(2).to_broadcast([128, 8, 64]), ALU.mult, r=aks + [("rec", qt)], w=[("att", qt)])
                for ck in range(4):
                    TR(S, pT[:, qt, ck, :], att[:, qt, ck * 128:(ck + 1) * 128], ident[:], r=[("att", qt), "ident"], w=["pT"])
                CP(S, attT[:, ab, :, qt * 128:(qt + 1) * 128], pT[:, qt], r=["pT"], w=[("attT", ab, qt)])

            def tail_out(qb, tt, dh):
                ab = qb % 2
                tok0 = qb * 256 + tt * 128
                if dh == 0:
                    S.dma("sp", xr[:, tt, :], x_in[tok0:tok0 + 128, :], w=[("xr", tt)])
                pw = pW[dh]
                for kc in range(8):
                    if kc < 2:
                        lhs, rk_ = ysT[:, kc, tok0:tok0 + 128], "ysT"
                    elif kc < 4:
                        lhs, rk_ = ypT[:, kc - 2, tok0:tok0 + 128], ("ypT", kc - 2)
                    else:
                        lhs, rk_ = attT[:, ab, kc - 4, tt * 128:(tt + 1) * 128], ("attT", ab, tt)
                    MM(S, pw[:], lhs, wout[:, kc, dh * 512:(dh + 1) * 512], start=(kc == 0), stop=(kc == 7),
                       r=[rk_, ("wout", kc)], w=[("pW", dh)])
                TT(S, xr[:, tt, dh * 512:(dh + 1) * 512], pw[:], xr[:, tt, dh * 512:(dh + 1) * 512], ALU.add,
                   r=[("pW", dh), ("xr", tt)], w=[("xr", tt)])
                if dh == 1:
                    S.dma("sp", x_out[tok0:tok0 + 128, :], xr[:, tt, :], r=[("xr", tt)])

            def tail_pieces(qb):
                return ([lambda qt=qt: tail_norm(qb, qt) for qt in range(2)]
                        + [lambda tt=tt, dh=dh: tail_out(qb, tt, dh) for tt in range(2) for dh in range(2)])

            LA = 2
            items = []
            for qb in range(nqb):
                base = [(qb, n, h) for n in range(qb) for h in range(8)]
                for k in range(8):
                    base.insert(min(len(base), (2 * k + 1) * qb // 2 + k), (qb, qb, k))
                items += base
            first_items = set()
            seen_heads = set()
            for it_ in items:
                if (it_[0], it_[2]) not in seen_heads:
                    seen_heads.add((it_[0], it_[2]))
                    first_items.add(it_)
            sched_at = {}
            emit_qload(0)
            emit_head(0)
            for j in range(LA):
                emit_S(items[j], j)
            for i, it in enumerate(items):
                j = i + LA
                if j < len(items):
                    nx = items[j]
                    if nx[0] != items[j - 1][0]:
                        emit_head(nx[0])
                    emit_S(nx, j)
                emit_PV(it, i)
                for fn in sched_at.pop(i, ()):
                    fn()
                if i + 1 == len(items):
                    for fn in tail_pieces(it[0]):
                        fn()
                elif items[i + 1][0] != it[0]:
                    n_next = (it[0] + 2) * 8
                    pcs = tail_pieces(it[0])
                    offs = [(k + 1) * n_next // (len(pcs) + 1) for k in range(len(pcs))]
                    for k, fn in enumerate(pcs):
                        sched_at.setdefault(i + 1 + offs[k], []).append(fn)
            assert not sched_at
            if "mix" in self.dbg and l == 0:
                S.dma("sp", self.dbg_out["ysT"], ysT[:], r=["ysT"])
                S.dma("sp", self.dbg_out["ypT"], ypT[:], r=[("ypT", 0), ("ypT", 1)])
                S.dma("sp", self.dbg_out["kT"], kT[:], r=["kT"])
            S.emit()

    def block_A(self, l, x_in, x_out):
        nc = self.nc
        with ExitStack() as es0:
            sb0 = lambda name, shape, dt=F32: es0.enter_context(nc.sbuf_tensor(self.nm(name), shape, dt))
            R = {}
            R["ysT"] = sb0("ysT", [128, 2, SEQ], BF16)
            R["ypT"] = sb0("ypT", [128, 2, SEQ], BF16)
            with ExitStack() as es1:
                sb1 = lambda name, shape, dt=F32: es1.enter_context(nc.sbuf_tensor(self.nm(name), shape, dt))
                R["u_sb"] = sb1("u_sb", [128, 2, SEQ], BF16)
                R["Wss"] = sb1("Wss", [128, 2, 4, 8, 2, 128], BF16)
                R["Vm"] = sb1("Vm", [128, 8, 2, 8, 128], BF16)
                R["Kmat"] = sb1("Kmat", [128, 2, 8, 128], BF16)
                R["Ak"] = sb1("Ak", [128, 9, 8, 3])
                R["chp"] = sb1("chp", [128, 2, 4])
                R["glu_sb"] = sb1("glu_sb", [128, 2, 256], BF16)
                self.block_proj(l, x_in, "up", R)
                if self.stop_after != ("A0", l):
                    self.block_ssm(l, R)
                if self.stop_after in (("AS", l), ("A0", l)):
                    S = Sched(nc)
                    S.dma("sp", self.dbg_out["ysT"], R["ysT"][:])
                    S.dma("sp", self.dbg_out["ypT"], R["ypT"][:])
                    S.dma("sp", self.dbg_out["u"], R["u_sb"][:])
                    S.dma("sp", self.dbg_out["Kmat"], R["Kmat"][:])
                    S.dma("sp", self.dbg_out["Ak"], R["Ak"][:])
                    S.emit()
                    return
            with ExitStack() as es2:
                sb2 = lambda name, shape, dt=F32: es2.enter_context(nc.sbuf_tensor(self.nm(name), shape, dt))
                R["kT"] = sb2("kT", [128, 4, SEQ], BF16)
                R["Vaug"] = sb2("Vaug", [128, 32, 8, 65], BF16)
                R["ksum"] = sb2("ksum", [128, 4, 16])
                R["kmeanT"] = sb2("kmeanT", [128, 4, 16], BF16)
                self.block_proj(l, x_in, "qkv", R)
                if self.stop_after == ("A1", l):
                    S = Sched(nc)
                    S.dma("sp", self.dbg_out["kT"], R["kT"][:])
                    S.dma("sp", self.dbg_out["Vaug"], R["Vaug"][:])
                    S.dma("sp", self.dbg_out["kmeanT"], R["kmeanT"][:])
                    S.emit()
                    return
                self.block_attn(l, x_in, x_out, R)

    def build_a2only(self, nqb):
        nc, d = self.nc, self.d
        ti = {}
        for name, shp in (("t_ysT", [128, 2, SEQ]), ("t_ypT", [128, 2, SEQ]), ("t_kT", [128, 4, SEQ]), ("t_V", [128, 32, 8, 64]),
                          ("t_kmean", [128, 4, 16]), ("t_q", [4, 128, SEQ])):
            ti[name] = nc.dram_tensor(name, shp, F32, kind="ExternalInput").ap()
        with ExitStack() as es:
            sb = lambda name, shape, dt=F32: es.enter_context(nc.sbuf_tensor(self.nm(name), shape, dt))
            R = {"ysT": sb("ysT", [128, 2, SEQ], BF16), "ypT": sb("ypT", [128, 2, SEQ], BF16), "kT": sb("kT", [128, 4, SEQ], BF16),
                 "Vaug": sb("Vaug", [128, 32, 8, 65], BF16), "ksum": sb("ksum", [128, 4, 16]), "kmeanT": sb("kmeanT", [128, 4, 16], BF16)}
            S = Sched(nc)
            S.dma("pool", R["ysT"][:], ti["t_ysT"]); S.dma("pool", R["ypT"][:], ti["t_ypT"]); S.dma("pool", R["kT"][:], ti["t_kT"])
            for kt4 in range(8):
                S.dma("pool", R["Vaug"][:, 4 * kt4:4 * kt4 + 4, :, 0:64], ti["t_V"][:, 4 * kt4:4 * kt4 + 4], w=["V"])
            S.dma("pool", R["kmeanT"][:], ti["t_kmean"])
            MSET(S, R["Vaug"][:, :, :, 64:65], 1.0, w=["vones"])
            S.dma("pool", self.qT_d, ti["t_q"])
            S.emit()
            self.block_attn(0, d["x"], self.out, R, nqb=nqb)
        return nc

    def build(self):
        d = self.d
        if isinstance(self.stop_after, tuple) and self.stop_after[0] == "A2only":
            return self.build_a2only(self.stop_after[1])
        for l in range(self.n_layers):
            x_in = d["x"] if l == 0 else self.x1
            last = (l == self.n_layers - 1)
            if self.stop_after == "Bonly":
                self.block_B(l, d["x"], self.out, False)
                return self.nc
            if self.stop_after in (("A", l), ("AS", l), ("A0", l), ("A1", l)):
                self.block_A(l, x_in, self.out)
                return self.nc
            self.block_A(l, x_in, self.xmid)
            self.block_B(l, self.xmid, self.out if last else self.x1, last and self.stop_after is None)
        return self.nc


_CONSTS = None


def _prep_inputs(inputs):
    global _CONSTS
    if _CONSTS is None:
        _CONSTS = _consts()
    inp = {k: np.asarray(v) for k, v in inputs.items()}
    lay = _host_layouts(inp)
    shared = {}
    for k in RAW_SHAPES:
        if k != "x":
            shared[k] = np.ascontiguousarray(inp[k], np.float32)
    shared.update(lay)
    shared.update(_CONSTS)
    maps = []
    for b in range(8):
        m = dict(shared)
        m["x"] = np.ascontiguousarray(inp["x"][b], np.float32)
        maps.append(m)
    return maps


def kernel(**inputs):
    maps = _prep_inputs(inputs)
    nc = Prog().build()
    res = run_bass_kernel_spmd(nc, maps, core_ids=list(range(8)))
    return np.stack([np.asarray(r["out"]) for r in res.results], axis=0).astype(np.float32)
```

```python
import math
from contextlib import ExitStack

import numpy as np
import concourse.bass as bass
import concourse.mybir as mybir
from concourse.bass_utils import run_bass_kernel_spmd

F32 = mybir.dt.float32
BF16 = mybir.dt.bfloat16
ALU = mybir.AluOpType
AF = mybir.ActivationFunctionType
AX = mybir.AxisListType

SEQ = 4096
DM = 1024
NL = 2
EPS = 1e-6
NEG = -30000.0

ENGINES = ("pe", "act", "dve", "pool", "sp")
SYNC_ALL_SAME_ENGINE = False
N_DMA_SEMS = 24
N_DMA_SEMS_SP = 16


class _Op:
    __slots__ = ("eng", "fn", "deps", "is_dma", "needs_inc", "inc_idx", "dsem", "dval", "gidx")

    def __init__(self, eng, fn, is_dma, gidx):
        self.eng = eng
        self.fn = fn
        self.deps = []
        self.is_dma = is_dma
        self.needs_inc = False
        self.inc_idx = 0
        self.dsem = None
        self.dval = 0
        self.gidx = gidx


class Sched:
    uid = 0

    def __init__(self, nc):
        self.nc = nc
        self.ops = {e: [] for e in ENGINES}
        self.last_write = {}
        self.readers = {}
        self.n = 0
        self.dma_count = 0
        self.dma_count_pool = 0
        self.dma_last = {}

    def _add(self, eng, fn, reads, writes, is_dma):
        op = _Op(eng, fn, is_dma, self.n)
        self.n += 1
        deps = {}
        raw = set()
        for k in reads:
            w = self.last_write.get(k)
            if w is not None:
                deps[w.gidx] = w
                raw.add(w.gidx)
        for k in writes:
            w = self.last_write.get(k)
            if w is not None:
                deps[w.gidx] = w
            for r in self.readers.get(k, ()):
                deps[r.gidx] = r
        if is_dma:
            if eng == "pool":
                j = N_DMA_SEMS_SP + self.dma_count_pool % (N_DMA_SEMS - N_DMA_SEMS_SP)
                self.dma_count_pool += 1
            else:
                j = self.dma_count % N_DMA_SEMS_SP
                self.dma_count += 1
            prev = self.dma_last.get(j)
            if prev is not None:
                deps[prev.gidx] = prev
                op.dval = prev.dval + 16
            else:
                op.dval = 16
            op.dsem = j
            self.dma_last[j] = op
        deps.pop(op.gidx, None)
        op.deps = [(dd, ((g in raw) or SYNC_ALL_SAME_ENGINE) and eng != "pe") for g, dd in deps.items()]
        for k in writes:
            self.last_write[k] = op
            self.readers[k] = []
        for k in reads:
            if k in writes:
                continue
            self.readers.setdefault(k, []).append(op)
        self.ops[eng].append(op)
        return op

    def op(self, eng, fn, reads=(), writes=()):
        return self._add(eng, fn, tuple(reads), tuple(writes), False)

    def dma(self, eng, out, in_, r=(), w=()):
        return self._add(eng, lambda e: e.dma_start(out=out, in_=in_), tuple(r), tuple(w), True)

    def emit(self):
        nc = self.nc
        for e in ENGINES:
            for op in self.ops[e]:
                for d, raw in op.deps:
                    if (not d.is_dma) and (d.eng != op.eng or raw):
                        d.needs_inc = True
        for e in ENGINES:
            c = 0
            for op in self.ops[e]:
                if op.needs_inc and not op.is_dma:
                    c += 1
                    op.inc_idx = c
        Sched.uid += 1
        esem = {e: nc.alloc_semaphore(name="s%d_%s" % (Sched.uid, e)) for e in ENGINES}
        dsem = [nc.alloc_semaphore(name="d%d_%d" % (Sched.uid, j)) for j in range(N_DMA_SEMS)]
        with ExitStack() as es:
            block = es.enter_context(nc.Block())
            engobj = {"pe": "tensor", "act": "scalar", "dve": "vector", "pool": "gpsimd", "sp": "sync"}

            def make(e):
                def body(eng):
                    waited = {}
                    for op in self.ops[e]:
                        for d, raw in op.deps:
                            if d.is_dma:
                                key = ("d", d.dsem)
                                val = d.dval
                                sem = dsem[d.dsem]
                            else:
                                if d.eng == e and not raw:
                                    continue
                                key = ("e", d.eng)
                                val = d.inc_idx
                                sem = esem[d.eng]
                            if waited.get(key, 0) >= val:
                                continue
                            waited[key] = val
                            eng.wait_ge(sem, val)
                        ins = op.fn(eng)
                        if op.is_dma:
                            ins.then_inc(dsem[op.dsem], 16)
                        elif op.needs_inc:
                            ins.then_inc(esem[e], 1)
                    if e == "sp":
                        for d in self.dma_last.values():
                            key = ("d", d.dsem)
                            if waited.get(key, 0) >= d.dval:
                                continue
                            waited[key] = d.dval
                            eng.wait_ge(dsem[d.dsem], d.dval)
                return body

            for e in ENGINES:
                getattr(block, engobj[e])(make(e))
        nc.clear_and_free_semaphores(list(esem.values()) + dsem)
        nc.all_engine_barrier()


def MM(S, out, lhsT, rhs, start=True, stop=True, r=(), w=()):
    S.op("pe", lambda e: e.matmul(out, lhsT, rhs, start=start, stop=stop), r, w)


def TR(S, out, in_, ident, r=(), w=()):
    S.op("pe", lambda e: e.transpose(out, in_, ident), r, w)


def ACT(S, out, in_, func, r=(), w=(), **kw):
    S.op("act", lambda e: e.activation(out=out, in_=in_, func=func, **kw), r, w)


def TT(S, out, in0, in1, op, r=(), w=(), eng="dve"):
    S.op(eng, lambda e: e.tensor_tensor(out=out, in0=in0, in1=in1, op=op), r, w)


def TS(S, out, in0, s1, s2, op0, op1=None, r=(), w=(), eng="dve"):
    if op1 is None:
        S.op(eng, lambda e: e.tensor_scalar(out=out, in0=in0, scalar1=s1, scalar2=None, op0=op0), r, w)
    else:
        S.op(eng, lambda e: e.tensor_scalar(out=out, in0=in0, scalar1=s1, scalar2=s2, op0=op0, op1=op1), r, w)


def STT(S, out, in0, scalar, in1, op0, op1, r=(), w=(), eng="dve"):
    S.op(eng, lambda e: e.scalar_tensor_tensor(out=out, in0=in0, scalar=scalar, in1=in1, op0=op0, op1=op1), r, w)


def CP(S, out, in_, r=(), w=(), eng="dve"):
    S.op(eng, lambda e: e.tensor_copy(out=out, in_=in_), r, w)


def MSET(S, ap, val, w=(), eng="pool"):
    S.op(eng, lambda e: e.memset(ap, val), (), w)


def _t5_bucket_np(rel):
    rel = np.asarray(rel, dtype=np.int64)
    n = np.maximum(rel, 0)
    max_exact = 16
    nf = np.maximum(n, 1).astype(np.float32)
    large = max_exact + (np.log(nf / np.float32(max_exact)) / np.float32(math.log(128 / max_exact))
                         * np.float32(32 - max_exact)).astype(np.int32)
    large = np.minimum(large, 31)
    return np.where(n < max_exact, n, large)


def _consts():
    c = {}
    c["ident"] = np.eye(128, dtype=np.float32)
    c["exch"] = np.eye(128, dtype=np.float32)[::-1].copy()
    rel = np.arange(512) - 127
    b = _t5_bucket_np(rel)
    oh = np.zeros((32, 512), np.float32)
    oh[b[rel >= 0], np.arange(512)[rel >= 0]] = 1.0
    ohm = np.zeros((128, 512), np.float32)
    ohm[0:32] = oh
    ohm[32, rel < 0] = NEG
    c["ohm"] = ohm
    negm = np.zeros((128, 16, 16), np.float32)
    for qb in range(16):
        negm[:, qb, qb:] = -1e30
    c["negm"] = negm
    wins = (2, 4, 8, 16)
    winv = np.zeros((128, 2), np.float32)
    invc = np.zeros((128, 2, 16), np.float32)
    t = np.arange(16)
    for ct in range(2):
        for half in range(2):
            w = wins[2 * ct + half]
            winv[64 * half:64 * half + 64, ct] = 1.0 / w
            invc[64 * half:64 * half + 64, ct, :] = 1.0 / np.minimum(t + 1, w)
    c["winv"] = winv
    c["invc"] = invc
    esel = np.zeros((32, 16, 128), np.float32)
    for e in range(16):
        esel[e, e, :] = 1.0
        esel[16 + e, e, :] = 1.0
    c["esel"] = esel
    c["neghalfpi"] = np.full((128, 1), math.pi / 2, np.float32)
    return c


CONST_SHAPES = {"ident": [128, 128], "exch": [128, 128], "ohm": [128, 512],
                "negm": [128, 16, 16], "winv": [128, 2], "invc": [128, 2, 16], "esel": [32, 16, 128],
                "neghalfpi": [128, 1]}


def _host_layouts(inp):
    o = {}
    rep = lambda v: np.ascontiguousarray(np.broadcast_to(v[:, None, :], (v.shape[0], 128, v.shape[1])), np.float32)
    o["g1b"] = rep(inp["norm1_g"])
    o["g2b"] = rep(inp["norm2_g"])
    o["gfb"] = np.ascontiguousarray(np.broadcast_to(inp["final_norm_g"][None, :], (128, DM)), np.float32)
    rbp = np.zeros((128, 128), np.float32)
    rbp[0:32, 0:8] = inp["rel_bias"]
    rbp[32, 0:8] = 1.0
    o["rb"] = rbp
    o["c31b"] = np.ascontiguousarray(np.broadcast_to(inp["rel_bias"][31][None, :], (128, 8)), np.float32)
    def sm(a):
        return np.ascontiguousarray(a.reshape(NL, 8, 2, 64).transpose(0, 2, 3, 1).reshape(NL, 128, 8), np.float32)
    ldt = np.broadcast_to(inp["ssm_log_dt"][:, :, None], (NL, 16, 64))
    o["ssmA"] = np.ascontiguousarray(np.stack([sm(inp["ssm_a_re"]), sm(inp["ssm_a_im"]), sm(ldt)], axis=-1))
    def bexp(b):
        out = np.zeros((NL, 128, 8, 128), np.float32)
        for g in range(16):
            P, two = g // 2, g % 2
            cl = (g % 8) * 16
            out[:, two * 64:(two + 1) * 64, P, cl:cl + 16] = b[:, g]
        return out
    def cexp(cm):
        out = np.zeros((NL, 128, 8, 128), np.float32)
        for g in range(16):
            P, two = g // 2, g % 2
            cl = (g % 8) * 16
            out[:, two * 64:(two + 1) * 64, P, cl:cl + 16] = cm[:, g].transpose(0, 2, 1)
        return out
    o["bexp"] = np.stack([bexp(inp["ssm_b_re"]), bexp(inp["ssm_b_im"])], axis=1)
    o["cexp"] = np.stack([cexp(inp["ssm_c_re"]), cexp(inp["ssm_c_im"])], axis=1)
    chm = lambda v: np.ascontiguousarray(v.reshape(NL, 2, 128).transpose(0, 2, 1), np.float32)
    o["chp"] = np.ascontiguousarray(np.stack([chm(inp["ssm_d"]), chm(inp["ssm_glu_b"]), chm(inp["pool_b"]),
                                              chm(inp["pool_scale"])], axis=-1))
    pw = np.zeros((NL, 2, 128, 128), np.float32)
    for ct in range(2):
        for half in range(2):
            pw[:, ct, 64 * half:64 * half + 64, 64 * half:64 * half + 64] = inp["pool_w"][:, 2 * ct + half]
    o["poolw"] = pw
    wr = np.concatenate([inp["moe_group_w"], inp["moe_router_w"].transpose(0, 2, 1, 3).reshape(NL, DM, 16)], axis=-1)
    o["wr"] = np.ascontiguousarray(wr, np.float32)
    br = np.concatenate([inp["moe_group_b"], inp["moe_router_b"].reshape(NL, 16)], axis=-1)
    o["brb"] = np.ascontiguousarray(np.broadcast_to(br[:, None, :], (NL, 128, 20)), np.float32)
    return o


LAYOUT_SHAPES = {"g1b": [NL, 128, DM], "g2b": [NL, 128, DM], "gfb": [128, DM], "rb": [128, 128], "c31b": [128, 8],
                 "ssmA": [NL, 128, 8, 3], "bexp": [NL, 2, 128, 8, 128], "cexp": [NL, 2, 128, 8, 128],
                 "chp": [NL, 128, 2, 4], "poolw": [NL, 2, 128, 128], "wr": [NL, DM, 20], "brb": [NL, 128, 20]}
RAW_SHAPES = {"x": [SEQ, DM], "w_in": [NL, DM, 2048], "ssm_glu_w": [NL, 256, 256], "w_out": [NL, DM, DM],
              "moe_w_gate": [NL, 16, DM, 256], "moe_w_up": [NL, 16, DM, 256], "moe_w_down": [NL, 16, 256, DM]}


class Prog:
    def __init__(self, n_layers=NL, stop_after=None, dbg=()):
        self.nc = nc = bass.Bass("TRN2", target_bir_lowering=False)
        self.n_layers = n_layers
        self.stop_after = stop_after
        self.dbg = set(dbg)
        self.d = {}
        for name, shp in list(RAW_SHAPES.items()) + list(LAYOUT_SHAPES.items()) + list(CONST_SHAPES.items()):
            self.d[name] = nc.dram_tensor(name, shp, F32, kind="ExternalInput").ap()
        self.out = nc.dram_tensor("out", [SEQ, DM], F32, kind="ExternalOutput").ap()
        self.xmid = nc.dram_tensor("xmid", [SEQ, DM], F32).ap()
        self.x1 = nc.dram_tensor("x1", [SEQ, DM], F32).ap()
        self.qT_d = nc.dram_tensor("qT_d", [4, 128, SEQ], BF16).ap()
        self.Fd = nc.dram_tensor("Fd", [8, 512], F32).ap()
        self.dbg_out = {}

    def nm(self, name):
        self.uid = getattr(self, "uid", 0) + 1
        return "t%d_%s" % (self.uid, name)

    def dbg_tensor(self, name, shape, dt=F32):
        t = self.nc.dram_tensor("dbg_" + name, shape, dt, kind="ExternalOutput").ap()
        self.dbg_out[name] = t
        return t

    def norm_tile(self, S, T, x_ap, xkey, g_tile, hT_dst, hT_key, idx):
        self.norm_tile_a(S, T, x_ap, xkey, g_tile, idx)
        self.norm_tile_b(S, T, hT_dst, hT_key, idx)

    def norm_tile_a(self, S, T, x_ap, xkey, g_tile, idx):
        b = idx % 2
        junk, ss, h = T.get("junk"), T["ss"], T["h"]
        if junk is None:
            ACT(S, h[:, b, :], x_ap, AF.Square, r=[xkey], w=[("h", b), ("ss", b)], accum_out=ss[:, b, 0:1])
        else:
            ACT(S, junk[:, 0, :], x_ap, AF.Square, r=[xkey], w=["junk", ("ss", b)], accum_out=ss[:, b, 0:1])
        TS(S, ss[:, b, 1:2], ss[:, b, 0:1], 1.0 / DM, EPS, ALU.mult, ALU.add, r=[("ss", b)], w=[("ss1", b)])
        ACT(S, ss[:, b, 2:3], ss[:, b, 1:2], AF.Sqrt, r=[("ss1", b)], w=[("ss2", b)])
        S.op("dve", lambda e: e.reciprocal(out=ss[:, b, 3:4], in_=ss[:, b, 2:3]), [("ss2", b)], [("ss3", b)])
        STT(S, h[:, b, :], x_ap, ss[:, b, 3:4], g_tile[:], ALU.mult, ALU.mult, r=[xkey, ("ss3", b), "g"], w=[("h", b)])

    def norm_tile_b(self, S, T, hT_dst, hT_key, idx):
        b = idx % 2
        h, pst = T["h"], T["pst"]
        for kc in range(8):
            TR(S, pst[:, b, kc, :], h[:, b, kc * 128:(kc + 1) * 128], T["ident_bf"][:],
               r=[("h", b), "ident_bf"], w=[("pst", b)])
        if idx % 2 == 0:
            ACT(S, hT_dst, pst[:, b, :, :], AF.Copy, r=[("pst", b)], w=[hT_key])
        else:
            CP(S, hT_dst, pst[:, b, :, :], r=[("pst", b)], w=[hT_key])

    def alloc_norm(self, es, T, junk=True):
        nc = self.nc
        if junk:
            T["junk"] = es.enter_context(nc.sbuf_tensor(self.nm("junk"), [128, 1, DM], BF16))
        T["ss"] = es.enter_context(nc.sbuf_tensor(self.nm("ss"), [128, 2, 4], F32))
        T["h"] = es.enter_context(nc.sbuf_tensor(self.nm("h"), [128, 2, DM], BF16))
        T["pst"] = es.enter_context(nc.psum_tensor(self.nm("pst"), [128, 2, 8, 128], BF16))
        T["ident_bf"] = es.enter_context(nc.sbuf_tensor(self.nm("ident_bf"), [128, 128], BF16))

    def load_ident(self, S, T):
        S.dma("pool", T["ident_bf"][:], self.d["ident"], w=["ident_bf"])

    def block_B(self, l, x_in, x_out, final):
        nc, d = self.nc, self.d
        with ExitStack() as es:
            sb = lambda name, shape, dt=F32: es.enter_context(nc.sbuf_tensor(self.nm(name), shape, dt))
            T = {}
            self.alloc_norm(es, T)
            wd = sb("wd", [128, 32, DM], BF16)
            actall = sb("actall", [128, 32, 1024], BF16)
            h2T = sb("h2T", [128, 8, 1024], BF16)
            wg = sb("wg", [128, 2, 8, 256], BF16)
            wu = sb("wu", [128, 2, 8, 256], BF16)
            g2 = sb("g2", [128, DM])
            gf = sb("gf", [128, DM]) if final else None
            xt = sb("xt", [128, 2, DM])
            wr = sb("wr", [128, 8, 20], BF16)
            brb = sb("brb", [128, 20])
            esel = sb("esel", [32, 16, 128], BF16)
            combT = sb("combT", [32, 1024], BF16)
            rt = sb("rt", [128, 2, 96])
            hl = sb("hl", [128, 2, 32], BF16)
            cb = sb("cb", [128, 2, 512])
            sg = sb("sg", [128, 2, 512])
            fin = sb("fin", [128, 2, 4]) if final else None
            psA = [es.enter_context(nc.psum_tensor(self.nm("psA"), [128, 512], F32)) for i in range(2)]
            psB = [es.enter_context(nc.psum_tensor(self.nm("psB"), [128, 512], F32)) for i in range(2)]
            psC = es.enter_context(nc.psum_tensor(self.nm("psC"), [128, 512], F32))
            psL = es.enter_context(nc.psum_tensor(self.nm("psL"), [128, 512], F32))
            S = Sched(nc)
            self.load_ident(S, T)
            S.dma("sp", g2[:], d["g2b"][l], w=["g"])
            if final:
                S.dma("sp", gf[:], d["gfb"], w=["gf"])
            S.dma("sp", brb[:], d["brb"][l], w=["brb"])
            S.dma("pool", wr[:], d["wr"][l].rearrange("(kc p) n -> p kc n", p=128), w=["wr"])
            S.dma("pool", esel[:], d["esel"], w=["esel"])
            wdv = d["moe_w_down"][l].rearrange("e (ft p) d -> p e ft d", p=128)
            wd4 = wd[:].rearrange("p (e ft) d -> p e ft d", ft=2)

            def load_wd():
                for e4 in range(8):
                    S.dma("pool", wd4[:, 2 * e4:2 * e4 + 2], wdv[:, 2 * e4:2 * e4 + 2], w=[("wd", e4)])

            def load_w(e):
                b = e % 2
                S.dma("pool", wg[:, b], d["moe_w_gate"][l, e].rearrange("(kc p) f -> p kc f", p=128), w=[("wg", b)])
                S.dma("pool", wu[:, b], d["moe_w_up"][l, e].rearrange("(kc p) f -> p kc f", p=128), w=[("wu", b)])

            xtR = sb("xtR", [128, 2, DM])

            def R_a(sc, tt):
                b = tt % 2
                tok0 = sc * 1024 + tt * 128
                if tt == 0:
                    S.dma("sp", xtR[:, 0, :], x_in[tok0:tok0 + 128, :], w=[("xtR", 0)])
                if tt + 1 < 8:
                    S.dma("sp", xtR[:, 1 - b, :], x_in[tok0 + 128:tok0 + 256, :], w=[("xtR", 1 - b)])
                self.norm_tile_a(S, T, xtR[:, b, :], ("xtR", b), g2, tt)

            def R_b1(sc, tt):
                self.norm_tile_b(S, T, h2T[:, :, tt * 128:(tt + 1) * 128], ("h2T", tt), tt)

            def R_b2(sc, tt):
                b = tt % 2
                tok0 = sc * 1024 + tt * 128
                lg = psL[:, b * 64:b * 64 + 20]
                for kc in range(8):
                    MM(S, lg, h2T[:, kc, tt * 128:(tt + 1) * 128], wr[:, kc, :], start=(kc == 0), stop=(kc == 7),
                       r=[("h2T", tt), "wr"], w=["psL"])
                R = rt[:, b, :]
                rk = ("rt", b)
                lgs = R[:, 0:20]
                TT(S, lgs, lg, brb[:], ALU.add, r=["psL", "brb"], w=[rk])
                S.op("dve", lambda e, R=R: e.reduce_max(out=R[:, 20:21], in_=R[:, 0:4], axis=AX.X), [rk], [rk])
                TS(S, R[:, 21:22], R[:, 20:21], -1.0, None, ALU.mult, r=[rk], w=[rk])
                ACT(S, R[:, 24:28], R[:, 0:4], AF.Exp, r=[rk], w=[rk], bias=R[:, 21:22], accum_out=R[:, 22:23])
                S.op("dve", lambda e, R=R: e.reciprocal(out=R[:, 23:24], in_=R[:, 22:23]), [rk], [rk])
                TS(S, R[:, 28:32], R[:, 0:4], R[:, 20:21], None, ALU.is_ge, r=[rk], w=[rk])
                TS(S, R[:, 28:32], R[:, 28:32], -1.0, 1e30, ALU.add, ALU.mult, r=[rk], w=[rk])
                em = R[:, 32:48]
                TT(S, em.rearrange("p (g e) -> p g e", e=4), R[:, 4:20].rearrange("p (g e) -> p g e", e=4),
                   R[:, 28:32].unsqueeze(2).to_broadcast([128, 4, 4]), ALU.add, r=[rk], w=[rk])
                S.op("dve", lambda e, R=R: e.max(out=R[:, 48:56], in_=R[:, 32:48]), [rk], [rk])
                TS(S, R[:, 56:57], R[:, 48:49], -1.0, None, ALU.mult, r=[rk], w=[rk])
                ACT(S, R[:, 64:80], em, AF.Exp, r=[rk], w=[rk], bias=R[:, 56:57])
                ACT(S, R[:, 57:58], R[:, 49:50], AF.Exp, r=[rk], w=[rk], bias=R[:, 56:57])
                TS(S, R[:, 57:58], R[:, 57:58], 1.0, None, ALU.add, r=[rk], w=[rk])
                S.op("dve", lambda e, R=R: e.reciprocal(out=R[:, 58:59], in_=R[:, 57:58]), [rk], [rk])
                TT(S, R[:, 59:60], R[:, 58:59], R[:, 23:24], ALU.mult, r=[rk], w=[rk])
                TS(S, R[:, 80:96], em, R[:, 49:50], None, ALU.is_ge, r=[rk], w=[rk])
                STT(S, R[:, 64:80], R[:, 64:80], R[:, 59:60], R[:, 80:96], ALU.mult, ALU.mult, r=[rk], w=[rk])
                CP(S, hl[:, b, 0:16], R[:, 64:80], r=[rk], w=[("hl", b)])
                TT(S, hl[:, b, 16:32], R[:, 64:80], hl[:, b, 0:16], ALU.subtract, r=[rk, ("hl", b)], w=[("hl", b)])
                if "comb" in self.dbg and l == 0:
                    S.dma("sp", self.dbg_out["comb"][tok0:tok0 + 128, :], R[:, 64:80], r=[rk])

            def R_c(sc, tt):
                b = tt % 2
                hlT = T["pst"][0:32, b, 0, :]
                TR(S, hlT, hl[:, b, :], T["ident_bf"][:], r=[("hl", b), "ident_bf"], w=[("pst", b)])
                CP(S, combT[:, tt * 128:(tt + 1) * 128], hlT, r=[("pst", b)], w=[("combT", tt // 4)])

            def D(sc, tt):
                b = tt % 2
                half = tt // 4
                tok0 = sc * 1024 + tt * 128
                if tt == 0:
                    S.dma("sp", xt[:, 0, :], x_in[tok0:tok0 + 128, :], w=[("xt", 0)])
                if tt + 1 < 8:
                    S.dma("sp", xt[:, 1 - b, :], x_in[tok0 + 128:tok0 + 256, :], w=[("xt", 1 - b)])
                for dh in range(2):
                    ps = psA[dh] if b == 0 else psB[dh]
                    pk = ("psA", dh) if b == 0 else ("psB", dh)
                    for ft in range(32):
                        MM(S, ps[:], actall[:, ft, tt * 128:(tt + 1) * 128], wd[:, ft, dh * 512:(dh + 1) * 512],
                           start=(ft == 0), stop=(ft == 31), r=[("actall", half), ("wd", ft // 4)], w=[pk])
                    TT(S, xt[:, b, dh * 512:(dh + 1) * 512], ps[:], xt[:, b, dh * 512:(dh + 1) * 512], ALU.add,
                       r=[pk, ("xt", b)], w=[("xt", b)])
                if final:
                    ACT(S, T["junk"][:, 0, :], xt[:, b, :], AF.Square, r=[("xt", b)], w=["junk", ("fin", b)],
                        accum_out=fin[:, b, 0:1])
                    TS(S, fin[:, b, 1:2], fin[:, b, 0:1], 1.0 / DM, EPS, ALU.mult, ALU.add, r=[("fin", b)], w=[("fin1", b)])
                    ACT(S, fin[:, b, 2:3], fin[:, b, 1:2], AF.Sqrt, r=[("fin1", b)], w=[("fin2", b)])
                    S.op("dve", lambda e, b=b: e.reciprocal(out=fin[:, b, 3:4], in_=fin[:, b, 2:3]), [("fin2", b)], [("fin3", b)])
                    STT(S, xt[:, b, :], xt[:, b, :], fin[:, b, 3:4], gf[:], ALU.mult, ALU.mult,
                        r=[("xt", b), ("fin3", b), "gf"], w=[("xt", b)])
                S.dma("sp", x_out[tok0:tok0 + 128, :], xt[:, b, :], r=[("xt", b)])

            def E(sc):
                for e in range(16):
                    wb = e % 2
                    if e + 1 < 16:
                        load_w(e + 1)
                    elif sc + 1 < 4:
                        load_w(0)
                    for half in range(2):
                        hk = [("h2T", 4 * half + i) for i in range(4)]
                        MM(S, psC[:], esel[:, e, :], combT[:, half * 512:(half + 1) * 512],
                           r=["esel", ("combT", half)], w=["psC"])
                        ACT(S, cb[:, half, :], psC[:], AF.Copy, r=["psC"], w=[("cb", half)])
                        for ft in range(2):
                            pb = (half * 2 + ft) % 2
                            for kc in range(8):
                                MM(S, psA[pb][:], wg[:, wb, kc, ft * 128:(ft + 1) * 128], h2T[:, kc, half * 512:(half + 1) * 512],
                                   start=(kc == 0), stop=(kc == 7), r=hk + [("wg", wb)], w=[("psA", pb)])
                            for kc in range(8):
                                MM(S, psB[pb][:], wu[:, wb, kc, ft * 128:(ft + 1) * 128], h2T[:, kc, half * 512:(half + 1) * 512],
                                   start=(kc == 0), stop=(kc == 7), r=hk + [("wu", wb)], w=[("psB", pb)])
                            ACT(S, sg[:, pb, :], psA[pb][:], AF.Silu, r=[("psA", pb)], w=[("sg", pb)])
                            TT(S, sg[:, pb, :], psB[pb][:], sg[:, pb, :], ALU.mult, r=[("psB", pb), ("sg", pb)], w=[("sg", pb)])
                            TT(S, actall[:, 2 * e + ft, half * 512:(half + 1) * 512], sg[:, pb, :], cb[:, half, :], ALU.mult,
                               r=[("sg", pb), ("cb", half)], w=[("actall", half)], eng="pool")

            def R_prologue(sc):
                R_a(sc, 0)
                R_a(sc, 1)
                R_b1(sc, 0)
                R_b2(sc, 0)
                R_b1(sc, 1)
                R_a(sc, 2)

            def R_step(sc, tt):
                R_c(sc, tt)
                if tt + 1 < 8:
                    R_b2(sc, tt + 1)
                if tt + 2 < 8:
                    R_b1(sc, tt + 2)
                if tt + 3 < 8:
                    R_a(sc, tt + 3)

            R_prologue(0)
            for tt in range(8):
                R_step(0, tt)
            load_w(0)
            load_wd()
            for sc in range(4):
                E(sc)
                nxt = sc + 1 < 4
                if nxt:
                    R_prologue(sc + 1)
                for tt in range(8):
                    D(sc, tt)
                    if nxt:
                        R_step(sc + 1, tt)
            S.emit()


    def cmul(self, S, o_re, o_im, a_re, a_im, b_re, b_im, t1, t2, r, w, tk):
        TT(S, t1, a_re, b_re, ALU.mult, r=r, w=[tk + "1"])
        TT(S, t2, a_im, b_im, ALU.mult, r=r, w=[tk + "2"])
        TT(S, o_re, t1, t2, ALU.subtract, r=[tk + "1", tk + "2"], w=w)
        TT(S, t1, a_re, b_im, ALU.mult, r=r, w=[tk + "1"])
        TT(S, t2, a_im, b_re, ALU.mult, r=r, w=[tk + "2"])
        TT(S, o_im, t1, t2, ALU.add, r=[tk + "1", tk + "2"], w=w)

    def block_proj(self, l, x_in, mode, R):
        nc, d = self.nc, self.d
        ncols = 512 if mode == "up" else 1536
        c0 = 0 if mode == "up" else 512
        with ExitStack() as es:
            sb = lambda name, shape, dt=F32: es.enter_context(nc.sbuf_tensor(self.nm(name), shape, dt))
            ps = lambda name, shape, dt=F32: es.enter_context(nc.psum_tensor(self.nm(name), shape, dt))
            T = {}
            self.alloc_norm(es, T, junk=False)
            win = sb("win", [128, 8, ncols], BF16)
            g1 = sb("g1", [128, DM])
            NXB = 3
            xt = sb("xt", [128, NXB, DM])
            hT = sb("hT", [128, 2, 8, 512], BF16)
            psP = [ps("psP", [128, 512]) for _ in range(2)]
            S = Sched(nc)
            self.load_ident(S, T)
            S.dma("sp", g1[:], d["g1b"][l], w=["g"])
            wv = d["w_in"][l].rearrange("(kc p) n -> p kc n", p=128)
            for kc in range(8):
                S.dma("pool", win[:, kc, :], wv[:, kc, c0:c0 + ncols], w=[("win", kc)])
            wkeys = [("win", kc) for kc in range(8)]
            if mode == "up":
                pbuf = sb("pbuf", [128, 2, 2, 528])
                sA = sb("sA", [128, 2, 528])
                sB = sb("sB", [128, 2, 528])
                dlt = sb("dlt", [128, 2, 512], BF16)
                tfix = sb("tfix", [128, 16])
                poolw = sb("poolw", [128, 2, 128], BF16)
                winv = sb("winv", [128, 2])
                invc = sb("invc", [128, 2, 16])
                psQ = ps("psQ", [128, 2, 512])
                S.dma("pool", poolw[:], d["poolw"][l].rearrange("c k m -> k c m"), w=["poolw"])
                S.dma("sp", winv[:], d["winv"], w=["winv"])
                S.dma("sp", invc[:], d["invc"], w=["invc"])
                S.dma("sp", R["chp"][:], d["chp"][l], w=["chp"])
                MSET(S, pbuf[:, 0, :, 0:16], 0.0, w=[("pbuf", 0)])
                self.ssm_setup(S, l, es, R, T)
            else:
                qst = sb("qst", [128, 2, 4, 512], BF16)
                MSET(S, R["Vaug"][:, :, :, 64:65], 1.0, w=["vones"])
                MSET(S, R["ksum"][:], 0.0, w=["ksum"])
            def load_x(Tg):
                S.dma("sp", xt[:, Tg % NXB, :], x_in[Tg * 128:(Tg + 1) * 128, :], w=[("xt", Tg % NXB)])

            def norm_a(Tg):
                self.norm_tile_a(S, T, xt[:, Tg % NXB, :], ("xt", Tg % NXB), g1, Tg)

            for Tg in range(NXB):
                load_x(Tg)
            norm_a(0)
            norm_a(1)
            for c in range(8):
                hb = c % 2
                for tt in range(4):
                    Tg = c * 4 + tt
                    self.norm_tile_b(S, T, hT[:, hb, :, tt * 128:(tt + 1) * 128], ("hT", hb, tt), Tg)
                    if Tg + NXB < 32:
                        load_x(Tg + NXB)
                    if Tg + 2 < 32:
                        norm_a(Tg + 2)
                    if mode == "up" and tt == 2 and c > 0:
                        self.pool_chunk_back(S, l, c - 1, 1 - hb, pbuf, sA, sB, dlt, tfix, poolw, winv, invc, psQ, R)
                hk = [("hT", hb, tt) for tt in range(4)]
                nmt = ncols // 128 if mode == "up" else 8
                for mt in range(nmt):
                    pb = mt % 2
                    for kc in range(8):
                        MM(S, psP[pb][:], win[:, kc, mt * 128:(mt + 1) * 128], hT[:, hb, kc, :], start=(kc == 0),
                           stop=(kc == 7), r=hk + wkeys, w=[("psP", pb)])
                    if mode == "up":
                        if mt < 2:
                            ACT(S, R["u_sb"][:, mt, :].rearrange("p (t s) -> p t s", t=8)[:, :, c * 64:(c + 1) * 64],
                                psP[pb][:].rearrange("p (s t) -> p t s", t=8), AF.Copy, r=[("psP", pb)], w=[("u", mt)])
                        else:
                            ACT(S, pbuf[:, hb, mt - 2, 16:528], psP[pb][:], AF.Copy, r=[("psP", pb)], w=[("pbuf", hb)])
                    else:
                        if mt < 4:
                            ACT(S, qst[:, hb, mt, :], psP[pb][:], AF.Copy, r=[("psP", pb)], w=[("qst", hb)], scale=0.125)
                        else:
                            hp = mt - 4
                            for bl in range(2):
                                blk = 2 * c + bl
                                ACT(S, R["kT"][:, hp, blk * 256:(blk + 1) * 256], psP[pb][:, bl * 256:(bl + 1) * 256], AF.Copy,
                                    r=[("psP", pb)], w=["kT", "ksum"], accum_out=R["ksum"][:, hp, blk:blk + 1])
                if mode == "up":
                    self.pool_chunk_front(S, l, c, hb, pbuf, sA, sB)
                    if c == 7:
                        self.pool_chunk_back(S, l, c, hb, pbuf, sA, sB, dlt, tfix, poolw, winv, invc, psQ, R)
                else:
                    S.dma("sp", self.qT_d[:, :, c * 512:(c + 1) * 512].rearrange("hp p t -> p hp t"), qst[:, hb], r=[("qst", hb)])
                    for tt in range(4):
                        pb = tt % 2
                        for kc in range(8):
                            MM(S, psP[pb][:], hT[:, hb, kc, tt * 128:(tt + 1) * 128], win[:, kc, 1024:1536], start=(kc == 0),
                               stop=(kc == 7), r=hk + wkeys, w=[("psP", pb)])
                        CP(S, R["Vaug"][:, c * 4 + tt, :, 0:64], psP[pb][:].rearrange("p (h d) -> p h d", d=64),
                           r=[("psP", pb)], w=["Vaug"])
            if mode == "qkv":
                TS(S, R["kmeanT"][:], R["ksum"][:], 1.0 / 256.0, None, ALU.mult, r=["ksum"], w=["kmeanT"])
            S.emit()

    def pool_chunk_front(self, S, l, c, hb, pbuf, sA, sB):
        pk = ("pbuf", hb)
        if c + 1 < 8:
            CP(S, pbuf[:, 1 - hb, :, 0:16], pbuf[:, hb, :, 512:528], r=[pk], w=[("pbuf", 1 - hb)], eng="pool")
        add = lambda o, a, b_, r, w: TT(S, o, a, b_, ALU.add, r=r, w=w, eng="pool")
        for ct in range(2):
            p = pbuf[:, hb, ct, :]
            a_, b_ = sA[:, ct, :], sB[:, ct, :]
            ka, kb = ("sA", ct), ("sB", ct)
            add(a_[:, 1:528], p[:, 1:528], p[:, 0:527], [pk], [ka])
            if ct == 0:
                add(b_[64:128, 3:528], a_[64:128, 3:528], a_[64:128, 1:526], [ka], [kb])
            else:
                add(b_[:, 3:528], a_[:, 3:528], a_[:, 1:526], [ka], [kb])
                add(a_[:, 7:528], b_[:, 7:528], b_[:, 3:524], [kb], [ka])
                add(b_[64:128, 15:528], a_[64:128, 15:528], a_[64:128, 7:520], [ka], [kb])

    def pool_chunk_back(self, S, l, c, hb, pbuf, sA, sB, dlt, tfix, poolw, winv, invc, psQ, R):
        pk = ("pbuf", hb)
        for ct in range(2):
            p = pbuf[:, hb, ct, :]
            for half, src, sk in ((0, sA[:, ct, :], ("sA", ct)), (1, sB[:, ct, :], ("sB", ct))):
                rows = slice(64 * half, 64 * half + 64)
                STT(S, dlt[rows, ct, :], src[rows, 16:528], winv[rows, ct:ct + 1], p[rows, 16:528], ALU.mult, ALU.subtract,
                    r=[sk, pk, "winv"], w=[("dlt", ct)])
                if c == 0:
                    TT(S, tfix[rows, :], src[rows, 16:32], invc[rows, ct, :], ALU.mult, r=[sk, "invc"], w=["tfix"])
                    TT(S, dlt[rows, ct, 0:16], tfix[rows, :], p[rows, 16:32], ALU.subtract, r=["tfix", pk], w=[("dlt", ct)])
            MM(S, psQ[:, ct, :], poolw[:, ct, :], dlt[:, ct, :], r=["poolw", ("dlt", ct)], w=[("psQ", ct)])
        for ct in range(2):
            TS(S, R["ypT"][:, ct, c * 512:(c + 1) * 512], psQ[:, ct, :], R["chp"][:, ct, 2:3], R["chp"][:, ct, 3:4], ALU.add, ALU.mult,
               r=[("psQ", ct), "chp"], w=[("ypT", ct)])

    def ssm_setup(self, S, l, es, R, T):
        nc, d = self.nc, self.d
        sb = lambda name, shape, dt=F32: es.enter_context(nc.sbuf_tensor(self.nm(name), shape, dt))
        ps = lambda name, shape, dt=F32: es.enter_context(nc.psum_tensor(self.nm(name), shape, dt))
        sa = sb("sa", [128, 8, 3])
        sm = sb("sm", [128, 24, 8])
        Lre = sb("Lre", [128, 9, 8]); Lim = sb("Lim", [128, 9, 8])
        Fre = sb("Fre", [128, 8, 8]); Fim = sb("Fim", [128, 8, 8])
        tA = sb("tA", [128, 8, 8]); tB = sb("tB", [128, 8, 8])
        Bre = sb("Bre", [128, 8, 128]); Bim = sb("Bim", [128, 8, 128])
        Cre = sb("Cre", [128, 8, 128]); Cim = sb("Cim", [128, 8, 128]); nCim = sb("nCim", [128, 8, 128])
        scr = R["ysT"][:].bitcast(F32)
        carve = lambda i: scr[:, i // 2, (i % 2) * 1024:(i % 2) * 1024 + 1024].rearrange("p (a b) -> p a b", b=128)
        LBre, LBim, t1, t2 = carve(0), carve(1), carve(2), carve(3)
        LBb = sb("LBb", [128, 2, 8, 128], BF16)
        hpi = sb("hpi", [128, 1])
        psW = ps("psW", [128, 8, 128], BF16)
        psK = ps("psK", [128, 512])
        glu = R["glu_sb"]
        S.dma("pool", glu[:], d["ssm_glu_w"][l].rearrange("(kc p) n -> p kc n", p=128), w=["glu"])
        S.dma("sp", sa[:], d["ssmA"][l], w=["sa"])
        S.dma("sp", hpi[:], d["neghalfpi"], w=["hpi"])
        S.dma("sp", Bre[:], d["bexp"][l, 0], w=["Bre"])
        S.dma("sp", Bim[:], d["bexp"][l, 1], w=["Bim"])
        S.dma("sp", Cre[:], d["cexp"][l, 0], w=["Cre"])
        S.dma("sp", Cim[:], d["cexp"][l, 1], w=["Cim"])
        TS(S, nCim[:], Cim[:], -1.0, None, ALU.mult, r=["Cim"], w=["nCim"])
        a_re, a_im, ldt = sa[:, :, 0], sa[:, :, 1], sa[:, :, 2]
        k = lambda i: ("sm", i)
        sl = lambda i: sm[:, i, :]
        ACT(S, sl(0), ldt, AF.Exp, r=["sa"], w=[k(0)])
        TT(S, sl(1), a_re, sl(0), ALU.mult, r=["sa", k(0)], w=[k(1)])
        TT(S, sl(2), a_im, sl(0), ALU.mult, r=["sa", k(0)], w=[k(2)])
        ACT(S, sl(3), sl(1), AF.Exp, r=[k(1)], w=[k(3)], scale=1.0 / 32)
        ACT(S, sl(4), sl(2), AF.Sin, r=[k(2)], w=[k(4)], scale=1.0 / 32)
        ACT(S, sl(5), sl(2), AF.Sin, r=[k(2), "hpi"], w=[k(5)], scale=1.0 / 32, bias=hpi[:, 0:1])
        TT(S, sl(6), sl(3), sl(5), ALU.mult, r=[k(3), k(5)], w=[k(6)])
        TT(S, sl(7), sl(3), sl(4), ALU.mult, r=[k(3), k(4)], w=[k(7)])
        for _ in range(5):
            TT(S, sl(8), sl(6), sl(6), ALU.mult, r=[k(6)], w=[k(8)])
            TT(S, sl(9), sl(7), sl(7), ALU.mult, r=[k(7)], w=[k(9)])
            TT(S, sl(10), sl(6), sl(7), ALU.mult, r=[k(6), k(7)], w=[k(10)])
            TT(S, sl(6), sl(8), sl(9), ALU.subtract, r=[k(8), k(9)], w=[k(6)])
            TS(S, sl(7), sl(10), 2.0, None, ALU.mult, r=[k(10)], w=[k(7)])
        MSET(S, Lre[:, 0, :], 1.0, w=[("L", 0)], eng="dve")
        MSET(S, Lim[:, 0, :], 0.0, w=[("L", 0)], eng="dve")
        CP(S, Lre[:, 1, :], sl(6), r=[k(6)], w=[("L", 1)])
        CP(S, Lim[:, 1, :], sl(7), r=[k(7)], w=[("L", 1)])
        for j in range(1, 8):
            self.cmul(S, Lre[:, j + 1, :], Lim[:, j + 1, :], Lre[:, j, :], Lim[:, j, :], sl(6), sl(7), sl(8), sl(9),
                      r=[("L", j), k(6), k(7)], w=[("L", j + 1)], tk="smt")
        Ak = R["Ak"]
        CP(S, Ak[:, 0, :, 0], Lre[:, 8, :], r=[("L", 8)], w=[("Ak", 0)])
        CP(S, Ak[:, 0, :, 1], Lim[:, 8, :], r=[("L", 8)], w=[("Ak", 0)])
        for kk in range(8):
            self.cmul(S, Ak[:, kk + 1, :, 0], Ak[:, kk + 1, :, 1], Ak[:, kk, :, 0], Ak[:, kk, :, 1], Ak[:, kk, :, 0], Ak[:, kk, :, 1],
                      sl(8), sl(9), r=[("Ak", kk)], w=[("Ak", kk + 1)], tk="smt")
        for kk in range(9):
            TS(S, Ak[:, kk, :, 2], Ak[:, kk, :, 1], -1.0, None, ALU.mult, r=[("Ak", kk)], w=[("Akn", kk)])
        TT(S, sl(11), a_re, a_re, ALU.mult, r=["sa"], w=[k(11)])
        TT(S, sl(12), a_im, a_im, ALU.mult, r=["sa"], w=[k(12)])
        TT(S, sl(11), sl(11), sl(12), ALU.add, r=[k(11), k(12)], w=[k(11)])
        S.op("dve", lambda e: e.reciprocal(out=sm[:, 12, :], in_=sm[:, 11, :]), [k(11)], [k(12)])
        TS(S, sl(13), sl(6), -1.0, None, ALU.add, r=[k(6)], w=[k(13)])
        TT(S, sl(14), sl(13), a_re, ALU.mult, r=[k(13), "sa"], w=[k(14)])
        TT(S, sl(15), sl(7), a_im, ALU.mult, r=[k(7), "sa"], w=[k(15)])
        TT(S, sl(14), sl(14), sl(15), ALU.add, r=[k(14), k(15)], w=[k(14)])
        TT(S, sl(16), sl(14), sl(12), ALU.mult, r=[k(14), k(12)], w=[k(16)])
        TT(S, sl(14), sl(7), a_re, ALU.mult, r=[k(7), "sa"], w=[k(14)])
        TT(S, sl(15), sl(13), a_im, ALU.mult, r=[k(13), "sa"], w=[k(15)])
        TT(S, sl(14), sl(14), sl(15), ALU.subtract, r=[k(14), k(15)], w=[k(14)])
        TT(S, sl(17), sl(14), sl(12), ALU.mult, r=[k(14), k(12)], w=[k(17)])
        Lk = [("L", j) for j in range(9)]
        bc = lambda ap: ap.unsqueeze(1).to_broadcast([128, 8, 8])
        self.cmul(S, Fre[:], Fim[:], Lre[:, 0:8, :], Lim[:, 0:8, :], bc(sl(16)), bc(sl(17)), tA[:], tB[:],
                  r=Lk + [k(16), k(17)], w=["F"], tk="tAB")
        bc3 = lambda ap: ap.unsqueeze(2).to_broadcast([128, 8, 128])
        Wss, Kmat, Vm = R["Wss"], R["Kmat"], R["Vm"]
        for j in range(8):
            self.cmul(S, LBre, LBim, bc3(Fre[:, j, :]), bc3(Fim[:, j, :]), Bre[:], Bim[:], t1, t2,
                      r=["F", "Bre", "Bim"], w=["LB"], tk="t12")
            CP(S, LBb[:, 0], LBre[:], r=["LB"], w=["LBb"])
            CP(S, LBb[:, 1], LBim[:], r=["LB"], w=["LBb"])
            for ri in range(2):
                for P in range(8):
                    TR(S, psW[:, P, :], LBb[:, ri, P, :], T["ident_bf"][:], r=["LBb", "ident_bf"], w=["psW"])
                ACT(S, Wss[:, :, :, 7 - j, ri, :].rearrange("p a b s -> p (a b) s"), psW[:], AF.Copy, r=["psW"], w=["Wss"])
            for ct in range(2):
                reg = psK[:, ct * 128:(ct + 1) * 128]
                for i in range(4):
                    P = ct * 4 + i
                    MM(S, reg, LBre[:, P, :], Cre[:, P, :], start=(i == 0), stop=False, r=["LB", "Cre"], w=["psK"])
                for i in range(4):
                    P = ct * 4 + i
                    MM(S, reg, LBim[:, P, :], nCim[:, P, :], start=False, stop=(i == 3), r=["LB", "nCim"], w=["psK"])
            ACT(S, Kmat[:, :, j, :], psK[:, 0:256].rearrange("p (c m) -> p c m", m=128), AF.Copy, r=["psK"], w=["Kmat"])
        for tau in range(8):
            self.cmul(S, LBre, LBim, bc3(Lre[:, tau + 1, :]), bc3(Lim[:, tau + 1, :]), Cre[:], Cim[:], t1[:], t2[:],
                      r=Lk + ["Cre", "Cim"], w=["LB"], tk="t12")
            CP(S, Vm[:, :, 0, tau, :], LBre[:], r=["LB"], w=["Vm"])
            TS(S, Vm[:, :, 1, tau, :], LBim[:], -1.0, None, ALU.mult, r=["LB"], w=["Vm"])

    def block_ssm(self, l, R):
        nc = self.nc
        with ExitStack() as es:
            sb = lambda name, shape, dt=F32: es.enter_context(nc.sbuf_tensor(self.nm(name), shape, dt))
            ps = lambda name, shape, dt=F32: es.enter_context(nc.psum_tensor(self.nm(name), shape, dt))
            Xs = sb("Xs", [128, 2, 2, 768])
            XsP = None
            Xprev = sb("Xprev", [128, 8, 2, 512], BF16)
            t1 = sb("t1s", [128, 4096])
            ygb = sb("ygb", [128, 2, 4096], BF16)
            sgl = sb("sgl", [128, 2, 512])
            psZ = [ps("psZ", [128, 512]) for _ in range(2)]
            psZP = None
            psY = [ps("psY", [128, 512]) for _ in range(2)]
            psG = [ps("psG", [128, 512]) for _ in range(2)]
            S = Sched(nc)
            u_sb, Wss, Kmat, Vm, Ak, chp, glu, ysT = (R[k_] for k_ in ("u_sb", "Wss", "Kmat", "Vm", "Ak", "chp", "glu_sb", "ysT"))
            allk = lambda nm_: [(nm_, pp_, ri_) for pp_ in range(2) for ri_ in range(2)]
            MSET(S, Xs[:, :, :, 0:256], 0.0, w=allk("Xs"))
            MSET(S, Xprev[:, :, :, 0:1], 0.0, w=["Xprev"])
            u4 = [u_sb[:, ct, :].rearrange("p (t s) -> p t s", t=8) for ct in range(2)]

            def scan_chain(P, eng):
                ct, pi = P // 4, P % 4
                X, nm_, zb = (Xs, "Xs", psZ) if eng == "dve" else (XsP, "XsP", psZP)
                zn = "psZ" if eng == "dve" else "psZP"
                for ri in range(2):
                    z = zb[ri]
                    for sg in range(8):
                        MM(S, z[:], Wss[:, ct, pi, sg, ri, :], u4[ct][:, sg, :], start=(sg == 0), stop=(sg == 7),
                           r=["Wss", ("u", ct)], w=[(zn, ri)])
                    if eng == "dve":
                        CP(S, X[:, 0, ri, 256:768], z[:], r=[(zn, ri)], w=[(nm_, 0, ri)])
                    else:
                        ACT(S, X[:, 0, ri, 256:768], z[:], AF.Copy, r=[(zn, ri)], w=[(nm_, 0, ri)])
                pp = 0
                for kk in range(9):
                    o = 1 << kk
                    src, dst = X[:, pp], X[:, 1 - pp]
                    sre, sim, dre, dim = (nm_, pp, 0), (nm_, pp, 1), (nm_, 1 - pp, 0), (nm_, 1 - pp, 1)
                    are, aim, naim = Ak[:, kk, P:P + 1, 0], Ak[:, kk, P:P + 1, 1], Ak[:, kk, P:P + 1, 2]
                    sh = slice(256 - o, 768 - o)
                    STT(S, dst[:, 0, 256:768], src[:, 0, sh], are, src[:, 0, 256:768], ALU.mult, ALU.add, r=[sre], w=[dre], eng=eng)
                    STT(S, dst[:, 1, 256:768], src[:, 1, sh], are, src[:, 1, 256:768], ALU.mult, ALU.add, r=[sim], w=[dim], eng=eng)
                    STT(S, dst[:, 0, 256:768], src[:, 1, sh], naim, dst[:, 0, 256:768], ALU.mult, ALU.add, r=[sim, dre], w=[dre], eng=eng)
                    STT(S, dst[:, 1, 256:768], src[:, 0, sh], aim, dst[:, 1, 256:768], ALU.mult, ALU.add, r=[sre, dim], w=[dim], eng=eng)
                    pp = 1 - pp
                CP(S, Xprev[:, P, :, 1:512], X[:, pp, :, 256:767], r=[(nm_, pp, 0), (nm_, pp, 1)], w=[("Xprev", P)], eng=eng)

            for P in range(8):
                scan_chain(P, "dve")
            for ct in range(2):
                t1v = t1[:].rearrange("p (s t) -> p t s", t=8)
                for tau in range(8):
                    y = psY[tau % 2]
                    yk = ("psY", tau % 2)
                    n_mm = tau + 1 + 8
                    i = 0
                    for sg in range(tau + 1):
                        MM(S, y[:], Kmat[:, ct, tau - sg, :], u4[ct][:, sg, :], start=(i == 0), stop=(i == n_mm - 1), r=[], w=[yk])
                        i += 1
                    for pi in range(4):
                        for ri in range(2):
                            MM(S, y[:], Vm[:, ct * 4 + pi, ri, tau, :], Xprev[:, ct * 4 + pi, ri, :], start=(i == 0),
                               stop=(i == n_mm - 1), r=["Xprev", ("Xprev", ct * 4 + pi)], w=[yk])
                            i += 1
                    STT(S, t1v[:, tau, :], u4[ct][:, tau, :], chp[:, ct, 0:1], y[:], ALU.mult, ALU.add, r=[yk], w=["t1"])
                for cc in range(8):
                    ACT(S, ygb[:, ct, cc * 512:(cc + 1) * 512], t1[:, cc * 512:(cc + 1) * 512], AF.Gelu_apprx_tanh,
                        r=["t1"], w=[("ygb", ct)])
            for cc in range(8):
                for mt in range(2):
                    g = psG[mt]
                    for kc in range(2):
                        MM(S, g[:], glu[:, kc, mt * 128:(mt + 1) * 128], ygb[:, kc, cc * 512:(cc + 1) * 512], start=(kc == 0),
                           stop=(kc == 1), r=[("ygb", 0), ("ygb", 1)], w=[("psG", mt)])
                    ACT(S, sgl[:, mt, :], g[:], AF.Sigmoid, r=[("psG", mt)], w=[("sgl", mt)], bias=chp[:, mt, 1:2])
                    TT(S, ysT[:, mt, cc * 512:(cc + 1) * 512], ygb[:, mt, cc * 512:(cc + 1) * 512], sgl[:, mt, :], ALU.mult,
                       r=[("sgl", mt), ("ygb", mt)], w=["ysT"])
            S.emit()

    def block_attn(self, l, x_in, x_out, R, nqb=16):
        nc, d = self.nc, self.d
        with ExitStack() as es:
            sb = lambda name, shape, dt=F32: es.enter_context(nc.sbuf_tensor(self.nm(name), shape, dt))
            ps = lambda name, shape, dt=F32: es.enter_context(nc.psum_tensor(self.nm(name), shape, dt))
            kT, Vaug, kmeanT, ysT, ypT = (R[k_] for k_ in ("kT", "Vaug", "kmeanT", "ysT", "ypT"))
            wout = sb("wout", [128, 8, DM], BF16)
            qT = sb("qT", [128, 2, 4, 2, 256], BF16)
            G = sb("G", [128, 8, 256])
            G2 = sb("G2", [128, 8, 256])
            HK = sb("HK", [128, 8, 256])
            c31 = sb("c31", [128, 8])
            negm = sb("negm", [128, 16, 16])
            PT = sb("PT", [128, 3, 2, 256], BF16)
            tmp = sb("tmp", [128, 3, 2, 256])
            gate = sb("gate", [128, 16, 16])
            top8 = sb("top8", [128, 16, 8])
            sel = sb("sel", [128, 2, 2, 8, 16])
            acc = sb("acc", [128, 2, 2, 8, 65])
            rec = sb("rec", [128, 2, 8])
            att = sb("att", [128, 2, 512], BF16)
            attT = sb("attT", [128, 2, 4, 256], BF16)
            xr = sb("xr", [128, 2, DM])
            ident = sb("identb", [128, 128], BF16)
            exch = sb("exch", [128, 128])
            rb = sb("rb", [128, 128]); ohm = sb("ohm", [128, 512])
            Fs = sb("Fs", [8, 512])
            pW = [ps("pW", [128, 512]) for _ in range(2)]
            pOb = [ps("pO", [128, 512]) for _ in range(2)]
            pO = [t_[:, 0:455].rearrange("p (s d) -> p s d", d=65) for t_ in pOb]
            pT = ps("pT", [128, 2, 4, 128], BF16)
            st = [ps("st", [128, 2, 256]) for _ in range(3)]
            S = Sched(nc)
            S.dma("pool", ident[:], d["ident"], w=["ident"])
            for nm_, t_ in (("exch", exch), ("rb", rb), ("ohm", ohm), ("negm", negm)):
                S.dma("sp", t_[:], d[nm_], w=[nm_])
            S.dma("sp", c31[:], d["c31b"], w=["c31"])
            wv = d["w_out"][l].rearrange("(kc p) n -> p kc n", p=128)
            for kc in range(8):
                S.dma("pool", wout[:, kc, :], wv[:, kc, :], w=[("wout", kc)])
            MM(S, pW[0][:], rb[:], ohm[:], r=["rb", "ohm"], w=[("pW", 0)])
            CP(S, Fs[:], pW[0][0:8, :], r=[("pW", 0)], w=["Fs"])
            fd = S.dma("sp", self.Fd, Fs[:], r=["Fs"], w=["Fd"])
            for off, dst, nm_ in ((0, G, "G"), (128, G2, "G2")):
                S.dma("sp", HK[:], bass.AP(tensor=self.Fd.tensor, offset=off, ap=[[1, 128], [512, 8], [1, 256]]), r=["Fd"], w=["HK"])
                for h in range(8):
                    pw = pW[h % 2]
                    MM(S, pw[:, 0:256], exch[:], HK[:, h, :], r=["exch", "HK"], w=[("pW", h % 2)])
                    CP(S, dst[:, h, :], pw[:, 0:256], r=[("pW", h % 2)], w=[nm_])
            MSET(S, qT[:], 0.0, w=[("qT", 0), ("qT", 1)])
            oslot = [0]

            def emit_qload(qb):
                qbuf = qb % 2
                qk = ("qT", qbuf)
                for par_ in range(2):
                    rs = slice(64 * par_, 64 * par_ + 64)
                    S.dma("sp", qT[rs, qbuf, :, par_, :], self.qT_d[:, rs, qb * 256:(qb + 1) * 256].rearrange("hp p t -> p hp t"),
                          w=[qk])

            def emit_head(qb):
                qbuf = qb % 2
                qk = ("qT", qbuf)
                if qb + 1 < nqb:
                    emit_qload(qb + 1)
                if qb >= 4:
                    pg = pW[0][:, 0:256].rearrange("p (i n) -> p i n", n=16)
                    for h in range(8):
                        hp = h // 2
                        for qt in range(2):
                            MM(S, pg[:, qt * 8 + h, :], qT[:, qbuf, hp, h % 2, qt * 128:(qt + 1) * 128], kmeanT[:, hp, :],
                               r=[qk, "kmeanT"], w=[("pW", 0)])
                    TT(S, gate[:], pg, negm[:, qb, :].unsqueeze(1).to_broadcast([128, 16, 16]), ALU.add,
                       r=[("pW", 0), "negm"], w=["gate"])
                    for i_ in range(16):
                        S.op("dve", lambda e, i_=i_: e.max(out=top8[:, i_, :], in_=gate[:, i_, :]), ["gate"], ["top8"])
                    TT(S, sel[:, qbuf].rearrange("p q h n -> p (q h) n"), gate[:],
                       top8[:, :, 2].unsqueeze(2).to_broadcast([128, 16, 16]), ALU.is_ge, r=["gate", "top8"], w=[("sel", qbuf)])

            def emit_S(it, idx):
                qb, n, h = it
                qbuf, hp, par, b = qb % 2, h // 2, h % 2, idx % 3
                own, prev = (n == qb), (n == qb - 1)
                stb, ptb = st[b], PT[:, b]
                sk, pk, tk, qk = ("st", b), ("PT", b), ("tmp", b), ("qT", qbuf)
                q_all = qT[:, qbuf, hp, par, :]
                MM(S, stb[:, 0, :], kT[:, hp, n * 256:n * 256 + 128], q_all, r=[qk, "kT"], w=[sk])
                if own:
                    MM(S, stb[:, 1, 128:256], kT[:, hp, n * 256 + 128:n * 256 + 256], q_all[:, 128:256], r=[qk, "kT"], w=[sk])
                    TT(S, tmp[:, b, 0, :], stb[:, 0, :], G[:, h, :], ALU.add, r=[sk, "G"], w=[tk])
                    TT(S, tmp[:, b, 1, 128:256], stb[:, 1, 128:256], G[:, h, 0:128], ALU.add, r=[sk, "G"], w=[tk])
                    ACT(S, ptb[:, 0, :], tmp[:, b, 0, :], AF.Exp, r=[tk], w=[pk])
                    ACT(S, ptb[:, 1, 128:256], tmp[:, b, 1, 128:256], AF.Exp, r=[tk], w=[pk])
                else:
                    MM(S, stb[:, 1, :], kT[:, hp, n * 256 + 128:n * 256 + 256], q_all, r=[qk, "kT"], w=[sk])
                    if prev:
                        TT(S, tmp[:, b, 1, :], stb[:, 1, :], G2[:, h, :], ALU.add, r=[sk, "G2"], w=[tk])
                        ACT(S, ptb[:, 0, :], stb[:, 0, :], AF.Exp, r=[sk, tk, "c31"], w=[pk], bias=c31[:, h:h + 1])
                        ACT(S, ptb[:, 1, :], tmp[:, b, 1, :], AF.Exp, r=[tk], w=[pk])
                    else:
                        ACT(S, ptb[:], stb[:], AF.Exp, r=[sk, "c31"], w=[pk], bias=c31[:, h:h + 1])

            def emit_PV(it, idx):
                qb, n, h = it
                qbuf, b = qb % 2, idx % 3
                own = (n == qb)
                ptb, pk = PT[:, b], ("PT", b)
                ok = ("pO", idx % 2)
                for qt in range(2):
                    o = pO[idx % 2][:, qt, :]
                    khs = [0] if (own and qt == 0) else [0, 1]
                    for kh in khs:
                        MM(S, o, ptb[:, kh, qt * 128:(qt + 1) * 128], Vaug[:, n * 2 + kh, h, :], start=(kh == khs[0]),
                           stop=(kh == khs[-1]), r=[pk, "Vaug", "vones"], w=[ok])
                for qt in range(2):
                    o = pO[idx % 2][:, qt, :]
                    ak = ("acc", qbuf, qt, h)
                    if it in first_items:
                        if own or qb < 4:
                            CP(S, acc[:, qbuf, qt, h, :], o, r=[ok], w=[ak])
                        else:
                            TS(S, acc[:, qbuf, qt, h, :], o, sel[:, qbuf, qt, h, n:n + 1], None, ALU.mult,
                               r=[ok, ("sel", qbuf)], w=[ak])
                    elif own or qb < 4:
                        TT(S, acc[:, qbuf, qt, h, :], o, acc[:, qbuf, qt, h, :], ALU.add, r=[ok, ak], w=[ak])
                    else:
                        STT(S, acc[:, qbuf, qt, h, :], o, sel[:, qbuf, qt, h, n:n + 1], acc[:, qbuf, qt, h, :], ALU.mult, ALU.add,
                            r=[ok, ak, ("sel", qbuf)], w=[ak])

            def tail_norm(qb, qt):
                ab = qb % 2
                aks = [("acc", ab, qt, h) for h in range(8)]
                S.op("dve", lambda e: e.reciprocal(out=rec[:, qt, :], in_=acc[:, ab, qt, :, 64]), aks, [("rec", qt)])
                TT(S, att[:, qt, :].rearrange("p (h d) -> p h d", d=64), acc[:, ab, qt, :, 0:64],
                   rec[:, qt, :].unsqueeze(2).to_broadcast([128, 8, 64]), ALU.mult, r=aks + [("rec", qt)], w=[("att", qt)])
                for ck in range(4):
                    TR(S, pT[:, qt, ck, :], att[:, qt, ck * 128:(ck + 1) * 128], ident[:], r=[("att", qt), "ident"], w=["pT"])
                CP(S, attT[:, ab, :, qt * 128:(qt + 1) * 128], pT[:, qt], r=["pT"], w=[("attT", ab, qt)])

            def tail_out(qb, tt, dh):
                ab = qb % 2
                tok0 = qb * 256 + tt * 128
                if dh == 0:
                    S.dma("sp", xr[:, tt, :], x_in[tok0:tok0 + 128, :], w=[("xr", tt)])
                pw = pW[dh]
                for kc in range(8):
                    if kc < 2:
                        lhs, rk_ = ysT[:, kc, tok0:tok0 + 128], "ysT"
                    elif kc < 4:
                        lhs, rk_ = ypT[:, kc - 2, tok0:tok0 + 128], ("ypT", kc - 2)
                    else:
                        lhs, rk_ = attT[:, ab, kc - 4, tt * 128:(tt + 1) * 128], ("attT", ab, tt)
                    MM(S, pw[:], lhs, wout[:, kc, dh * 512:(dh + 1) * 512], start=(kc == 0), stop=(kc == 7),
                       r=[rk_, ("wout", kc)], w=[("pW", dh)])
                TT(S, xr[:, tt, dh * 512:(dh + 1) * 512], pw[:], xr[:, tt, dh * 512:(dh + 1) * 512], ALU.add,
                   r=[("pW", dh), ("xr", tt)], w=[("xr", tt)])
                if dh == 1:
                    S.dma("sp", x_out[tok0:tok0 + 128, :], xr[:, tt, :], r=[("xr", tt)])

            def tail_pieces(qb):
                return ([lambda qt=qt: tail_norm(qb, qt) for qt in range(2)]
                        + [lambda tt=tt, dh=dh: tail_out(qb, tt, dh) for tt in range(2) for dh in range(2)])

            LA = 2
            items = []
            for qb in range(nqb):
                base = [(qb, n, h) for n in range(qb) for h in range(8)]
                for k in range(8):
                    base.insert(min(len(base), (2 * k + 1) * qb // 2 + k), (qb, qb, k))
                items += base
            first_items = set()
            seen_heads = set()
            for it_ in items:
                if (it_[0], it_[2]) not in seen_heads:
                    seen_heads.add((it_[0], it_[2]))
                    first_items.add(it_)
            sched_at = {}
            emit_qload(0)
            emit_head(0)
            for j in range(LA):
                emit_S(items[j], j)
            for i, it in enumerate(items):
                j = i + LA
                if j < len(items):
                    nx = items[j]
                    if nx[0] != items[j - 1][0]:
                        emit_head(nx[0])
                    emit_S(nx, j)
                emit_PV(it, i)
                for fn in sched_at.pop(i, ()):
                    fn()
                if i + 1 == len(items):
                    for fn in tail_pieces(it[0]):
                        fn()
                elif items[i + 1][0] != it[0]:
                    n_next = (it[0] + 2) * 8
                    pcs = tail_pieces(it[0])
                    offs = [(k + 1) * n_next // (len(pcs) + 1) for k in range(len(pcs))]
                    for k, fn in enumerate(pcs):
                        sched_at.setdefault(i + 1 + offs[k], []).append(fn)
            assert not sched_at
            if "mix" in self.dbg and l == 0:
                S.dma("sp", self.dbg_out["ysT"], ysT[:], r=["ysT"])
                S.dma("sp", self.dbg_out["ypT"], ypT[:], r=[("ypT", 0), ("ypT", 1)])
                S.dma("sp", self.dbg_out["kT"], kT[:], r=["kT"])
            S.emit()

    def block_A(self, l, x_in, x_out):
        nc = self.nc
        with ExitStack() as es0:
            sb0 = lambda name, shape, dt=F32: es0.enter_context(nc.sbuf_tensor(self.nm(name), shape, dt))
            R = {}
            R["ysT"] = sb0("ysT", [128, 2, SEQ], BF16)
            R["ypT"] = sb0("ypT", [128, 2, SEQ], BF16)
            with ExitStack() as es1:
                sb1 = lambda name, shape, dt=F32: es1.enter_context(nc.sbuf_tensor(self.nm(name), shape, dt))
                R["u_sb"] = sb1("u_sb", [128, 2, SEQ], BF16)
                R["Wss"] = sb1("Wss", [128, 2, 4, 8, 2, 128], BF16)
                R["Vm"] = sb1("Vm", [128, 8, 2, 8, 128], BF16)
                R["Kmat"] = sb1("Kmat", [128, 2, 8, 128], BF16)
                R["Ak"] = sb1("Ak", [128, 9, 8, 3])
                R["chp"] = sb1("chp", [128, 2, 4])
                R["glu_sb"] = sb1("glu_sb", [128, 2, 256], BF16)
                self.block_proj(l, x_in, "up", R)
                if self.stop_after != ("A0", l):
                    self.block_ssm(l, R)
                if self.stop_after in (("AS", l), ("A0", l)):
                    S = Sched(nc)
                    S.dma("sp", self.dbg_out["ysT"], R["ysT"][:])
                    S.dma("sp", self.dbg_out["ypT"], R["ypT"][:])
                    S.dma("sp", self.dbg_out["u"], R["u_sb"][:])
                    S.dma("sp", self.dbg_out["Kmat"], R["Kmat"][:])
                    S.dma("sp", self.dbg_out["Ak"], R["Ak"][:])
                    S.emit()
                    return
            with ExitStack() as es2:
                sb2 = lambda name, shape, dt=F32: es2.enter_context(nc.sbuf_tensor(self.nm(name), shape, dt))
                R["kT"] = sb2("kT", [128, 4, SEQ], BF16)
                R["Vaug"] = sb2("Vaug", [128, 32, 8, 65], BF16)
                R["ksum"] = sb2("ksum", [128, 4, 16])
                R["kmeanT"] = sb2("kmeanT", [128, 4, 16], BF16)
                self.block_proj(l, x_in, "qkv", R)
                if self.stop_after == ("A1", l):
                    S = Sched(nc)
                    S.dma("sp", self.dbg_out["kT"], R["kT"][:])
                    S.dma("sp", self.dbg_out["Vaug"], R["Vaug"][:])
                    S.dma("sp", self.dbg_out["kmeanT"], R["kmeanT"][:])
                    S.emit()
                    return
                self.block_attn(l, x_in, x_out, R)

    def build_a2only(self, nqb):
        nc, d = self.nc, self.d
        ti = {}
        for name, shp in (("t_ysT", [128, 2, SEQ]), ("t_ypT", [128, 2, SEQ]), ("t_kT", [128, 4, SEQ]), ("t_V", [128, 32, 8, 64]),
                          ("t_kmean", [128, 4, 16]), ("t_q", [4, 128, SEQ])):
            ti[name] = nc.dram_tensor(name, shp, F32, kind="ExternalInput").ap()
        with ExitStack() as es:
            sb = lambda name, shape, dt=F32: es.enter_context(nc.sbuf_tensor(self.nm(name), shape, dt))
            R = {"ysT": sb("ysT", [128, 2, SEQ], BF16), "ypT": sb("ypT", [128, 2, SEQ], BF16), "kT": sb("kT", [128, 4, SEQ], BF16),
                 "Vaug": sb("Vaug", [128, 32, 8, 65], BF16), "ksum": sb("ksum", [128, 4, 16]), "kmeanT": sb("kmeanT", [128, 4, 16], BF16)}
            S = Sched(nc)
            S.dma("pool", R["ysT"][:], ti["t_ysT"]); S.dma("pool", R["ypT"][:], ti["t_ypT"]); S.dma("pool", R["kT"][:], ti["t_kT"])
            for kt4 in range(8):
                S.dma("pool", R["Vaug"][:, 4 * kt4:4 * kt4 + 4, :, 0:64], ti["t_V"][:, 4 * kt4:4 * kt4 + 4], w=["V"])
            S.dma("pool", R["kmeanT"][:], ti["t_kmean"])
            MSET(S, R["Vaug"][:, :, :, 64:65], 1.0, w=["vones"])
            S.dma("pool", self.qT_d, ti["t_q"])
            S.emit()
            self.block_attn(0, d["x"], self.out, R, nqb=nqb)
        return nc

    def build(self):
        d = self.d
        if isinstance(self.stop_after, tuple) and self.stop_after[0] == "A2only":
            return self.build_a2only(self.stop_after[1])
        for l in range(self.n_layers):
            x_in = d["x"] if l == 0 else self.x1
            last = (l == self.n_layers - 1)
            if self.stop_after == "Bonly":
                self.block_B(l, d["x"], self.out, False)
                return self.nc
            if self.stop_after in (("A", l), ("AS", l), ("A0", l), ("A1", l)):
                self.block_A(l, x_in, self.out)
                return self.nc
            self.block_A(l, x_in, self.xmid)
            self.block_B(l, self.xmid, self.out if last else self.x1, last and self.stop_after is None)
        return self.nc


_CONSTS = None


def _prep_inputs(inputs):
    global _CONSTS
    if _CONSTS is None:
        _CONSTS = _consts()
    inp = {k: np.asarray(v) for k, v in inputs.items()}
    lay = _host_layouts(inp)
    shared = {}
    for k in RAW_SHAPES:
        if k != "x":
            shared[k] = np.ascontiguousarray(inp[k], np.float32)
    shared.update(lay)
    shared.update(_CONSTS)
    maps = []
    for b in range(8):
        m = dict(shared)
        m["x"] = np.ascontiguousarray(inp["x"][b], np.float32)
        maps.append(m)
    return maps


def kernel(**inputs):
    maps = _prep_inputs(inputs)
    nc = Prog().build()
    res = run_bass_kernel_spmd(nc, maps, core_ids=list(range(8)))
    return np.stack([np.asarray(r["out"]) for r in res.results], axis=0).astype(np.float32)
```
